# Optimizing a Trainium2 kernel written in Bass

```python
import jax
import jax.numpy as jnp
from jax import lax
import numpy as np

D_MODEL = 1024
BATCH = 8
SEQ = 4096
DEPTH = 2

N_BRANCHES = 4
BRANCH_WIDTH = D_MODEL // N_BRANCHES
LRU_CONV = 4
LRU_BLOCKS = 8
LRU_BLOCK = BRANCH_WIDTH // LRU_BLOCKS
LRU_C = 8.0
POOL_WINDOWS = (2, 4, 8, 16)
POOL_GROUP = BRANCH_WIDTH // len(POOL_WINDOWS)
HGRN_HEADS = 4
HGRN_DK = BRANCH_WIDTH // HGRN_HEADS
HGRN_DV = BRANCH_WIDTH // HGRN_HEADS
HGRN_CHUNK = 64
SCONV_WIDTH = 3
D_FF = 4 * D_MODEL
N_MIX_SLOTS = 10
IN_WIDTH = N_MIX_SLOTS * BRANCH_WIDTH + N_BRANCHES * D_MODEL
EPS = 1e-6

kernel_name = "hybrid_gated_mixer_trunk"


def rms_norm(x, gain):
    xf = x.astype(jnp.float32)
    y = xf * lax.rsqrt(jnp.mean(xf * xf, axis=-1, keepdims=True) + EPS)
    return (y * gain.astype(jnp.float32)).astype(x.dtype)


def causal_depthwise_conv(x, w):
    k = w.shape[0]
    c = x.shape[-1]
    return lax.conv_general_dilated(
        x, w.astype(x.dtype).reshape(k, 1, c), window_strides=(1,),
        padding=[(k - 1, 0)], dimension_numbers=("NWC", "WIO", "NWC"),
        feature_group_count=c)


def block_diag_linear(x, w, b):
    bsz, t, _ = x.shape
    nb, bi, bo = w.shape
    y = jnp.einsum("btnc,ncd->btnd", x.reshape(bsz, t, nb, bi), w.astype(x.dtype))
    return y.reshape(bsz, t, nb * bo) + b.astype(x.dtype)


def rg_lru_branch(x_in, gate_in, conv_w, conv_b, w_a, b_a, w_x, b_x, lam):
    xc = causal_depthwise_conv(x_in, conv_w) + conv_b.astype(x_in.dtype)
    r = jax.nn.sigmoid(block_diag_linear(xc, w_a, b_a).astype(jnp.float32))
    i = jax.nn.sigmoid(block_diag_linear(xc, w_x, b_x).astype(jnp.float32))
    log_a = -LRU_C * r * jax.nn.softplus(-lam.astype(jnp.float32))
    a = jnp.exp(log_a)
    mult = jnp.sqrt(-jnp.expm1(2.0 * log_a))
    u = mult * i * xc.astype(jnp.float32)

    def combine(left, right):
        a_l, h_l = left
        a_r, h_r = right
        return a_l * a_r, a_r * h_l + h_r

    _, h = lax.associative_scan(combine, (a, u), axis=1)
    return (h * jax.nn.gelu(gate_in.astype(jnp.float32))).astype(x_in.dtype)


def pool_branch(x, w, scale):
    bsz, t, _ = x.shape
    xf = x.astype(jnp.float32)
    cs0 = jnp.pad(jnp.cumsum(xf, axis=1), ((0, 0), (1, 0), (0, 0)))
    pos = jnp.arange(1, t + 1, dtype=jnp.float32)[:, None]
    means = []
    for g, win in enumerate(POOL_WINDOWS):
        c = cs0[..., g * POOL_GROUP:(g + 1) * POOL_GROUP]
        window_sum = c[:, 1:] - jnp.pad(c, ((0, 0), (win - 1, 0), (0, 0)))[:, :t]
        means.append(window_sum / jnp.minimum(pos, float(win)))
    d = (jnp.concatenate(means, axis=-1) - xf).astype(x.dtype)
    d = d.reshape(bsz, t, len(POOL_WINDOWS), POOL_GROUP)
    y = jnp.einsum("btgc,gcd->btgd", d, w.astype(x.dtype)).reshape(bsz, t, BRANCH_WIDTH)
    return y * scale.astype(x.dtype)


def hgrn2_chunked(q, log_f, k, v):
    bsz, t, h, dk = q.shape
    dv = v.shape[-1]
    nc = t // HGRN_CHUNK

    def chunks(z):
        return z.reshape(bsz, nc, HGRN_CHUNK, h, z.shape[-1]).transpose(1, 0, 3, 2, 4)

    causal = jnp.tril(jnp.ones((HGRN_CHUNK, HGRN_CHUNK), dtype=bool))[:, :, None]

    def step(state, inp):
        q_c, lf_c, k_c, v_c = inp
        b = jnp.cumsum(lf_c, axis=2)
        diff = b[:, :, :, None, :] - b[:, :, None, :, :]
        decay = jnp.exp(jnp.where(causal, diff, -jnp.inf))
        scores = jnp.einsum("bhtk,bhsk,bhtsk->bhts", q_c, k_c, decay)
        o = (jnp.einsum("bhts,bhsv->bhtv", scores, v_c)
             + jnp.einsum("bhtk,bhkv->bhtv", q_c * jnp.exp(b), state))
        b_last = b[:, :, -1:, :]
        state = (jnp.exp(b_last[:, :, 0, :, None]) * state
                 + jnp.einsum("bhsk,bhsv->bhkv", k_c * jnp.exp(b_last - b), v_c))
        return state, o

    s0 = jnp.zeros((bsz, h, dk, dv), jnp.float32)
    _, o = lax.scan(step, s0, (chunks(q), chunks(log_f), chunks(k), chunks(v)))
    return o.transpose(1, 0, 3, 2, 4).reshape(bsz, t, h, dv)


def hgrn2_branch(hq, hf, hi, hg, lb, norm_gain):
    bsz, t, _ = hq.shape
    shp_k = (bsz, t, HGRN_HEADS, HGRN_DK)
    lb = lb.reshape(HGRN_HEADS, HGRN_DK)
    f = lb + (1.0 - lb) * jax.nn.sigmoid(hf.astype(jnp.float32).reshape(shp_k))
    o = hgrn2_chunked(hq.astype(jnp.float32).reshape(shp_k), jnp.log(f), 1.0 - f,
                      hi.astype(jnp.float32).reshape(bsz, t, HGRN_HEADS, HGRN_DV))
    o = rms_norm(o, norm_gain).reshape(bsz, t, BRANCH_WIDTH)
    return (o * jax.nn.silu(hg.astype(jnp.float32))).astype(hq.dtype)


def short_conv_branch(gate_b, gate_c, xs, w):
    return gate_b * causal_depthwise_conv(gate_c * xs, w)


def setup_inputs(seed: int = 0) -> dict:
    key = jax.random.key(seed)
    ks = jax.random.split(key, 24)
    f32 = jnp.float32
    W = BRANCH_WIDTH

    def nrm(k, shape, scale):
        return jax.random.normal(k, shape, f32) * scale

    def gain(k, shape):
        return 1.0 + 0.05 * jax.random.normal(k, shape, f32)

    u = jax.random.uniform(ks[12], (DEPTH, W), f32, minval=0.9, maxval=0.999)
    s = u ** (1.0 / LRU_C)
    lam = jnp.log(s) - jnp.log1p(-s)
    return {
        "x": jax.random.normal(ks[0], (BATCH, SEQ, D_MODEL), f32),
        "norm_mix_pre": gain(ks[1], (DEPTH, D_MODEL)),
        "norm_mix_post": gain(ks[2], (DEPTH, D_MODEL)),
        "norm_mlp_pre": gain(ks[3], (DEPTH, D_MODEL)),
        "norm_mlp_post": gain(ks[4], (DEPTH, D_MODEL)),
        "w_in": nrm(ks[5], (DEPTH, D_MODEL, IN_WIDTH), D_MODEL ** -0.5),
        "lru_conv_w": nrm(ks[6], (DEPTH, LRU_CONV, W), LRU_CONV ** -0.5),
        "lru_conv_b": nrm(ks[7], (DEPTH, W), 0.02),
        "lru_w_a": nrm(ks[8], (DEPTH, LRU_BLOCKS, LRU_BLOCK, LRU_BLOCK), LRU_BLOCK ** -0.5),
        "lru_b_a": nrm(ks[9], (DEPTH, W), 0.02),
        "lru_w_x": nrm(ks[10], (DEPTH, LRU_BLOCKS, LRU_BLOCK, LRU_BLOCK), LRU_BLOCK ** -0.5),
        "lru_b_x": nrm(ks[11], (DEPTH, W), 0.02),
        "lru_lambda": lam,
        "pool_w": nrm(ks[13], (DEPTH, len(POOL_WINDOWS), POOL_GROUP, POOL_GROUP), POOL_GROUP ** -0.5),
        "pool_scale": 1.0 + 0.1 * jax.random.normal(ks[14], (DEPTH, W), f32),
        "hgrn_lower_bound": nrm(ks[15], (DEPTH, W), 0.5),
        "hgrn_norm": gain(ks[16], (DEPTH, HGRN_DV)),
        "sconv_w": nrm(ks[17], (DEPTH, SCONV_WIDTH, W), SCONV_WIDTH ** -0.5),
        "w_branch": nrm(ks[18], (DEPTH, N_BRANCHES, W, D_MODEL), W ** -0.5),
        "w_out": nrm(ks[19], (DEPTH, D_MODEL, D_MODEL), D_MODEL ** -0.5),
        "w_up": nrm(ks[20], (DEPTH, D_MODEL, D_FF), D_MODEL ** -0.5),
        "w_down": nrm(ks[21], (DEPTH, D_FF, D_MODEL), D_FF ** -0.5),
    }


def reference(x, norm_mix_pre, norm_mix_post, norm_mlp_pre, norm_mlp_post, w_in,
              lru_conv_w, lru_conv_b, lru_w_a, lru_b_a, lru_w_x, lru_b_x, lru_lambda,
              pool_w, pool_scale, hgrn_lower_bound, hgrn_norm, sconv_w,
              w_branch, w_out, w_up, w_down):
    bsz, t, _ = x.shape
    lb_cum = jnp.cumsum(jax.nn.softmax(hgrn_lower_bound.astype(jnp.float32), axis=0), axis=0)
    lower_bounds = lb_cum - lb_cum[0:1]
    split_points = [BRANCH_WIDTH * (j + 1) for j in range(N_MIX_SLOTS)]
    h = x
    for l in range(DEPTH):
        u = rms_norm(h, norm_mix_pre[l])
        proj = jnp.einsum("btd,dp->btp", u, w_in[l])
        (a_x, a_gate, p_x, c_q, c_f, c_i, c_g, s_b, s_c, s_x,
         gate_logits) = jnp.split(proj, split_points, axis=-1)
        y_a = rg_lru_branch(a_x, a_gate, lru_conv_w[l], lru_conv_b[l], lru_w_a[l], lru_b_a[l],
                            lru_w_x[l], lru_b_x[l], lru_lambda[l])
        y_b = pool_branch(p_x, pool_w[l], pool_scale[l])
        y_c = hgrn2_branch(c_q, c_f, c_i, c_g, lower_bounds[l], hgrn_norm[l])
        y_d = short_conv_branch(s_b, s_c, s_x, sconv_w[l])
        ys = jnp.stack([y_a, y_b, y_c, y_d], axis=2)
        branch_out = jnp.einsum("btkc,kcd->btkd", ys, w_branch[l])
        gates = jax.nn.sigmoid(gate_logits.reshape(bsz, t, N_BRANCHES, D_MODEL))
        merged = jnp.sum(gates * branch_out, axis=2)
        mix = jnp.einsum("btd,de->bte", merged, w_out[l])
        h = h + rms_norm(mix, norm_mix_post[l])
        u = rms_norm(h, norm_mlp_pre[l])
        hid = jnp.square(jax.nn.relu(jnp.einsum("btd,df->btf", u, w_up[l])))
        m = jnp.einsum("btf,fd->btd", hid, w_down[l])
        h = h + rms_norm(m, norm_mlp_post[l])
    return h
```

```python
import numpy as np
from contextlib import ExitStack
import concourse.bass as bass
import concourse.mybir as mybir
from concourse.bass_utils import run_bass_kernel_spmd

F32 = mybir.dt.float32
BF16 = mybir.dt.bfloat16
AF = mybir.ActivationFunctionType
ALU = mybir.AluOpType

P = 128
TT = 512
D = 1024
NSLOT = 4
NPG = 31
PGE = 4096
EPS = 1e-6
TW = 528
NTMP = 28
NCST = 1058
NV = 64
_STAGE = 99


class _Stop(Exception):
    pass


class T:
    __slots__ = ("name", "w", "r")

    def __init__(self, name):
        self.name = name
        self.w = None
        self.r = {}


def _flat(ts):
    for t in ts:
        if isinstance(t, T):
            yield t
        else:
            yield from _flat(t)


class Sch:
    def __init__(self, nc, es):
        self.nc = nc
        self.es = es
        self.E = {"pe": nc.tensor, "act": nc.scalar, "dve": nc.vector, "pool": nc.gpsimd, "sp": nc.sync}
        self.sem = {}
        self.val = {}
        self.waited = {e: {} for e in self.E}
        for e in ("pe", "act", "dve", "pool"):
            self.newsem(e)

    def newsem(self, name):
        self.sem[name] = self.es.enter_context(self.nc.semaphore(name))
        self.val[name] = 0

    def op(self, eng, fn, reads=(), writes=(), dma_sem=None):
        deps = {}
        R = list(_flat(reads))
        Wr = list(_flat(writes))
        for t in R:
            if t.w is not None:
                s, v = t.w
                deps[s] = max(deps.get(s, 0), v)
        for t in Wr:
            if t.w is not None:
                s, v = t.w
                deps[s] = max(deps.get(s, 0), v)
            for s, v in t.r.items():
                deps[s] = max(deps.get(s, 0), v)
        E = self.E[eng]
        for s, v in deps.items():
            if eng == "pe" and s == "pe":
                continue
            if self.waited[eng].get(s, 0) < v:
                E.wait_ge(self.sem[s], v)
                self.waited[eng][s] = v
        inst = fn(E)
        if dma_sem is not None:
            s, inc = dma_sem, 16
        else:
            s, inc = eng, 1
        self.val[s] += inc
        inst.then_inc(self.sem[s], inc)
        v = self.val[s]
        for t in R:
            t.r[s] = v
        for t in Wr:
            t.w = (s, v)
            t.r = {}
        return inst


def build_program(NT, L, dbg=False):
    SEQ = NT * TT
    nc = bass.Bass("TRN2", target_bir_lowering=False)
    x_d = nc.dram_tensor("x", [SEQ, D], F32, kind="ExternalInput").ap()
    w_in_d = nc.dram_tensor("w_in", [L, D, 6656], F32, kind="ExternalInput").ap()
    w_br_d = nc.dram_tensor("w_branch", [L, D, D], F32, kind="ExternalInput").ap()
    w_out_d = nc.dram_tensor("w_out", [L, D, D], F32, kind="ExternalInput").ap()
    w_up_d = nc.dram_tensor("w_up", [L, D, 4096], F32, kind="ExternalInput").ap()
    w_dn_d = nc.dram_tensor("w_down", [L, 4096, D], F32, kind="ExternalInput").ap()
    pv_d = nc.dram_tensor("pvec", [P, L, NV], F32, kind="ExternalInput").ap()
    bd_d = nc.dram_tensor("bdw", [P, L, 6, P], F32, kind="ExternalInput").ap()
    cst_d = nc.dram_tensor("cst", [P, NCST], F32, kind="ExternalInput").ap()
    y_d = nc.dram_tensor("y", [SEQ, D], F32, kind="ExternalOutput").ap()
    wpg_d = nc.dram_tensor("wpg", [L, NPG, P, PGE], BF16, kind="Internal").ap()
    wbp_d = nc.dram_tensor("wbp", [L, 8, P, 1024], BF16, kind="Internal").ap()

    es = ExitStack()
    with es:
        sch = Sch(nc, es)
        for s in range(NSLOT):
            sch.newsem(f"slot{s}")
        for s in range(2):
            sch.newsem(f"wbs{s}")
        for nm in ("cst", "xin", "yout"):
            sch.newsem(nm)
        for j in range(3):
            sch.newsem(f"stg{j}")
        for j in range(2):
            sch.newsem(f"bgi{j}")
            sch.newsem(f"bgo{j}")
        for s in range(NSLOT):
            sch.newsem(f"st{s}")

        def sb(name, shape, dt):
            return es.enter_context(nc.sbuf_tensor(name, shape, dt))

        h_t = sb("h", [P, 8, TT], F32)
        u_t = sb("u", [P, 8, TT], BF16)
        mix_t = sb("mix", [P, 8, TT], F32)
        ysmg_t = sb("ysmg", [P, 16, TT], BF16)
        slots = [sb(f"slot{s}", [P, PGE], BF16) for s in range(NSLOT)]
        wbs = [sb(f"wbs{s}", [P, 1024], BF16) for s in range(2)]
        tmp_t = sb("tmp", [P, NTMP * TW], F32)
        cst_t = sb("cst_sb", [P, NCST], F32)
        pv_t = sb("pv_sb", [P, L, NV], F32)
        bd_t = sb("bd_sb", [P, L, 6, P], F32)
        dv_t = sb("dv_sb", [P, L, 8], F32)
        cbf_t = sb("cbf", [P, 2, P], BF16)
        sqb_t = sb("sqb", [P, 2, TT], BF16)
        rstd_t = sb("rstd", [P, TT], F32)
        vz_t = sb("vz", [P, 4, 4, P], BF16)
        halo_t = sb("halo", [P, L, 2, 20], F32)
        hst_t = sb("hst", [P, L, 2], F32)
        sst_t = sb("sst", [P, L, 2, 64], F32)
        sm_t = sb("sm", [P, 4, 8], F32)
        vtok_t = sb("vtok", [P, 4, 256], BF16)
        bgs_t = sb("bgs", [P, 2, 2048], F32)
        bgo_t = sb("bgo", [P, 2, 2048], BF16)
        bgsT = [T("bgs0"), T("bgs1")]
        bgoT = [T("bgo0"), T("bgo1")]
        psb = [es.enter_context(nc.psum_tensor(f"ps{i}", [P, TT], F32)) for i in range(8)]

        hT = [T(f"h{c}") for c in range(8)]
        uT = [T(f"u{c}") for c in range(8)]
        mixT = [T(f"mix{c}") for c in range(8)]
        ioT = [[mixT[2 * tb], mixT[2 * tb + 1]] for tb in range(4)]
        ysT = [T(f"ys{c}") for c in range(8)]
        mgT = [T(f"mg{c}") for c in range(8)]
        slotT = [T(f"slot{s}") for s in range(NSLOT)]
        wbsT = [T(f"wbs{s}") for s in range(2)]
        tmpT = [T(f"tmp{k}") for k in range(NTMP)]
        hidT = [[tmpT[k] for k in range((fc * 256) // TW, ((fc + 1) * 256 - 1) // TW + 1)] for fc in range(32)]
        psT = [T(f"ps{i}") for i in range(8)]
        cstT = T("cst")
        cbfT = T("cbf")
        dvT = T("dv")
        sqbT = [T("sqb0"), T("sqb1")]
        rstdT = T("rstd")
        vzT = T("vz")
        haloT = [[T(f"halo{l}{c}") for c in range(2)] for l in range(L)]
        hstT = [[T(f"hst{l}{c}") for c in range(2)] for l in range(L)]
        sstT = [[T(f"sst{l}{c}") for c in range(2)] for l in range(L)]
        smT = T("sm")
        vtokT = T("vtok")
        wpgT = [T(f"wpg{l}") for l in range(L)]

        io_ap = mix_t[:].rearrange("p c t -> p (c t)").rearrange("p (tb d) -> p tb d", tb=4)
        hid_ap = tmp_t[:, 0:8192].bitcast(BF16).rearrange("p (f t) -> p f t", t=TT)

        def tm(k, a=0, b=TW):
            return tmp_t[:, k * TW + a:k * TW + b]

        def tmb(k, half):
            return tmp_t[:, k * TW:k * TW + 512].bitcast(BF16)[:, half * 512:(half + 1) * 512]

        ident = cst_t[:, 0:128]
        cmask2 = cst_t[:, 384:512]
        nstart = cst_t[:, 512:1024]
        invc = cst_t[:, 1024:1056].rearrange("p (c t) -> p c t", c=2)
        ones_bf = cbf_t[:, 0, :]
        bones_bf = cbf_t[:, 1, :]

        bank_ctr = [0]

        def bank():
            i = bank_ctr[0] % 8
            bank_ctr[0] += 1
            return psb[i], psT[i]

        def ACT(out, in_, func, R, Wt, bias=None, scale=None):
            kw = {}
            if bias is not None:
                kw["bias"] = bias
            if scale is not None:
                kw["scale"] = scale
            return sch.op("act", lambda e: e.activation(out=out, in_=in_, func=func, **kw), R, Wt)

        def TTOP(out, in0, in1, op, R, Wt, eng="dve"):
            return sch.op(eng, lambda e: e.tensor_tensor(out=out, in0=in0, in1=in1, op=op), R, Wt)

        def TS(out, in0, s1, s2, op0, op1, R, Wt, eng="dve"):
            if op1 is None:
                return sch.op(eng, lambda e: e.tensor_scalar(out=out, in0=in0, scalar1=s1, scalar2=None, op0=op0), R, Wt)
            return sch.op(eng, lambda e: e.tensor_scalar(out=out, in0=in0, scalar1=s1, scalar2=s2, op0=op0, op1=op1), R, Wt)

        def STT(out, in0, sc, in1, op0, op1, R, Wt):
            return sch.op("dve", lambda e: e.scalar_tensor_tensor(out=out, in0=in0, scalar=sc, in1=in1, op0=op0, op1=op1), R, Wt)

        def CP(out, in_, R, Wt, eng="dve"):
            if eng == "act":
                return sch.op("act", lambda e: e.copy(out=out, in_=in_), R, Wt)
            return sch.op(eng, lambda e: e.tensor_copy(out=out, in_=in_), R, Wt)

        def MM(out, lhsT, rhs, st, sp, R, Wt):
            return sch.op("pe", lambda e: e.matmul(out, lhsT=lhsT, rhs=rhs, start=st, stop=sp), R, Wt)

        def TR(out, in_, R, Wt):
            return sch.op("pe", lambda e: e.transpose(out, in_, ident), R + [cstT], Wt)

        n_c = 0
        for dst, src in ((cst_t[:], cst_d), (pv_t[:], pv_d), (bd_t[:], bd_d)):
            nc.sync.dma_start(out=dst, in_=src).then_inc(sch.sem["cst"], 16)
            n_c += 16
        sch.val["cst"] = n_c
        cstT.w = ("cst", n_c)

        NSTG = 3
        stgT = [[tmpT[k] for k in range((j * 4096) // TW, ((j + 1) * 4096 - 1) // TW + 1)] for j in range(NSTG)]
        cvt_ctr = [0]
        cast_engs = ("dve", "act", "pool")
        KH = [(k, hh) for k in (0, 1, 3, 2) for hh in range(2)]
        depT = {}

        def page_pieces(l):
            wi = w_in_d[l]

            def dcp(src2d, pg):
                return [(h * 2048, 2048, (lambda a: a.rearrange("p (dc n) -> p dc n", dc=4)),
                         src2d[h * 512:(h + 1) * 512, :].rearrange("(dc p) n -> p dc n", p=P),
                         wpg_d[l, pg][:, h * 2048:(h + 1) * 2048], ("pg", pg)) for h in range(2)]
            for g in range(5):
                yield dcp(wi[:, g * 512:(g + 1) * 512], g)
            for pi_, (k, hh) in enumerate(KH):
                c0 = 2560 + k * 1024 + hh * 512
                yield dcp(wi[:, c0:c0 + 512], 5 + pi_)
            for hh in range(2):
                yield dcp(w_out_d[l][:, hh * 512:(hh + 1) * 512], 13 + hh)
            for g in range(8):
                yield dcp(w_up_d[l][:, g * 512:(g + 1) * 512], 15 + g)
            for e in range(8):
                yield [(h * 2048, 2048, (lambda a: a.rearrange("p (fc n) -> p fc n", fc=16)),
                        w_dn_d[l][h * 2048:(h + 1) * 2048, e * 128:(e + 1) * 128].rearrange("(fc p) n -> p fc n", p=P),
                        wpg_d[l, 23 + e][:, h * 2048:(h + 1) * 2048], ("pg", 23 + e)) for h in range(2)]
            for half in range(2):
                pcs = []
                for q4 in range(4):
                    k, hh = KH[half * 4 + q4]
                    pcs.append((q4 * 1024, 1024, (lambda a: a.rearrange("p (cc n) -> p cc n", cc=2)),
                                w_br_d[l][k * 256:(k + 1) * 256, hh * 512:(hh + 1) * 512].rearrange("(cc p) n -> p cc n", p=P),
                                wbp_d[l, half * 4 + q4], ("wb", half * 4 + q4)))
                yield pcs

        def convert_fg(pieces):
            n = cvt_ctr[0]
            cvt_ctr[0] += 1
            j = n % NSTG
            s_ = n % NSLOT
            stg = tmp_t[:, j * 4096:(j + 1) * 4096]
            for (off, size, vf, src, dst, key) in pieces:
                sch.op("sp", lambda e: e.dma_start(out=vf(stg[:, off:off + size]), in_=src), [], [stgT[j]], dma_sem=f"stg{j}")
            CP(slots[s_][:], stg, [stgT[j]], [slotT[s_]], eng=cast_engs[n % 3])
            for (off, size, vf, src, dst, key) in pieces:
                sch.op("act", lambda e: e.dma_start(out=dst, in_=slots[s_][:, off:off + size]), [slotT[s_]], [], dma_sem=f"st{s_}")

        bg_ctr = [0]
        bg_pending = [None]

        def bg_flush():
            if bg_pending[0] is None:
                return
            j, ch, base, l = bg_pending[0]
            bg_pending[0] = None
            tot = sum(pc[1] for pc in ch)
            CP(bgo_t[:, j, 0:tot], bgs_t[:, j, 0:tot], [bgsT[j]], [bgoT[j]], eng="pool")
            ts = []
            for (off, size, vf, src, dst, key) in ch:
                t = T("wdep")
                sch.op("pool", lambda e: e.dma_start(out=dst, in_=bgo_t[:, j, off - base:off - base + size]), [bgoT[j]], [t], dma_sem=f"bgo{j}")
                depT.setdefault((l,) + key, []).append(t)
                ts.append(t)
            for t in ts:
                t.w = (f"bgo{j}", sch.val[f"bgo{j}"])

        def convert_bg(l, pieces):
            chunks, cur, cs = [], [], 0
            for pc in pieces:
                if cs + pc[1] > 2048:
                    chunks.append(cur)
                    cur, cs = [], 0
                cur.append(pc)
                cs += pc[1]
            chunks.append(cur)
            for ch in chunks:
                n = bg_ctr[0]
                bg_ctr[0] += 1
                j = n % 2
                base = ch[0][0]
                for (off, size, vf, src, dst, key) in ch:
                    sch.op("pool", lambda e: e.dma_start(out=vf(bgs_t[:, j, off - base:off - base + size]), in_=src), [], [bgsT[j]], dma_sem=f"bgi{j}")
                bg_flush()
                bg_pending[0] = (j, ch, base, l)

        wpgT0 = []
        for l in range(L if _STAGE >= 2 else 0):
            for pieces in page_pieces(l):
                if l == 0:
                    convert_fg(pieces)
                else:
                    convert_bg(l, pieces)
            if l == 0:
                wpgT0 = [T(f"wpg0_{s_}") for s_ in range(NSLOT)]
                for s_ in range(NSLOT):
                    if sch.val[f"st{s_}"] > 0:
                        wpgT0[s_].w = (f"st{s_}", sch.val[f"st{s_}"])
            else:
                bg_flush()

        def wdep(l, kind, idx):
            return wpgT0 if l == 0 else depT.get((l, kind, idx), [])

        sch.op("pool", lambda e: e.memset(halo_t[:], 0.0), [], [haloT])
        sch.op("pool", lambda e: e.memset(hst_t[:], 0.0), [], [hstT])
        sch.op("pool", lambda e: e.memset(sst_t[:], 0.0), [], [sstT])
        sch.op("pool", lambda e: e.memset(vz_t[:], 0.0), [], [vzT])
        CP(cbf_t[:], cst_t[:, 128:384].rearrange("p (a b) -> p a b", a=2), [cstT], [cbfT])
        for l in range(L):
            lam = pv_t[:, l, 46:48]
            ACT(dv_t[:, l, 0:2], lam, AF.Exp, [cstT], [dvT], scale=-1.0)
            TS(dv_t[:, l, 0:2], dv_t[:, l, 0:2], 1.0, None, ALU.add, None, [dvT], [dvT])
            ACT(dv_t[:, l, 0:2], dv_t[:, l, 0:2], AF.Ln, [dvT], [dvT])
            TS(dv_t[:, l, 0:2], dv_t[:, l, 0:2], -8.0, None, ALU.mult, None, [dvT], [dvT])
            if l == 0:
                sch.op("dve", lambda e: e.memset(dv_t[:, l, 2:4], 0.0), [], [dvT])
            else:
                assert l == 1
                TTOP(dv_t[:, l, 2:4], pv_t[:, l, 52:54], pv_t[:, l, 50:52], ALU.subtract, [cstT], [dvT])
                ACT(dv_t[:, l, 2:4], dv_t[:, l, 2:4], AF.Sigmoid, [dvT], [dvT])
            TS(dv_t[:, l, 4:6], dv_t[:, l, 2:4], -1.0, 1.0, ALU.mult, ALU.add, [dvT], [dvT])

        pgq = [(l, pg) for i in range(NT) for l in range(L) for pg in range(NPG)]
        wbq = [(l, e) for i in range(NT) for l in range(L) for e in range(8)]
        lp = [0]
        up_ = [0]
        wlp = [0]
        wup = [0]

        def issue_load():
            if lp[0] >= len(pgq):
                return
            l, pg = pgq[lp[0]]
            s = lp[0] % NSLOT
            sch.op("sp", lambda e: e.dma_start(out=slots[s][:], in_=wpg_d[l, pg]), [wdep(l, "pg", pg)], [slotT[s]], dma_sem=f"slot{s}")
            lp[0] += 1

        def issue_wb():
            if wlp[0] >= len(wbq):
                return
            l, e_ = wbq[wlp[0]]
            s = wlp[0] % 2
            sch.op("sp", lambda e: e.dma_start(out=wbs[s][:], in_=wbp_d[l, e_]), [wdep(l, "wb", e_)], [wbsT[s]], dma_sem=f"wbs{s}")
            wlp[0] += 1

        def next_page():
            s = up_[0] % NSLOT
            up_[0] += 1
            return slots[s], slotT[s]

        def next_wb():
            s = wup[0] % 2
            wup[0] += 1
            return wbs[s], wbsT[s]

        if _STAGE >= 3:
            for _ in range(NSLOT):
                issue_load()
            for _ in range(2):
                issue_wb()

        def norm(src_ap, srcT, gcol, l, mode):
            pb, pbT = bank()
            for c in range(8):
                k = c % 2
                ACT(sqb_t[:, k, :], src_ap(c), AF.Square, [srcT[c]], [sqbT[k]])
                MM(pb[:], ones_bf, sqb_t[:, k, :], c == 0, c == 7, [cbfT, sqbT[k]], [pbT])
            ACT(rstd_t[:], pb[:], AF.Sqrt, [pbT], [rstdT], bias=EPS, scale=1.0 / D)
            sch.op("dve", lambda e: e.reciprocal(out=rstd_t[:], in_=rstd_t[:]), [rstdT], [rstdT])
            for c in range(8):
                g = pv_t[:, l, gcol + c:gcol + c + 1]
                if mode == "u":
                    STT(u_t[:, c, :], src_ap(c), g, rstd_t[:], ALU.mult, ALU.mult, [srcT[c], rstdT, cstT], [uT[c]])
                else:
                    k = 25 + c % 2
                    STT(tm(k, 0, 512), src_ap(c), g, rstd_t[:], ALU.mult, ALU.mult, [srcT[c], rstdT, cstT], [tmpT[k]])
                    TTOP(h_t[:, c, :], h_t[:, c, :], tm(k, 0, 512), ALU.add, [hT[c], tmpT[k]], [hT[c]], eng=pool_eng[0])

        EV = {}
        for c in range(2):
            b0 = c * 9
            EV[c] = dict(xa=b0, ga=b0 + 1, px=b0 + 2, q=b0 + 3, sgm=b0 + 4, sg=b0 + 5, sc=b0 + 6, sb=b0 + 7, z=b0 + 8)
        WK = list(range(18, 28))

        def proj_fm(sl, slT, jj, outb):
            for dc in range(8):
                MM(outb[0][:], sl[:, dc * 512 + jj * 128:dc * 512 + (jj + 1) * 128], u_t[:, dc, :], dc == 0, dc == 7,
                   [slT, uT[dc]], [outb[1]])

        pool_eng = ["pool"]

        def tile_layer(i, l):
            pool_eng[0] = "dve" if (i == 0 and l == 0 and L > 1) else "pool"
            pv = lambda a, b=None: pv_t[:, l, a:(a + 1 if b is None else b)]
            if l == 0:
                sch.op("sp", lambda e: e.dma_start(out=io_ap, in_=x_d[i * TT:(i + 1) * TT, :].rearrange("(tb p) d -> p tb d", p=P)),
                       [], [ioT], dma_sem="xin")
                for c in range(8):
                    pb, pbT = bank()
                    for tb in range(4):
                        TR(pb[:, tb * 128:(tb + 1) * 128], io_ap[:, tb, c * 128:(c + 1) * 128], [ioT[tb]], [pbT])
                    CP(h_t[:, c, :], pb[:], [pbT], [hT[c]], eng=("act" if c % 2 else "dve"))
            norm(lambda c: h_t[:, c, :], hT, 0, l, "u")
            if _STAGE < 4:
                raise _Stop()
            sl, slT = next_page()
            for c in range(2):
                e_ = EV[c]
                b = bank(); proj_fm(sl, slT, c, b)
                CP(tm(e_["xa"], 0, 3), halo_t[:, l, c, 0:3], [haloT[l][c]], [tmpT[e_["xa"]]])
                CP(tm(e_["xa"], 3, 515), b[0][:], [b[1]], [tmpT[e_["xa"]]], eng="act")
                CP(halo_t[:, l, c, 0:3], tm(e_["xa"], 512, 515), [tmpT[e_["xa"]]], [haloT[l][c]])
            for c in range(2):
                e_ = EV[c]
                b = bank(); proj_fm(sl, slT, 2 + c, b)
                ACT(tm(e_["ga"], 0, 512), b[0][:], AF.Gelu_apprx_tanh, [b[1]], [tmpT[e_["ga"]]])
            issue_load()
            sl, slT = next_page()
            for c in range(2):
                e_ = EV[c]
                b = bank(); proj_fm(sl, slT, c, b)
                CP(tm(e_["px"], 0, 15), halo_t[:, l, c, 3:18], [haloT[l][c]], [tmpT[e_["px"]]])
                CP(tm(e_["px"], 15, 527), b[0][:], [b[1]], [tmpT[e_["px"]]], eng="act")
                CP(halo_t[:, l, c, 3:18], tm(e_["px"], 512, 527), [tmpT[e_["px"]]], [haloT[l][c]])
            for c in range(2):
                e_ = EV[c]
                b = bank(); proj_fm(sl, slT, 2 + c, b)
                CP(tm(e_["q"], 0, 512), b[0][:], [b[1]], [tmpT[e_["q"]]], eng="act")
            issue_load()

            xc_k = [WK[0], WK[1]]
            for c in range(2):
                e_ = EV[c]
                xa = e_["xa"]; xc = xc_k[c]
                cw = lambda k: pv(32 + c * 4 + k)
                TS(tm(xc, 0, 512), tm(xa, 0, 512), cw(0), pv(40 + c), ALU.mult, ALU.add, [tmpT[xa], cstT], [tmpT[xc]])
                for k in range(1, 4):
                    STT(tm(xc, 0, 512), tm(xa, k, k + 512), cw(k), tm(xc, 0, 512), ALU.mult, ALU.add, [tmpT[xa], tmpT[xc], cstT], [tmpT[xc]])

            sl, slT = next_page()
            for c in range(2):
                e_ = EV[c]
                b = bank(); proj_fm(sl, slT, c, b)
                ACT(tm(e_["sgm"], 0, 512), b[0][:], AF.Sigmoid, [b[1]], [tmpT[e_["sgm"]]])
            for half in range(2):
                b = bank()
                for t2 in range(2):
                    tb = half * 2 + t2
                    for dc in range(8):
                        MM(b[0][:, t2 * 256:(t2 + 1) * 256], u_t[:, dc, tb * 128:(tb + 1) * 128], sl[:, dc * 512 + 256:dc * 512 + 512],
                           dc == 0, dc == 7, [slT, uT[dc]], [b[1]])
                CP(vtok_t[:, half * 2:half * 2 + 2, :], b[0][:].rearrange("p (a b) -> p a b", a=2), [b[1]], [vtokT])
                bv = b[0][:].rearrange("p (a h v) -> p a h v", a=2, h=4)
                for par in range(2):
                    for a_ in range(2):
                        CP(vz_t[:, half * 2 + a_, par::2, 64 * par:64 * par + 64], bv[:, a_, par::2, :], [b[1]], [vzT], eng=("act" if a_ else "dve"))
            issue_load()

            for c in range(2):
                e_ = EV[c]
                xc = xc_k[c]
                r_, i_, a2_, uu_, hs_ = WK[2], WK[3], WK[4], WK[5], WK[6]
                br = bank()
                MM(br[0][:], bd_t[:, l, 0 + c, :], tm(xc, 0, 512), True, True, [cstT, tmpT[xc]], [br[1]])
                bi = bank()
                MM(bi[0][:], bd_t[:, l, 2 + c, :], tm(xc, 0, 512), True, True, [cstT, tmpT[xc]], [bi[1]])
                ACT(tm(r_, 0, 512), br[0][:], AF.Sigmoid, [br[1], cstT], [tmpT[r_]], bias=pv(42 + c))
                ACT(tm(i_, 0, 512), bi[0][:], AF.Sigmoid, [bi[1], cstT], [tmpT[i_]], bias=pv(44 + c))
                ACT(tm(r_, 0, 512), tm(r_, 0, 512), AF.Exp, [tmpT[r_], dvT], [tmpT[r_]], scale=dv_t[:, l, c:c + 1])
                ACT(tm(a2_, 0, 512), tm(r_, 0, 512), AF.Square, [tmpT[r_]], [tmpT[a2_]])
                ACT(tm(a2_, 0, 512), tm(a2_, 0, 512), AF.Sqrt, [tmpT[a2_]], [tmpT[a2_]], bias=1.0, scale=-1.0)
                TTOP(tm(uu_, 0, 512), tm(a2_, 0, 512), tm(i_, 0, 512), ALU.mult, [tmpT[a2_], tmpT[i_]], [tmpT[uu_]])
                TTOP(tm(uu_, 0, 512), tm(uu_, 0, 512), tm(xc, 0, 512), ALU.mult, [tmpT[uu_], tmpT[xc]], [tmpT[uu_]])
                sch.op("dve", lambda e: e.tensor_tensor_scan(out=tm(hs_, 0, 512), data0=tm(r_, 0, 512), data1=tm(uu_, 0, 512),
                                                              initial=hst_t[:, l, c:c + 1], op0=ALU.mult, op1=ALU.add),
                       [tmpT[r_], tmpT[uu_], hstT[l][c]], [tmpT[hs_]])
                CP(hst_t[:, l, c:c + 1], tm(hs_, 511, 512), [tmpT[hs_]], [hstT[l][c]])
                TTOP(ysmg_t[:, 0 + c, :], tm(hs_, 0, 512), tm(e_["ga"], 0, 512), ALU.mult, [tmpT[hs_], tmpT[e_["ga"]]], [ysT[0 + c]])

            sl, slT = next_page()
            for c in range(2):
                e_ = EV[c]
                b = bank(); proj_fm(sl, slT, c, b)
                ACT(tm(e_["sg"], 0, 512), b[0][:], AF.Silu, [b[1]], [tmpT[e_["sg"]]])
            for c in range(2):
                e_ = EV[c]
                b = bank(); proj_fm(sl, slT, 2 + c, b)
                CP(tm(e_["sb"], 0, 512), b[0][:], [b[1]], [tmpT[e_["sb"]]], eng="act")
            issue_load()

            for c in range(2):
                e_ = EV[c]
                px = e_["px"]
                s2, s4, s8, s16, dd = WK[2], WK[3], WK[4], WK[5], WK[6]
                TTOP(tm(s2, 1, 527), tm(px, 1, 527), tm(px, 0, 526), ALU.add, [tmpT[px]], [tmpT[s2]])
                if c == 0:
                    TTOP(tmp_t[64:128, s4 * TW + 3:s4 * TW + 527], tmp_t[64:128, s2 * TW + 3:s2 * TW + 527],
                         tmp_t[64:128, s2 * TW + 1:s2 * TW + 525], ALU.add, [tmpT[s2]], [tmpT[s4]])
                    sel = [(0, 64, s2, 0.5), (64, 128, s4, 0.25)]
                else:
                    TTOP(tm(s4, 3, 527), tm(s2, 3, 527), tm(s2, 1, 525), ALU.add, [tmpT[s2]], [tmpT[s4]])
                    TTOP(tm(s8, 7, 527), tm(s4, 7, 527), tm(s4, 3, 523), ALU.add, [tmpT[s4]], [tmpT[s8]])
                    TTOP(tmp_t[64:128, s16 * TW + 15:s16 * TW + 527], tmp_t[64:128, s8 * TW + 15:s8 * TW + 527],
                         tmp_t[64:128, s8 * TW + 7:s8 * TW + 519], ALU.add, [tmpT[s8]], [tmpT[s16]])
                    sel = [(0, 64, s8, 0.125), (64, 128, s16, 0.0625)]
                for (p0, p1, sk, inv) in sel:
                    STT(tmp_t[p0:p1, dd * TW:dd * TW + 512], tmp_t[p0:p1, sk * TW + 15:sk * TW + 527], inv,
                        tmp_t[p0:p1, px * TW + 15:px * TW + 527], ALU.mult, ALU.subtract, [tmpT[sk], tmpT[px]], [tmpT[dd]])
                    if i == 0:
                        TTOP(tmp_t[p0:p1, dd * TW:dd * TW + 15], tmp_t[p0:p1, sk * TW + 15:sk * TW + 30], invc[p0:p1, c, 0:15],
                             ALU.mult, [tmpT[sk], cstT, tmpT[dd]], [tmpT[dd]])
                        TTOP(tmp_t[p0:p1, dd * TW:dd * TW + 15], tmp_t[p0:p1, dd * TW:dd * TW + 15], tmp_t[p0:p1, px * TW + 15:px * TW + 30],
                             ALU.subtract, [tmpT[px], tmpT[dd]], [tmpT[dd]])
                bp = bank()
                MM(bp[0][:], bd_t[:, l, 4 + c, :], tm(dd, 0, 512), True, True, [cstT, tmpT[dd]], [bp[1]])
                ACT(ysmg_t[:, 2 + c, :], bp[0][:], AF.Copy, [bp[1], cstT], [ysT[2 + c]], scale=pv(48 + c))

            sl, slT = next_page()
            for c in range(2):
                e_ = EV[c]
                b = bank(); proj_fm(sl, slT, c, b)
                CP(tm(e_["sc"], 0, 512), b[0][:], [b[1]], [tmpT[e_["sc"]]], eng="act")
            for c in range(2):
                e_ = EV[c]
                z = e_["z"]
                b = bank(); proj_fm(sl, slT, 2 + c, b)
                CP(tm(z, 0, 2), halo_t[:, l, c, 18:20], [haloT[l][c]], [tmpT[z]])
                TTOP(tm(z, 2, 514), b[0][:], tm(e_["sc"], 0, 512), ALU.mult, [b[1], tmpT[e_["sc"]]], [tmpT[z]])
                CP(halo_t[:, l, c, 18:20], tm(z, 512, 514), [tmpT[z]], [haloT[l][c]])
            issue_load()
            for c in range(2):
                e_ = EV[c]
                z = e_["z"]; acc = WK[2 + c]
                sw = lambda k: pv(55 + c * 3 + k)
                TS(tm(acc, 0, 512), tm(z, 0, 512), sw(0), None, ALU.mult, None, [tmpT[z], cstT], [tmpT[acc]])
                STT(tm(acc, 0, 512), tm(z, 1, 513), sw(1), tm(acc, 0, 512), ALU.mult, ALU.add, [tmpT[z], tmpT[acc], cstT], [tmpT[acc]])
                STT(tm(acc, 0, 512), tm(z, 2, 514), sw(2), tm(acc, 0, 512), ALU.mult, ALU.add, [tmpT[z], tmpT[acc], cstT], [tmpT[acc]])
                TTOP(ysmg_t[:, 6 + c, :], tm(acc, 0, 512), tm(e_["sb"], 0, 512), ALU.mult, [tmpT[acc], tmpT[e_["sb"]]], [ysT[6 + c]])

            if _STAGE < 5:
                raise _Stop()

            def hgrn_steps():
                for hc in range(2):
                    e_ = EV[hc]
                    q, fk, sg = e_["q"], e_["sgm"], e_["sg"]
                    w0, w1, w2, w3, w4 = WK[0], WK[1], WK[4], WK[5], WK[6]
                    hb = [WK[7], WK[8], WK[2], WK[3], WK[9]]
                    qe_s, qe_i, ke_i, osq = tmb(hb[0], 0), tmb(hb[0], 1), tmb(hb[1], 0), tmb(hb[1], 1)
                    kA, kB = tmb(hb[2], 0), tmb(hb[2], 1)
                    scm = [tmb(hb[3], 0), tmb(hb[3], 1)]
                    sbd = tmp_t[:, hb[4] * TW:hb[4] * TW + 512].bitcast(BF16)
                    v3 = lambda k: tm(k, 0, 512).rearrange("p (n t) -> p n t", t=64)
                    lb = dv_t[:, l, 2 + hc:3 + hc]
                    omlb = dv_t[:, l, 4 + hc:5 + hc]
                    TS(tm(fk, 0, 512), tm(fk, 0, 512), omlb, lb, ALU.mult, ALU.add, [tmpT[fk], dvT], [tmpT[fk]])
                    ACT(tm(w0, 0, 512), tm(fk, 0, 512), AF.Ln, [tmpT[fk]], [tmpT[w0]])
                    TS(tm(fk, 0, 512), tm(fk, 0, 512), -1.0, 1.0, ALU.mult, ALU.add, [tmpT[fk]], [tmpT[fk]])
                    sch.op("dve", lambda e: e.tensor_tensor_scan(out=tm(w1, 0, 512), data0=nstart, data1=tm(w0, 0, 512), initial=0.0,
                                                                  op0=ALU.mult, op1=ALU.add), [cstT, tmpT[w0]], [tmpT[w1]])
                    blast = v3(w1)[:, :, 63]
                    bmid = v3(w1)[:, :, 31]
                    TTOP(v3(w0), blast.unsqueeze(2).to_broadcast([P, 8, 64]), v3(w1), ALU.subtract, [tmpT[w1], tmpT[w0]], [tmpT[w0]])
                    ACT(tm(w2, 0, 512), tm(w1, 0, 512), AF.Exp, [tmpT[w1]], [tmpT[w2]])
                    ACT(tm(w0, 0, 512), tm(w0, 0, 512), AF.Exp, [tmpT[w0]], [tmpT[w0]])
                    ACT(sm_t[:, 0, :], bmid, AF.Exp, [tmpT[w1]], [smT], scale=-1.0)
                    TTOP(sm_t[:, 1, :], bmid, blast, ALU.subtract, [tmpT[w1]], [smT])
                    ACT(sm_t[:, 2, :], sm_t[:, 1, :], AF.Exp, [smT], [smT])
                    TTOP(qe_s, tm(q, 0, 512), tm(w2, 0, 512), ALU.mult, [tmpT[q], tmpT[w2]], [tmpT[hb[0]]])
                    TTOP(tm(w0, 0, 512), tm(fk, 0, 512), tm(w0, 0, 512), ALU.mult, [tmpT[fk], tmpT[w0]], [tmpT[w0]])
                    TTOP(qe_i.rearrange("p (n t) -> p n t", t=64), qe_s.rearrange("p (n t) -> p n t", t=64),
                         sm_t[:, 0, :].unsqueeze(2).to_broadcast([P, 8, 64]), ALU.mult, [tmpT[hb[0]], smT], [tmpT[hb[0]]])
                    TTOP(ke_i.rearrange("p (n t) -> p n t", t=64), v3(w0),
                         sm_t[:, 2, :].unsqueeze(2).to_broadcast([P, 8, 64]), ALU.mult, [tmpT[w0], smT], [tmpT[hb[1]]])
                    dec = v3(w2)[:, :, 63]
                    d3 = tm(w3, 0, 512).rearrange("p (v n) -> p v n", n=8)
                    CP(d3, dec.unsqueeze(1).to_broadcast([P, 64, 8]), [tmpT[w2]], [tmpT[w3]], eng=pool_eng[0])
                    sch.op(pool_eng[0], lambda e: e.memset(d3[:, :, 0], 0.0), [], [tmpT[w3]])
                    yield
                    bt = bank()
                    for tb in range(4):
                        TR(bt[0][:, tb * 128:(tb + 1) * 128], tm(w0, tb * 128, (tb + 1) * 128), [tmpT[w0]], [bt[1]])
                    yield
                    sch.op(pool_eng[0], lambda e: e.memset(kA[64:128, :], 0.0), [], [tmpT[hb[2]]])
                    sch.op(pool_eng[0], lambda e: e.memset(kB[0:64, :], 0.0), [], [tmpT[hb[2]]])
                    CP(kA[0:64, :], bt[0][0:64, :], [bt[1]], [tmpT[hb[2]]], eng="dve")
                    CP(kB[64:128, :], bt[0][64:128, :], [bt[1]], [tmpT[hb[2]]], eng="act")
                    bus = [bank(), bank()]
                    for n in range(8):
                        km = kA if n % 2 == 0 else kB
                        j = n // 2
                        bu = bus[n // 4]
                        MM(bu[0][:, (n % 4) * 128:(n % 4 + 1) * 128], km[:, j * 128:(j + 1) * 128], vtok_t[:, j, hc * 128:(hc + 1) * 128], True, True,
                           [tmpT[hb[2]], vtokT], [bu[1]])
                    yield
                    u3 = tm(w1, 0, 512).rearrange("p (v n) -> p v n", n=8)
                    for bi_ in range(2):
                        for hp in range(2):
                            src = bus[bi_][0][64 * hp:64 * hp + 64, :].rearrange("p (n c) -> p n c", c=128)[:, :, 64 * hp:64 * hp + 64]
                            CP(u3[64 * hp:64 * hp + 64, :, 4 * bi_:4 * bi_ + 4], src.rearrange("p n v -> p v n"), [bus[bi_][1], tmpT[w1]], [tmpT[w1]],
                               eng=("dve" if hp == 0 else "act"))
                    STT(u3[:, :, 0], sst_t[:, l, hc, :], tm(w2, 63, 64), u3[:, :, 0], ALU.mult, ALU.add, [sstT[l][hc], tmpT[w2], tmpT[w1]], [tmpT[w1]])
                    sch.op("dve", lambda e: e.tensor_tensor_scan(out=tm(w4, 0, 512), data0=tm(w3, 0, 512), data1=tm(w1, 0, 512), initial=0.0,
                                                                  op0=ALU.mult, op1=ALU.add), [tmpT[w3], tmpT[w1]], [tmpT[w4]])
                    s3 = tm(w4, 0, 512).rearrange("p (v n) -> p v n", n=8)
                    sbd3 = sbd.rearrange("p (n c) -> p n c", c=128)
                    sch.op(pool_eng[0], lambda e: e.memset(sbd, 0.0), [], [tmpT[hb[4]]])
                    for hp in range(2):
                        ps_ = slice(64 * hp, 64 * hp + 64)
                        CP(sbd3[ps_, 0, 64 * hp:64 * hp + 64], sst_t[ps_, l, hc, :], [sstT[l][hc]], [tmpT[hb[4]]])
                        CP(sbd3[ps_, 1:8, 64 * hp:64 * hp + 64], s3[ps_, :, 0:7].rearrange("p v n -> p n v"), [tmpT[w4]], [tmpT[hb[4]]])
                    CP(sst_t[:, l, hc, :], s3[:, :, 7], [tmpT[w4]], [sstT[l][hc]])
                    bsb = [bank(), bank()]
                    for hp in range(2):
                        for j in range(4):
                            MM(bsb[hp][0][:, j * 128:(j + 1) * 128], ke_i[64 * hp:64 * hp + 64, j * 128:(j + 1) * 128],
                               qe_i[64 * hp:64 * hp + 64, j * 128:(j + 1) * 128], True, True, [tmpT[hb[0]], tmpT[hb[1]]], [bsb[hp][1]])
                    for hp in range(2):
                        TTOP(scm[hp].rearrange("p (a t) -> p a t", t=128), bsb[hp][0][:].rearrange("p (a t) -> p a t", t=128),
                             cmask2.unsqueeze(1).to_broadcast([P, 4, 128]), ALU.mult, [bsb[hp][1], cstT], [tmpT[hb[3]]])
                    yield
                    bo = bank()
                    for j in range(4):
                        out_ = bo[0][:, j * 128:(j + 1) * 128]
                        MM(out_, vz_t[:, j, 2 * hc, :], scm[0][:, j * 128:(j + 1) * 128], True, False, [vzT, tmpT[hb[3]]], [bo[1]])
                        MM(out_, vz_t[:, j, 2 * hc + 1, :], scm[1][:, j * 128:(j + 1) * 128], False, False, [vzT, tmpT[hb[3]]], [bo[1]])
                        for n in (2 * j, 2 * j + 1):
                            MM(bo[0][:, n * 64:(n + 1) * 64], sbd3[:, n, :], qe_s[:, n * 64:(n + 1) * 64], False, n == 2 * j + 1,
                               [tmpT[hb[4]], tmpT[hb[0]]], [bo[1]])
                    yield
                    ACT(osq, bo[0][:], AF.Square, [bo[1]], [tmpT[hb[1]]])
                    bn = bank()
                    MM(bn[0][:], bones_bf, osq, True, True, [cbfT, tmpT[hb[1]]], [bn[1]])
                    ACT(tm(w3, 0, 512), bn[0][:], AF.Sqrt, [bn[1]], [tmpT[w3]], bias=EPS, scale=1.0 / 64)
                    sch.op("dve", lambda e: e.reciprocal(out=tm(w3, 0, 512), in_=tm(w3, 0, 512)), [tmpT[w3]], [tmpT[w3]])
                    TTOP(tm(w4, 0, 512), bo[0][:], tm(w3, 0, 512), ALU.mult, [bo[1], tmpT[w3]], [tmpT[w4]])
                    STT(ysmg_t[:, 4 + hc, :], tm(w4, 0, 512), pv(54), tm(sg, 0, 512), ALU.mult, ALU.mult, [tmpT[w4], tmpT[sg], cstT], [ysT[4 + hc]])
                    yield

            if _STAGE < 6:
                raise _Stop()
            hg = hgrn_steps()
            next(hg, None)
            slot_i = 0
            for ki, k in enumerate((0, 1, 3, 2)):
                if ki == 3:
                    for _ in hg:
                        pass
                for hh in range(2):
                    sl, slT = next_page()
                    wsl, wslT = next_wb()
                    for e4 in range(4):
                        e = hh * 4 + e4
                        gb = bank()
                        for dc in range(8):
                            MM(gb[0][:], sl[:, dc * 512 + e4 * 128:dc * 512 + (e4 + 1) * 128], u_t[:, dc, :], dc == 0, dc == 7, [slT, uT[dc]], [gb[1]])
                        bb = bank()
                        for cc in range(2):
                            MM(bb[0][:], wsl[:, cc * 512 + e4 * 128:cc * 512 + (e4 + 1) * 128], ysmg_t[:, k * 2 + cc, :], cc == 0, cc == 1,
                               [wslT, ysT[k * 2 + cc]], [bb[1]])
                        gs = EV[slot_i % 2]["xa"]
                        mt = EV[slot_i % 2]["ga"]
                        ACT(tm(gs, 0, 512), gb[0][:], AF.Sigmoid, [gb[1]], [tmpT[gs]])
                        if ki == 0:
                            TTOP(mix_t[:, e, :], bb[0][:], tm(gs, 0, 512), ALU.mult, [bb[1], tmpT[gs]], [mixT[e]])
                        else:
                            TTOP(tm(mt, 0, 512), bb[0][:], tm(gs, 0, 512), ALU.mult, [bb[1], tmpT[gs]], [tmpT[mt]])
                            if ki < 3:
                                TTOP(mix_t[:, e, :], mix_t[:, e, :], tm(mt, 0, 512), ALU.add, [mixT[e], tmpT[mt]], [mixT[e]], eng=pool_eng[0])
                            else:
                                TTOP(ysmg_t[:, 8 + e, :], mix_t[:, e, :], tm(mt, 0, 512), ALU.add, [mixT[e], tmpT[mt]], [mgT[e]], eng=pool_eng[0])
                        slot_i += 1
                        if ki < 3 and slot_i % 2 == 0:
                            next(hg, None)
                    issue_load()
                    issue_wb()
            for hh in range(2):
                sl, slT = next_page()
                for e2 in range(4):
                    ec = hh * 4 + e2
                    b = bank()
                    for dc in range(8):
                        MM(b[0][:], sl[:, dc * 512 + e2 * 128:dc * 512 + (e2 + 1) * 128], ysmg_t[:, 8 + dc, :], dc == 0, dc == 7,
                           [slT, mgT[dc]], [b[1]])
                    CP(mix_t[:, ec, :], b[0][:], [b[1]], [mixT[ec]], eng="act")
                issue_load()
            norm(lambda c: mix_t[:, c, :], mixT, 8, l, "res")

            if _STAGE < 7:
                raise _Stop()
            norm(lambda c: h_t[:, c, :], hT, 16, l, "u")
            for g in range(8):
                sl, slT = next_page()
                for j in range(4):
                    fc = g * 4 + j
                    b = bank()
                    for dc in range(8):
                        MM(b[0][:], sl[:, dc * 512 + j * 128:dc * 512 + (j + 1) * 128], u_t[:, dc, :], dc == 0, dc == 7, [slT, uT[dc]], [b[1]])
                    rl = WK[fc % 4]
                    ACT(tm(rl, 0, 512), b[0][:], AF.Relu, [b[1]], [tmpT[rl]])
                    TTOP(hid_ap[:, fc, :], b[0][:], tm(rl, 0, 512), ALU.mult, [b[1], tmpT[rl]], [hidT[fc]])
                issue_load()
            for e in range(8):
                sl, slT = next_page()
                b = bank()
                for fc in range(32):
                    MM(b[0][:], sl[:, fc * 128:(fc + 1) * 128], hid_ap[:, fc, :], fc == 0, fc == 31, [slT, hidT[fc]], [b[1]])
                CP(mix_t[:, e, :], b[0][:], [b[1]], [mixT[e]], eng="act")
                issue_load()
            norm(lambda c: mix_t[:, c, :], mixT, 24, l, "res")

            if l == L - 1:
                for tb in range(4):
                    for half in range(2):
                        b = bank()
                        for cc in range(4):
                            c = half * 4 + cc
                            TR(b[0][:, cc * 128:(cc + 1) * 128], h_t[:, c, tb * 128:(tb + 1) * 128], [hT[c]], [b[1]])
                        CP(io_ap[:, tb, half * 512:(half + 1) * 512], b[0][:], [b[1]], [ioT[tb]], eng=("act" if half else "dve"))
                sch.op("sp", lambda e: e.dma_start(out=y_d[i * TT:(i + 1) * TT, :].rearrange("(tb p) d -> p tb d", p=P), in_=io_ap),
                       [ioT], [], dma_sem="yout")

        try:
            if _STAGE < 3:
                raise _Stop()
            for i in range(NT):
                for l in range(L):
                    tile_layer(i, l)
        except _Stop:
            for nm in sch.sem:
                if sch.val[nm] > 0:
                    nc.sync.wait_ge(sch.sem[nm], sch.val[nm])
        nc.sync.wait_ge(sch.sem["yout"], sch.val["yout"])
        if dbg:
            print("sem values:", sch.val)
    return nc


def _cols(v):
    v = np.asarray(v, np.float32).reshape(-1, P)
    return v.T


def host_consts():
    c = np.zeros((P, NCST), np.float32)
    c[:, 0:128] = np.eye(P, dtype=np.float32)
    c[:, 128:256] = 1.0
    p = np.arange(P)
    c[:, 256:384] = (p[:, None] // 64 == p[None, :] // 64).astype(np.float32)
    c[:, 384:512] = ((p[:, None] // 64 == p[None, :] // 64) & ((p[:, None] % 64) <= (p[None, :] % 64))).astype(np.float32)
    t = np.arange(512)
    c[:, 512:1024] = (t % 64 != 0).astype(np.float32)[None, :]
    wins = np.array([[2, 4], [8, 16]])
    for ch in range(2):
        w = wins[ch][p // 64].astype(np.float32)
        tt = np.arange(16, dtype=np.float32)[None, :] + 1.0
        c[:, 1024 + ch * 16:1024 + (ch + 1) * 16] = 1.0 / np.minimum(tt, w[:, None])
    return c


def host_params(inp, L):
    pv = np.zeros((P, L, NV), np.float32)
    bd = np.zeros((P, L, 6, P), np.float32)
    for l in range(L):
        pv[:, l, 0:8] = _cols(inp["norm_mix_pre"][l])
        pv[:, l, 8:16] = _cols(inp["norm_mix_post"][l])
        pv[:, l, 16:24] = _cols(inp["norm_mlp_pre"][l])
        pv[:, l, 24:32] = _cols(inp["norm_mlp_post"][l])
        cw = np.asarray(inp["lru_conv_w"][l], np.float32)
        for c in range(2):
            for k in range(4):
                pv[:, l, 32 + c * 4 + k] = cw[k, c * 128:(c + 1) * 128]
        pv[:, l, 40:42] = _cols(inp["lru_conv_b"][l])
        pv[:, l, 42:44] = _cols(inp["lru_b_a"][l])
        pv[:, l, 44:46] = _cols(inp["lru_b_x"][l])
        pv[:, l, 46:48] = _cols(inp["lru_lambda"][l])
        pv[:, l, 48:50] = _cols(inp["pool_scale"][l])
        pv[:, l, 50:52] = _cols(inp["hgrn_lower_bound"][0])
        pv[:, l, 52:54] = _cols(inp["hgrn_lower_bound"][min(1, L - 1)])
        pv[:, l, 54] = np.tile(np.asarray(inp["hgrn_norm"][l], np.float32), 2)
        sw = np.asarray(inp["sconv_w"][l], np.float32)
        for c in range(2):
            for k in range(3):
                pv[:, l, 55 + c * 3 + k] = sw[k, c * 128:(c + 1) * 128]
        wa = np.asarray(inp["lru_w_a"][l], np.float32)
        wx = np.asarray(inp["lru_w_x"][l], np.float32)
        pw = np.asarray(inp["pool_w"][l], np.float32)
        for c in range(2):
            for n in range(4):
                bd[n * 32:(n + 1) * 32, l, 0 + c, n * 32:(n + 1) * 32] = wa[c * 4 + n]
                bd[n * 32:(n + 1) * 32, l, 2 + c, n * 32:(n + 1) * 32] = wx[c * 4 + n]
            for n in range(2):
                bd[n * 64:(n + 1) * 64, l, 4 + c, n * 64:(n + 1) * 64] = pw[c * 2 + n]
    return pv, bd


def make_in_maps(inp, n_cores, L):
    pv, bd = host_params(inp, L)
    cst = host_consts()
    f = lambda a: np.ascontiguousarray(np.asarray(a, np.float32))
    shared = {
        "w_in": f(inp["w_in"])[:L], "w_branch": f(inp["w_branch"])[:L].reshape(L, D, D), "w_out": f(inp["w_out"])[:L],
        "w_up": f(inp["w_up"])[:L], "w_down": f(inp["w_down"])[:L], "pvec": pv, "bdw": bd, "cst": cst,
    }
    x = f(inp["x"])
    return [dict(shared, x=np.ascontiguousarray(x[b])) for b in range(n_cores)]


def kernel(**inputs):
    B, S, _ = inputs["x"].shape
    L = inputs["w_in"].shape[0]
    nc = build_program(S // TT, L)
    in_maps = make_in_maps(inputs, B, L)
    res = run_bass_kernel_spmd(nc, in_maps, core_ids=list(range(B)))
    return np.stack([np.asarray(r["y"], np.float32) for r in res.results], axis=0)
```

```python
import numpy as np
from contextlib import ExitStack
import concourse.bass as bass
import concourse.mybir as mybir
from concourse.bass_utils import run_bass_kernel_spmd

F32 = mybir.dt.float32
BF16 = mybir.dt.bfloat16
AF = mybir.ActivationFunctionType
ALU = mybir.AluOpType

P = 128
TT = 512
D = 1024
NSLOT = 4
NPG = 31
PGE = 4096
EPS = 1e-6
TW = 528
NTMP = 28
NCST = 1058
NV = 64
_STAGE = 99


class _Stop(Exception):
    pass


class T:
    __slots__ = ("name", "w", "r")

    def __init__(self, name):
        self.name = name
        self.w = None
        self.r = {}


def _flat(ts):
    for t in ts:
        if isinstance(t, T):
            yield t
        else:
            yield from _flat(t)


class Sch:
    def __init__(self, nc, es):
        self.nc = nc
        self.es = es
        self.E = {"pe": nc.tensor, "act": nc.scalar, "dve": nc.vector, "pool": nc.gpsimd, "sp": nc.sync}
        self.sem = {}
        self.val = {}
        self.waited = {e: {} for e in self.E}
        for e in ("pe", "act", "dve", "pool"):
            self.newsem(e)

    def newsem(self, name):
        self.sem[name] = self.es.enter_context(self.nc.semaphore(name))
        self.val[name] = 0

    def op(self, eng, fn, reads=(), writes=(), dma_sem=None):
        deps = {}
        R = list(_flat(reads))
        Wr = list(_flat(writes))
        for t in R:
            if t.w is not None:
                s, v = t.w
                deps[s] = max(deps.get(s, 0), v)
        for t in Wr:
            if t.w is not None:
                s, v = t.w
                deps[s] = max(deps.get(s, 0), v)
            for s, v in t.r.items():
                deps[s] = max(deps.get(s, 0), v)
        E = self.E[eng]
        for s, v in deps.items():
            if eng == "pe" and s == "pe":
                continue
            if self.waited[eng].get(s, 0) < v:
                E.wait_ge(self.sem[s], v)
                self.waited[eng][s] = v
        inst = fn(E)
        if dma_sem is not None:
            s, inc = dma_sem, 16
        else:
            s, inc = eng, 1
        self.val[s] += inc
        inst.then_inc(self.sem[s], inc)
        v = self.val[s]
        for t in R:
            t.r[s] = v
        for t in Wr:
            t.w = (s, v)
            t.r = {}
        return inst


def build_program(NT, L, dbg=False):
    SEQ = NT * TT
    nc = bass.Bass("TRN2", target_bir_lowering=False)
    x_d = nc.dram_tensor("x", [SEQ, D], F32, kind="ExternalInput").ap()
    w_in_d = nc.dram_tensor("w_in", [L, D, 6656], F32, kind="ExternalInput").ap()
    w_br_d = nc.dram_tensor("w_branch", [L, D, D], F32, kind="ExternalInput").ap()
    w_out_d = nc.dram_tensor("w_out", [L, D, D], F32, kind="ExternalInput").ap()
    w_up_d = nc.dram_tensor("w_up", [L, D, 4096], F32, kind="ExternalInput").ap()
    w_dn_d = nc.dram_tensor("w_down", [L, 4096, D], F32, kind="ExternalInput").ap()
    pv_d = nc.dram_tensor("pvec", [P, L, NV], F32, kind="ExternalInput").ap()
    bd_d = nc.dram_tensor("bdw", [P, L, 6, P], F32, kind="ExternalInput").ap()
    cst_d = nc.dram_tensor("cst", [P, NCST], F32, kind="ExternalInput").ap()
    y_d = nc.dram_tensor("y", [SEQ, D], F32, kind="ExternalOutput").ap()
    wpg_d = nc.dram_tensor("wpg", [L, NPG, P, PGE], BF16, kind="Internal").ap()
    wbp_d = nc.dram_tensor("wbp", [L, 8, P, 1024], BF16, kind="Internal").ap()

    es = ExitStack()
    with es:
        sch = Sch(nc, es)
        for s in range(NSLOT):
            sch.newsem(f"slot{s}")
        for s in range(2):
            sch.newsem(f"wbs{s}")
        for nm in ("cst", "xin", "yout"):
            sch.newsem(nm)
        for j in range(3):
            sch.newsem(f"stg{j}")
        for j in range(2):
            sch.newsem(f"bgi{j}")
            sch.newsem(f"bgo{j}")
        for s in range(NSLOT):
            sch.newsem(f"st{s}")

        def sb(name, shape, dt):
            return es.enter_context(nc.sbuf_tensor(name, shape, dt))

        h_t = sb("h", [P, 8, TT], F32)
        u_t = sb("u", [P, 8, TT], BF16)
        mix_t = sb("mix", [P, 8, TT], F32)
        ysmg_t = sb("ysmg", [P, 16, TT], BF16)
        slots = [sb(f"slot{s}", [P, PGE], BF16) for s in range(NSLOT)]
        wbs = [sb(f"wbs{s}", [P, 1024], BF16) for s in range(2)]
        tmp_t = sb("tmp", [P, NTMP * TW], F32)
        cst_t = sb("cst_sb", [P, NCST], F32)
        pv_t = sb("pv_sb", [P, L, NV], F32)
        bd_t = sb("bd_sb", [P, L, 6, P], F32)
        dv_t = sb("dv_sb", [P, L, 8], F32)
        cbf_t = sb("cbf", [P, 2, P], BF16)
        sqb_t = sb("sqb", [P, 2, TT], BF16)
        rstd_t = sb("rstd", [P, TT], F32)
        vz_t = sb("vz", [P, 4, 4, P], BF16)
        halo_t = sb("halo", [P, L, 2, 20], F32)
        hst_t = sb("hst", [P, L, 2], F32)
        sst_t = sb("sst", [P, L, 2, 64], F32)
        sm_t = sb("sm", [P, 4, 8], F32)
        vtok_t = sb("vtok", [P, 4, 256], BF16)
        bgs_t = sb("bgs", [P, 2, 2048], F32)
        bgo_t = sb("bgo", [P, 2, 2048], BF16)
        bgsT = [T("bgs0"), T("bgs1")]
        bgoT = [T("bgo0"), T("bgo1")]
        psb = [es.enter_context(nc.psum_tensor(f"ps{i}", [P, TT], F32)) for i in range(8)]

        hT = [T(f"h{c}") for c in range(8)]
        uT = [T(f"u{c}") for c in range(8)]
        mixT = [T(f"mix{c}") for c in range(8)]
        ioT = [[mixT[2 * tb], mixT[2 * tb + 1]] for tb in range(4)]
        ysT = [T(f"ys{c}") for c in range(8)]
        mgT = [T(f"mg{c}") for c in range(8)]
        slotT = [T(f"slot{s}") for s in range(NSLOT)]
        wbsT = [T(f"wbs{s}") for s in range(2)]
        tmpT = [T(f"tmp{k}") for k in range(NTMP)]
        hidT = [[tmpT[k] for k in range((fc * 256) // TW, ((fc + 1) * 256 - 1) // TW + 1)] for fc in range(32)]
        psT = [T(f"ps{i}") for i in range(8)]
        cstT = T("cst")
        cbfT = T("cbf")
        dvT = T("dv")
        sqbT = [T("sqb0"), T("sqb1")]
        rstdT = T("rstd")
        vzT = T("vz")
        haloT = [[T(f"halo{l}{c}") for c in range(2)] for l in range(L)]
        hstT = [[T(f"hst{l}{c}") for c in range(2)] for l in range(L)]
        sstT = [[T(f"sst{l}{c}") for c in range(2)] for l in range(L)]
        smT = T("sm")
        vtokT = T("vtok")
        wpgT = [T(f"wpg{l}") for l in range(L)]

        io_ap = mix_t[:].rearrange("p c t -> p (c t)").rearrange("p (tb d) -> p tb d", tb=4)
        hid_ap = tmp_t[:, 0:8192].bitcast(BF16).rearrange("p (f t) -> p f t", t=TT)

        def tm(k, a=0, b=TW):
            return tmp_t[:, k * TW + a:k * TW + b]

        def tmb(k, half):
            return tmp_t[:, k * TW:k * TW + 512].bitcast(BF16)[:, half * 512:(half + 1) * 512]

        ident = cst_t[:, 0:128]
        cmask2 = cst_t[:, 384:512]
        nstart = cst_t[:, 512:1024]
        invc = cst_t[:, 1024:1056].rearrange("p (c t) -> p c t", c=2)
        ones_bf = cbf_t[:, 0, :]
        bones_bf = cbf_t[:, 1, :]

        bank_ctr = [0]

        def bank():
            i = bank_ctr[0] % 8
            bank_ctr[0] += 1
            return psb[i], psT[i]

        def ACT(out, in_, func, R, Wt, bias=None, scale=None):
            kw = {}
            if bias is not None:
                kw["bias"] = bias
            if scale is not None:
                kw["scale"] = scale
            return sch.op("act", lambda e: e.activation(out=out, in_=in_, func=func, **kw), R, Wt)

        def TTOP(out, in0, in1, op, R, Wt, eng="dve"):
            return sch.op(eng, lambda e: e.tensor_tensor(out=out, in0=in0, in1=in1, op=op), R, Wt)

        def TS(out, in0, s1, s2, op0, op1, R, Wt, eng="dve"):
            if op1 is None:
                return sch.op(eng, lambda e: e.tensor_scalar(out=out, in0=in0, scalar1=s1, scalar2=None, op0=op0), R, Wt)
            return sch.op(eng, lambda e: e.tensor_scalar(out=out, in0=in0, scalar1=s1, scalar2=s2, op0=op0, op1=op1), R, Wt)

        def STT(out, in0, sc, in1, op0, op1, R, Wt):
            return sch.op("dve", lambda e: e.scalar_tensor_tensor(out=out, in0=in0, scalar=sc, in1=in1, op0=op0, op1=op1), R, Wt)

        def CP(out, in_, R, Wt, eng="dve"):
            if eng == "act":
                return sch.op("act", lambda e: e.copy(out=out, in_=in_), R, Wt)
            return sch.op(eng, lambda e: e.tensor_copy(out=out, in_=in_), R, Wt)

        def MM(out, lhsT, rhs, st, sp, R, Wt):
            return sch.op("pe", lambda e: e.matmul(out, lhsT=lhsT, rhs=rhs, start=st, stop=sp), R, Wt)

        def TR(out, in_, R, Wt):
            return sch.op("pe", lambda e: e.transpose(out, in_, ident), R + [cstT], Wt)

        n_c = 0
        for dst, src in ((cst_t[:], cst_d), (pv_t[:], pv_d), (bd_t[:], bd_d)):
            nc.sync.dma_start(out=dst, in_=src).then_inc(sch.sem["cst"], 16)
            n_c += 16
        sch.val["cst"] = n_c
        cstT.w = ("cst", n_c)

        sch.op("pool", lambda e: e.memset(halo_t[:], 0.0), [], [haloT])
        sch.op("pool", lambda e: e.memset(hst_t[:], 0.0), [], [hstT])
        sch.op("pool", lambda e: e.memset(sst_t[:], 0.0), [], [sstT])
        sch.op("pool", lambda e: e.memset(vz_t[:], 0.0), [], [vzT])
        CP(cbf_t[:], cst_t[:, 128:384].rearrange("p (a b) -> p a b", a=2), [cstT], [cbfT])
        for l in range(L):
            lam = pv_t[:, l, 46:48]
            ACT(dv_t[:, l, 0:2], lam, AF.Exp, [cstT], [dvT], scale=-1.0)
            TS(dv_t[:, l, 0:2], dv_t[:, l, 0:2], 1.0, None, ALU.add, None, [dvT], [dvT])
            ACT(dv_t[:, l, 0:2], dv_t[:, l, 0:2], AF.Ln, [dvT], [dvT])
            TS(dv_t[:, l, 0:2], dv_t[:, l, 0:2], -8.0, None, ALU.mult, None, [dvT], [dvT])
            if l == 0:
                sch.op("dve", lambda e: e.memset(dv_t[:, l, 2:4], 0.0), [], [dvT])
            else:
                assert l == 1
                TTOP(dv_t[:, l, 2:4], pv_t[:, l, 52:54], pv_t[:, l, 50:52], ALU.subtract, [cstT], [dvT])
                ACT(dv_t[:, l, 2:4], dv_t[:, l, 2:4], AF.Sigmoid, [dvT], [dvT])
            TS(dv_t[:, l, 4:6], dv_t[:, l, 2:4], -1.0, 1.0, ALU.mult, ALU.add, [dvT], [dvT])

        NSTG = 3
        stgT = [[tmpT[k] for k in range((j * 4096) // TW, ((j + 1) * 4096 - 1) // TW + 1)] for j in range(NSTG)]
        cvt_ctr = [0]
        cast_engs = ("dve", "act", "pool")
        KH = [(k, hh) for k in (0, 1, 3, 2) for hh in range(2)]
        depT = {}

        def page_pieces(l):
            wi = w_in_d[l]

            def dcp(src2d, pg):
                return [(h * 2048, 2048, (lambda a: a.rearrange("p (dc n) -> p dc n", dc=4)),
                         src2d[h * 512:(h + 1) * 512, :].rearrange("(dc p) n -> p dc n", p=P),
                         wpg_d[l, pg][:, h * 2048:(h + 1) * 2048], ("pg", pg)) for h in range(2)]
            for g in range(5):
                yield dcp(wi[:, g * 512:(g + 1) * 512], g)
            for pi_, (k, hh) in enumerate(KH):
                c0 = 2560 + k * 1024 + hh * 512
                yield dcp(wi[:, c0:c0 + 512], 5 + pi_)
            for hh in range(2):
                yield dcp(w_out_d[l][:, hh * 512:(hh + 1) * 512], 13 + hh)
            for g in range(8):
                yield dcp(w_up_d[l][:, g * 512:(g + 1) * 512], 15 + g)
            for e in range(8):
                yield [(h * 2048, 2048, (lambda a: a.rearrange("p (fc n) -> p fc n", fc=16)),
                        w_dn_d[l][h * 2048:(h + 1) * 2048, e * 128:(e + 1) * 128].rearrange("(fc p) n -> p fc n", p=P),
                        wpg_d[l, 23 + e][:, h * 2048:(h + 1) * 2048], ("pg", 23 + e)) for h in range(2)]
            for half in range(2):
                pcs = []
                for q4 in range(4):
                    k, hh = KH[half * 4 + q4]
                    pcs.append((q4 * 1024, 1024, (lambda a: a.rearrange("p (cc n) -> p cc n", cc=2)),
                                w_br_d[l][k * 256:(k + 1) * 256, hh * 512:(hh + 1) * 512].rearrange("(cc p) n -> p cc n", p=P),
                                wbp_d[l, half * 4 + q4], ("wb", half * 4 + q4)))
                yield pcs

        def convert_fg(pieces):
            n = cvt_ctr[0]
            cvt_ctr[0] += 1
            j = n % NSTG
            s_ = n % NSLOT
            stg = tmp_t[:, j * 4096:(j + 1) * 4096]
            for (off, size, vf, src, dst, key) in pieces:
                sch.op("sp", lambda e: e.dma_start(out=vf(stg[:, off:off + size]), in_=src), [], [stgT[j]], dma_sem=f"stg{j}")
            CP(slots[s_][:], stg, [stgT[j]], [slotT[s_]], eng=cast_engs[n % 3])
            for (off, size, vf, src, dst, key) in pieces:
                sch.op("act", lambda e: e.dma_start(out=dst, in_=slots[s_][:, off:off + size]), [slotT[s_]], [], dma_sem=f"st{s_}")

        bg_ctr = [0]
        bg_pending = [None]

        def bg_flush():
            if bg_pending[0] is None:
                return
            j, ch, base, l = bg_pending[0]
            bg_pending[0] = None
            tot = sum(pc[1] for pc in ch)
            CP(bgo_t[:, j, 0:tot], bgs_t[:, j, 0:tot], [bgsT[j]], [bgoT[j]], eng="pool")
            ts = []
            for (off, size, vf, src, dst, key) in ch:
                t = T("wdep")
                sch.op("pool", lambda e: e.dma_start(out=dst, in_=bgo_t[:, j, off - base:off - base + size]), [bgoT[j]], [t], dma_sem=f"bgo{j}")
                depT.setdefault((l,) + key, []).append(t)
                ts.append(t)
            for t in ts:
                t.w = (f"bgo{j}", sch.val[f"bgo{j}"])

        def convert_bg(l, pieces):
            chunks, cur, cs = [], [], 0
            for pc in pieces:
                if cs + pc[1] > 2048:
                    chunks.append(cur)
                    cur, cs = [], 0
                cur.append(pc)
                cs += pc[1]
            chunks.append(cur)
            for ch in chunks:
                n = bg_ctr[0]
                bg_ctr[0] += 1
                j = n % 2
                base = ch[0][0]
                for (off, size, vf, src, dst, key) in ch:
                    sch.op("pool", lambda e: e.dma_start(out=vf(bgs_t[:, j, off - base:off - base + size]), in_=src), [], [bgsT[j]], dma_sem=f"bgi{j}")
                bg_flush()
                bg_pending[0] = (j, ch, base, l)

        wpgT0 = []
        for l in range(L if _STAGE >= 2 else 0):
            for pieces in page_pieces(l):
                if l == 0:
                    convert_fg(pieces)
                else:
                    convert_bg(l, pieces)
            if l == 0:
                wpgT0 = [T(f"wpg0_{s_}") for s_ in range(NSLOT)]
                for s_ in range(NSLOT):
                    if sch.val[f"st{s_}"] > 0:
                        wpgT0[s_].w = (f"st{s_}", sch.val[f"st{s_}"])
            else:
                bg_flush()

        def wdep(l, kind, idx):
            return wpgT0 if l == 0 else depT.get((l, kind, idx), [])

        pgq = [(l, pg) for i in range(NT) for l in range(L) for pg in range(NPG)]
        wbq = [(l, e) for i in range(NT) for l in range(L) for e in range(8)]
        lp = [0]
        up_ = [0]
        wlp = [0]
        wup = [0]

        def issue_load():
            if lp[0] >= len(pgq):
                return
            l, pg = pgq[lp[0]]
            s = lp[0] % NSLOT
            sch.op("sp", lambda e: e.dma_start(out=slots[s][:], in_=wpg_d[l, pg]), [wdep(l, "pg", pg)], [slotT[s]], dma_sem=f"slot{s}")
            lp[0] += 1

        def issue_wb():
            if wlp[0] >= len(wbq):
                return
            l, e_ = wbq[wlp[0]]
            s = wlp[0] % 2
            sch.op("sp", lambda e: e.dma_start(out=wbs[s][:], in_=wbp_d[l, e_]), [wdep(l, "wb", e_)], [wbsT[s]], dma_sem=f"wbs{s}")
            wlp[0] += 1

        def next_page():
            s = up_[0] % NSLOT
            up_[0] += 1
            return slots[s], slotT[s]

        def next_wb():
            s = wup[0] % 2
            wup[0] += 1
            return wbs[s], wbsT[s]

        if _STAGE >= 3:
            for _ in range(NSLOT):
                issue_load()
            for _ in range(2):
                issue_wb()

        def norm(src_ap, srcT, gcol, l, mode):
            pb, pbT = bank()
            for c in range(8):
                k = c % 2
                ACT(sqb_t[:, k, :], src_ap(c), AF.Square, [srcT[c]], [sqbT[k]])
                MM(pb[:], ones_bf, sqb_t[:, k, :], c == 0, c == 7, [cbfT, sqbT[k]], [pbT])
            ACT(rstd_t[:], pb[:], AF.Sqrt, [pbT], [rstdT], bias=EPS, scale=1.0 / D)
            sch.op("dve", lambda e: e.reciprocal(out=rstd_t[:], in_=rstd_t[:]), [rstdT], [rstdT])
            for c in range(8):
                g = pv_t[:, l, gcol + c:gcol + c + 1]
                if mode == "u":
                    STT(u_t[:, c, :], src_ap(c), g, rstd_t[:], ALU.mult, ALU.mult, [srcT[c], rstdT, cstT], [uT[c]])
                else:
                    k = 25 + c % 2
                    STT(tm(k, 0, 512), src_ap(c), g, rstd_t[:], ALU.mult, ALU.mult, [srcT[c], rstdT, cstT], [tmpT[k]])
                    TTOP(h_t[:, c, :], h_t[:, c, :], tm(k, 0, 512), ALU.add, [hT[c], tmpT[k]], [hT[c]], eng=pool_eng[0])

        EV = {}
        for c in range(2):
            b0 = c * 9
            EV[c] = dict(xa=b0, ga=b0 + 1, px=b0 + 2, q=b0 + 3, sgm=b0 + 4, sg=b0 + 5, sc=b0 + 6, sb=b0 + 7, z=b0 + 8)
        WK = list(range(18, 28))

        def proj_fm(sl, slT, jj, outb):
            for dc in range(8):
                MM(outb[0][:], sl[:, dc * 512 + jj * 128:dc * 512 + (jj + 1) * 128], u_t[:, dc, :], dc == 0, dc == 7,
                   [slT, uT[dc]], [outb[1]])

        pool_eng = ["pool"]

        def tile_layer(i, l):
            pool_eng[0] = "dve" if (i == 0 and l == 0 and L > 1) else "pool"
            pv = lambda a, b=None: pv_t[:, l, a:(a + 1 if b is None else b)]
            if l == 0:
                sch.op("sp", lambda e: e.dma_start(out=io_ap, in_=x_d[i * TT:(i + 1) * TT, :].rearrange("(tb p) d -> p tb d", p=P)),
                       [], [ioT], dma_sem="xin")
                for c in range(8):
                    pb, pbT = bank()
                    for tb in range(4):
                        TR(pb[:, tb * 128:(tb + 1) * 128], io_ap[:, tb, c * 128:(c + 1) * 128], [ioT[tb]], [pbT])
                    CP(h_t[:, c, :], pb[:], [pbT], [hT[c]], eng=("act" if c % 2 else "dve"))
            norm(lambda c: h_t[:, c, :], hT, 0, l, "u")
            if _STAGE < 4:
                raise _Stop()
            sl, slT = next_page()
            for c in range(2):
                e_ = EV[c]
                b = bank(); proj_fm(sl, slT, c, b)
                CP(tm(e_["xa"], 0, 3), halo_t[:, l, c, 0:3], [haloT[l][c]], [tmpT[e_["xa"]]])
                CP(tm(e_["xa"], 3, 515), b[0][:], [b[1]], [tmpT[e_["xa"]]], eng="act")
                CP(halo_t[:, l, c, 0:3], tm(e_["xa"], 512, 515), [tmpT[e_["xa"]]], [haloT[l][c]])
            for c in range(2):
                e_ = EV[c]
                b = bank(); proj_fm(sl, slT, 2 + c, b)
                ACT(tm(e_["ga"], 0, 512), b[0][:], AF.Gelu_apprx_tanh, [b[1]], [tmpT[e_["ga"]]])
            issue_load()
            sl, slT = next_page()
            for c in range(2):
                e_ = EV[c]
                b = bank(); proj_fm(sl, slT, c, b)
                CP(tm(e_["px"], 0, 15), halo_t[:, l, c, 3:18], [haloT[l][c]], [tmpT[e_["px"]]])
                CP(tm(e_["px"], 15, 527), b[0][:], [b[1]], [tmpT[e_["px"]]], eng="act")
                CP(halo_t[:, l, c, 3:18], tm(e_["px"], 512, 527), [tmpT[e_["px"]]], [haloT[l][c]])
            for c in range(2):
                e_ = EV[c]
                b = bank(); proj_fm(sl, slT, 2 + c, b)
                CP(tm(e_["q"], 0, 512), b[0][:], [b[1]], [tmpT[e_["q"]]], eng="act")
            issue_load()

            xc_k = [WK[0], WK[1]]
            for c in range(2):
                e_ = EV[c]
                xa = e_["xa"]; xc = xc_k[c]
                cw = lambda k: pv(32 + c * 4 + k)
                TS(tm(xc, 0, 512), tm(xa, 0, 512), cw(0), pv(40 + c), ALU.mult, ALU.add, [tmpT[xa], cstT], [tmpT[xc]])
                for k in range(1, 4):
                    STT(tm(xc, 0, 512), tm(xa, k, k + 512), cw(k), tm(xc, 0, 512), ALU.mult, ALU.add, [tmpT[xa], tmpT[xc], cstT], [tmpT[xc]])

            sl, slT = next_page()
            for c in range(2):
                e_ = EV[c]
                b = bank(); proj_fm(sl, slT, c, b)
                ACT(tm(e_["sgm"], 0, 512), b[0][:], AF.Sigmoid, [b[1]], [tmpT[e_["sgm"]]])
            for half in range(2):
                b = bank()
                for t2 in range(2):
                    tb = half * 2 + t2
                    for dc in range(8):
                        MM(b[0][:, t2 * 256:(t2 + 1) * 256], u_t[:, dc, tb * 128:(tb + 1) * 128], sl[:, dc * 512 + 256:dc * 512 + 512],
                           dc == 0, dc == 7, [slT, uT[dc]], [b[1]])
                CP(vtok_t[:, half * 2:half * 2 + 2, :], b[0][:].rearrange("p (a b) -> p a b", a=2), [b[1]], [vtokT])
                bv = b[0][:].rearrange("p (a h v) -> p a h v", a=2, h=4)
                for par in range(2):
                    for a_ in range(2):
                        CP(vz_t[:, half * 2 + a_, par::2, 64 * par:64 * par + 64], bv[:, a_, par::2, :], [b[1]], [vzT], eng=("act" if a_ else "dve"))
            issue_load()

            for c in range(2):
                e_ = EV[c]
                xc = xc_k[c]
                r_, i_, a2_, uu_, hs_ = WK[2], WK[3], WK[4], WK[5], WK[6]
                br = bank()
                MM(br[0][:], bd_t[:, l, 0 + c, :], tm(xc, 0, 512), True, True, [cstT, tmpT[xc]], [br[1]])
                bi = bank()
                MM(bi[0][:], bd_t[:, l, 2 + c, :], tm(xc, 0, 512), True, True, [cstT, tmpT[xc]], [bi[1]])
                ACT(tm(r_, 0, 512), br[0][:], AF.Sigmoid, [br[1], cstT], [tmpT[r_]], bias=pv(42 + c))
                ACT(tm(i_, 0, 512), bi[0][:], AF.Sigmoid, [bi[1], cstT], [tmpT[i_]], bias=pv(44 + c))
                ACT(tm(r_, 0, 512), tm(r_, 0, 512), AF.Exp, [tmpT[r_], dvT], [tmpT[r_]], scale=dv_t[:, l, c:c + 1])
                ACT(tm(a2_, 0, 512), tm(r_, 0, 512), AF.Square, [tmpT[r_]], [tmpT[a2_]])
                ACT(tm(a2_, 0, 512), tm(a2_, 0, 512), AF.Sqrt, [tmpT[a2_]], [tmpT[a2_]], bias=1.0, scale=-1.0)
                TTOP(tm(uu_, 0, 512), tm(a2_, 0, 512), tm(i_, 0, 512), ALU.mult, [tmpT[a2_], tmpT[i_]], [tmpT[uu_]])
                TTOP(tm(uu_, 0, 512), tm(uu_, 0, 512), tm(xc, 0, 512), ALU.mult, [tmpT[uu_], tmpT[xc]], [tmpT[uu_]])
                sch.op("dve", lambda e: e.tensor_tensor_scan(out=tm(hs_, 0, 512), data0=tm(r_, 0, 512), data1=tm(uu_, 0, 512),
                                                              initial=hst_t[:, l, c:c + 1], op0=ALU.mult, op1=ALU.add),
                       [tmpT[r_], tmpT[uu_], hstT[l][c]], [tmpT[hs_]])
                CP(hst_t[:, l, c:c + 1], tm(hs_, 511, 512), [tmpT[hs_]], [hstT[l][c]])
                TTOP(ysmg_t[:, 0 + c, :], tm(hs_, 0, 512), tm(e_["ga"], 0, 512), ALU.mult, [tmpT[hs_], tmpT[e_["ga"]]], [ysT[0 + c]])

            sl, slT = next_page()
            for c in range(2):
                e_ = EV[c]
                b = bank(); proj_fm(sl, slT, c, b)
                ACT(tm(e_["sg"], 0, 512), b[0][:], AF.Silu, [b[1]], [tmpT[e_["sg"]]])
            for c in range(2):
                e_ = EV[c]
                b = bank(); proj_fm(sl, slT, 2 + c, b)
                CP(tm(e_["sb"], 0, 512), b[0][:], [b[1]], [tmpT[e_["sb"]]], eng="act")
            issue_load()

            for c in range(2):
                e_ = EV[c]
                px = e_["px"]
                s2, s4, s8, s16, dd = WK[2], WK[3], WK[4], WK[5], WK[6]
                TTOP(tm(s2, 1, 527), tm(px, 1, 527), tm(px, 0, 526), ALU.add, [tmpT[px]], [tmpT[s2]])
                if c == 0:
                    TTOP(tmp_t[64:128, s4 * TW + 3:s4 * TW + 527], tmp_t[64:128, s2 * TW + 3:s2 * TW + 527],
                         tmp_t[64:128, s2 * TW + 1:s2 * TW + 525], ALU.add, [tmpT[s2]], [tmpT[s4]])
                    sel = [(0, 64, s2, 0.5), (64, 128, s4, 0.25)]
                else:
                    TTOP(tm(s4, 3, 527), tm(s2, 3, 527), tm(s2, 1, 525), ALU.add, [tmpT[s2]], [tmpT[s4]])
                    TTOP(tm(s8, 7, 527), tm(s4, 7, 527), tm(s4, 3, 523), ALU.add, [tmpT[s4]], [tmpT[s8]])
                    TTOP(tmp_t[64:128, s16 * TW + 15:s16 * TW + 527], tmp_t[64:128, s8 * TW + 15:s8 * TW + 527],
                         tmp_t[64:128, s8 * TW + 7:s8 * TW + 519], ALU.add, [tmpT[s8]], [tmpT[s16]])
                    sel = [(0, 64, s8, 0.125), (64, 128, s16, 0.0625)]
                for (p0, p1, sk, inv) in sel:
                    STT(tmp_t[p0:p1, dd * TW:dd * TW + 512], tmp_t[p0:p1, sk * TW + 15:sk * TW + 527], inv,
                        tmp_t[p0:p1, px * TW + 15:px * TW + 527], ALU.mult, ALU.subtract, [tmpT[sk], tmpT[px]], [tmpT[dd]])
                    if i == 0:
                        TTOP(tmp_t[p0:p1, dd * TW:dd * TW + 15], tmp_t[p0:p1, sk * TW + 15:sk * TW + 30], invc[p0:p1, c, 0:15],
                             ALU.mult, [tmpT[sk], cstT, tmpT[dd]], [tmpT[dd]])
                        TTOP(tmp_t[p0:p1, dd * TW:dd * TW + 15], tmp_t[p0:p1, dd * TW:dd * TW + 15], tmp_t[p0:p1, px * TW + 15:px * TW + 30],
                             ALU.subtract, [tmpT[px], tmpT[dd]], [tmpT[dd]])
                bp = bank()
                MM(bp[0][:], bd_t[:, l, 4 + c, :], tm(dd, 0, 512), True, True, [cstT, tmpT[dd]], [bp[1]])
                ACT(ysmg_t[:, 2 + c, :], bp[0][:], AF.Copy, [bp[1], cstT], [ysT[2 + c]], scale=pv(48 + c))

            sl, slT = next_page()
            for c in range(2):
                e_ = EV[c]
                b = bank(); proj_fm(sl, slT, c, b)
                CP(tm(e_["sc"], 0, 512), b[0][:], [b[1]], [tmpT[e_["sc"]]], eng="act")
            for c in range(2):
                e_ = EV[c]
                z = e_["z"]
                b = bank(); proj_fm(sl, slT, 2 + c, b)
                CP(tm(z, 0, 2), halo_t[:, l, c, 18:20], [haloT[l][c]], [tmpT[z]])
                TTOP(tm(z, 2, 514), b[0][:], tm(e_["sc"], 0, 512), ALU.mult, [b[1], tmpT[e_["sc"]]], [tmpT[z]])
                CP(halo_t[:, l, c, 18:20], tm(z, 512, 514), [tmpT[z]], [haloT[l][c]])
            issue_load()
            for c in range(2):
                e_ = EV[c]
                z = e_["z"]; acc = WK[2 + c]
                sw = lambda k: pv(55 + c * 3 + k)
                TS(tm(acc, 0, 512), tm(z, 0, 512), sw(0), None, ALU.mult, None, [tmpT[z], cstT], [tmpT[acc]])
                STT(tm(acc, 0, 512), tm(z, 1, 513), sw(1), tm(acc, 0, 512), ALU.mult, ALU.add, [tmpT[z], tmpT[acc], cstT], [tmpT[acc]])
                STT(tm(acc, 0, 512), tm(z, 2, 514), sw(2), tm(acc, 0, 512), ALU.mult, ALU.add, [tmpT[z], tmpT[acc], cstT], [tmpT[acc]])
                TTOP(ysmg_t[:, 6 + c, :], tm(acc, 0, 512), tm(e_["sb"], 0, 512), ALU.mult, [tmpT[acc], tmpT[e_["sb"]]], [ysT[6 + c]])

            if _STAGE < 5:
                raise _Stop()

            def hgrn_steps():
                for hc in range(2):
                    e_ = EV[hc]
                    q, fk, sg = e_["q"], e_["sgm"], e_["sg"]
                    w0, w1, w2, w3, w4 = WK[0], WK[1], WK[4], WK[5], WK[6]
                    hb = [WK[7], WK[8], WK[2], WK[3], WK[9]]
                    qe_s, qe_i, ke_i, osq = tmb(hb[0], 0), tmb(hb[0], 1), tmb(hb[1], 0), tmb(hb[1], 1)
                    kA, kB = tmb(hb[2], 0), tmb(hb[2], 1)
                    scm = [tmb(hb[3], 0), tmb(hb[3], 1)]
                    sbd = tmp_t[:, hb[4] * TW:hb[4] * TW + 512].bitcast(BF16)
                    v3 = lambda k: tm(k, 0, 512).rearrange("p (n t) -> p n t", t=64)
                    lb = dv_t[:, l, 2 + hc:3 + hc]
                    omlb = dv_t[:, l, 4 + hc:5 + hc]
                    TS(tm(fk, 0, 512), tm(fk, 0, 512), omlb, lb, ALU.mult, ALU.add, [tmpT[fk], dvT], [tmpT[fk]])
                    ACT(tm(w0, 0, 512), tm(fk, 0, 512), AF.Ln, [tmpT[fk]], [tmpT[w0]])
                    TS(tm(fk, 0, 512), tm(fk, 0, 512), -1.0, 1.0, ALU.mult, ALU.add, [tmpT[fk]], [tmpT[fk]])
                    sch.op("dve", lambda e: e.tensor_tensor_scan(out=tm(w1, 0, 512), data0=nstart, data1=tm(w0, 0, 512), initial=0.0,
                                                                  op0=ALU.mult, op1=ALU.add), [cstT, tmpT[w0]], [tmpT[w1]])
                    blast = v3(w1)[:, :, 63]
                    bmid = v3(w1)[:, :, 31]
                    TTOP(v3(w0), blast.unsqueeze(2).to_broadcast([P, 8, 64]), v3(w1), ALU.subtract, [tmpT[w1], tmpT[w0]], [tmpT[w0]])
                    ACT(tm(w2, 0, 512), tm(w1, 0, 512), AF.Exp, [tmpT[w1]], [tmpT[w2]])
                    ACT(tm(w0, 0, 512), tm(w0, 0, 512), AF.Exp, [tmpT[w0]], [tmpT[w0]])
                    ACT(sm_t[:, 0, :], bmid, AF.Exp, [tmpT[w1]], [smT], scale=-1.0)
                    TTOP(sm_t[:, 1, :], bmid, blast, ALU.subtract, [tmpT[w1]], [smT])
                    ACT(sm_t[:, 2, :], sm_t[:, 1, :], AF.Exp, [smT], [smT])
                    TTOP(qe_s, tm(q, 0, 512), tm(w2, 0, 512), ALU.mult, [tmpT[q], tmpT[w2]], [tmpT[hb[0]]])
                    TTOP(tm(w0, 0, 512), tm(fk, 0, 512), tm(w0, 0, 512), ALU.mult, [tmpT[fk], tmpT[w0]], [tmpT[w0]])
                    TTOP(qe_i.rearrange("p (n t) -> p n t", t=64), qe_s.rearrange("p (n t) -> p n t", t=64),
                         sm_t[:, 0, :].unsqueeze(2).to_broadcast([P, 8, 64]), ALU.mult, [tmpT[hb[0]], smT], [tmpT[hb[0]]])
                    TTOP(ke_i.rearrange("p (n t) -> p n t", t=64), v3(w0),
                         sm_t[:, 2, :].unsqueeze(2).to_broadcast([P, 8, 64]), ALU.mult, [tmpT[w0], smT], [tmpT[hb[1]]])
                    dec = v3(w2)[:, :, 63]
                    d3 = tm(w3, 0, 512).rearrange("p (v n) -> p v n", n=8)
                    CP(d3, dec.unsqueeze(1).to_broadcast([P, 64, 8]), [tmpT[w2]], [tmpT[w3]], eng=pool_eng[0])
                    sch.op(pool_eng[0], lambda e: e.memset(d3[:, :, 0], 0.0), [], [tmpT[w3]])
                    yield
                    bt = bank()
                    for tb in range(4):
                        TR(bt[0][:, tb * 128:(tb + 1) * 128], tm(w0, tb * 128, (tb + 1) * 128), [tmpT[w0]], [bt[1]])
                    yield
                    sch.op(pool_eng[0], lambda e: e.memset(kA[64:128, :], 0.0), [], [tmpT[hb[2]]])
                    sch.op(pool_eng[0], lambda e: e.memset(kB[0:64, :], 0.0), [], [tmpT[hb[2]]])
                    CP(kA[0:64, :], bt[0][0:64, :], [bt[1]], [tmpT[hb[2]]], eng="dve")
                    CP(kB[64:128, :], bt[0][64:128, :], [bt[1]], [tmpT[hb[2]]], eng="act")
                    bus = [bank(), bank()]
                    for n in range(8):
                        km = kA if n % 2 == 0 else kB
                        j = n // 2
                        bu = bus[n // 4]
                        MM(bu[0][:, (n % 4) * 128:(n % 4 + 1) * 128], km[:, j * 128:(j + 1) * 128], vtok_t[:, j, hc * 128:(hc + 1) * 128], True, True,
                           [tmpT[hb[2]], vtokT], [bu[1]])
                    yield
                    u3 = tm(w1, 0, 512).rearrange("p (v n) -> p v n", n=8)
                    for bi_ in range(2):
                        for hp in range(2):
                            src = bus[bi_][0][64 * hp:64 * hp + 64, :].rearrange("p (n c) -> p n c", c=128)[:, :, 64 * hp:64 * hp + 64]
                            CP(u3[64 * hp:64 * hp + 64, :, 4 * bi_:4 * bi_ + 4], src.rearrange("p n v -> p v n"), [bus[bi_][1], tmpT[w1]], [tmpT[w1]],
                               eng=("dve" if hp == 0 else "act"))
                    STT(u3[:, :, 0], sst_t[:, l, hc, :], tm(w2, 63, 64), u3[:, :, 0], ALU.mult, ALU.add, [sstT[l][hc], tmpT[w2], tmpT[w1]], [tmpT[w1]])
                    sch.op("dve", lambda e: e.tensor_tensor_scan(out=tm(w4, 0, 512), data0=tm(w3, 0, 512), data1=tm(w1, 0, 512), initial=0.0,
                                                                  op0=ALU.mult, op1=ALU.add), [tmpT[w3], tmpT[w1]], [tmpT[w4]])
                    s3 = tm(w4, 0, 512).rearrange("p (v n) -> p v n", n=8)
                    sbd3 = sbd.rearrange("p (n c) -> p n c", c=128)
                    sch.op(pool_eng[0], lambda e: e.memset(sbd, 0.0), [], [tmpT[hb[4]]])
                    for hp in range(2):
                        ps_ = slice(64 * hp, 64 * hp + 64)
                        CP(sbd3[ps_, 0, 64 * hp:64 * hp + 64], sst_t[ps_, l, hc, :], [sstT[l][hc]], [tmpT[hb[4]]])
                        CP(sbd3[ps_, 1:8, 64 * hp:64 * hp + 64], s3[ps_, :, 0:7].rearrange("p v n -> p n v"), [tmpT[w4]], [tmpT[hb[4]]])
                    CP(sst_t[:, l, hc, :], s3[:, :, 7], [tmpT[w4]], [sstT[l][hc]])
                    bsb = [bank(), bank()]
                    for hp in range(2):
                        for j in range(4):
                            MM(bsb[hp][0][:, j * 128:(j + 1) * 128], ke_i[64 * hp:64 * hp + 64, j * 128:(j + 1) * 128],
                               qe_i[64 * hp:64 * hp + 64, j * 128:(j + 1) * 128], True, True, [tmpT[hb[0]], tmpT[hb[1]]], [bsb[hp][1]])
                    for hp in range(2):
                        TTOP(scm[hp].rearrange("p (a t) -> p a t", t=128), bsb[hp][0][:].rearrange("p (a t) -> p a t", t=128),
                             cmask2.unsqueeze(1).to_broadcast([P, 4, 128]), ALU.mult, [bsb[hp][1], cstT], [tmpT[hb[3]]])
                    yield
                    bo = bank()
                    for j in range(4):
                        out_ = bo[0][:, j * 128:(j + 1) * 128]
                        MM(out_, vz_t[:, j, 2 * hc, :], scm[0][:, j * 128:(j + 1) * 128], True, False, [vzT, tmpT[hb[3]]], [bo[1]])
                        MM(out_, vz_t[:, j, 2 * hc + 1, :], scm[1][:, j * 128:(j + 1) * 128], False, False, [vzT, tmpT[hb[3]]], [bo[1]])
                        for n in (2 * j, 2 * j + 1):
                            MM(bo[0][:, n * 64:(n + 1) * 64], sbd3[:, n, :], qe_s[:, n * 64:(n + 1) * 64], False, n == 2 * j + 1,
                               [tmpT[hb[4]], tmpT[hb[0]]], [bo[1]])
                    yield
                    ACT(osq, bo[0][:], AF.Square, [bo[1]], [tmpT[hb[1]]])
                    bn = bank()
                    MM(bn[0][:], bones_bf, osq, True, True, [cbfT, tmpT[hb[1]]], [bn[1]])
                    ACT(tm(w3, 0, 512), bn[0][:], AF.Sqrt, [bn[1]], [tmpT[w3]], bias=EPS, scale=1.0 / 64)
                    sch.op("dve", lambda e: e.reciprocal(out=tm(w3, 0, 512), in_=tm(w3, 0, 512)), [tmpT[w3]], [tmpT[w3]])
                    TTOP(tm(w4, 0, 512), bo[0][:], tm(w3, 0, 512), ALU.mult, [bo[1], tmpT[w3]], [tmpT[w4]])
                    STT(ysmg_t[:, 4 + hc, :], tm(w4, 0, 512), pv(54), tm(sg, 0, 512), ALU.mult, ALU.mult, [tmpT[w4], tmpT[sg], cstT], [ysT[4 + hc]])
                    yield

            if _STAGE < 6:
                raise _Stop()
            hg = hgrn_steps()
            next(hg, None)
            slot_i = 0
            for ki, k in enumerate((0, 1, 3, 2)):
                if ki == 3:
                    for _ in hg:
                        pass
                for hh in range(2):
                    sl, slT = next_page()
                    wsl, wslT = next_wb()
                    for e4 in range(4):
                        e = hh * 4 + e4
                        gb = bank()
                        for dc in range(8):
                            MM(gb[0][:], sl[:, dc * 512 + e4 * 128:dc * 512 + (e4 + 1) * 128], u_t[:, dc, :], dc == 0, dc == 7, [slT, uT[dc]], [gb[1]])
                        bb = bank()
                        for cc in range(2):
                            MM(bb[0][:], wsl[:, cc * 512 + e4 * 128:cc * 512 + (e4 + 1) * 128], ysmg_t[:, k * 2 + cc, :], cc == 0, cc == 1,
                               [wslT, ysT[k * 2 + cc]], [bb[1]])
                        gs = EV[slot_i % 2]["xa"]
                        mt = EV[slot_i % 2]["ga"]
                        ACT(tm(gs, 0, 512), gb[0][:], AF.Sigmoid, [gb[1]], [tmpT[gs]])
                        if ki == 0:
                            TTOP(mix_t[:, e, :], bb[0][:], tm(gs, 0, 512), ALU.mult, [bb[1], tmpT[gs]], [mixT[e]])
                        else:
                            TTOP(tm(mt, 0, 512), bb[0][:], tm(gs, 0, 512), ALU.mult, [bb[1], tmpT[gs]], [tmpT[mt]])
                            if ki < 3:
                                TTOP(mix_t[:, e, :], mix_t[:, e, :], tm(mt, 0, 512), ALU.add, [mixT[e], tmpT[mt]], [mixT[e]], eng=pool_eng[0])
                            else:
                                TTOP(ysmg_t[:, 8 + e, :], mix_t[:, e, :], tm(mt, 0, 512), ALU.add, [mixT[e], tmpT[mt]], [mgT[e]], eng=pool_eng[0])
                        slot_i += 1
                        if ki < 3 and slot_i % 2 == 0:
                            next(hg, None)
                    issue_load()
                    issue_wb()
            for hh in range(2):
                sl, slT = next_page()
                for e2 in range(4):
                    ec = hh * 4 + e2
                    b = bank()
                    for dc in range(8):
                        MM(b[0][:], sl[:, dc * 512 + e2 * 128:dc * 512 + (e2 + 1) * 128], ysmg_t[:, 8 + dc, :], dc == 0, dc == 7,
                           [slT, mgT[dc]], [b[1]])
                    CP(mix_t[:, ec, :], b[0][:], [b[1]], [mixT[ec]], eng="act")
                issue_load()
            norm(lambda c: mix_t[:, c, :], mixT, 8, l, "res")

            if _STAGE < 7:
                raise _Stop()
            norm(lambda c: h_t[:, c, :], hT, 16, l, "u")
            for g in range(8):
                sl, slT = next_page()
                for j in range(4):
                    fc = g * 4 + j
                    b = bank()
                    for dc in range(8):
                        MM(b[0][:], sl[:, dc * 512 + j * 128:dc * 512 + (j + 1) * 128], u_t[:, dc, :], dc == 0, dc == 7, [slT, uT[dc]], [b[1]])
                    rl = WK[fc % 4]
                    ACT(tm(rl, 0, 512), b[0][:], AF.Relu, [b[1]], [tmpT[rl]])
                    TTOP(hid_ap[:, fc, :], b[0][:], tm(rl, 0, 512), ALU.mult, [b[1], tmpT[rl]], [hidT[fc]])
                issue_load()
            for e in range(8):
                sl, slT = next_page()
                b = bank()
                for fc in range(32):
                    MM(b[0][:], sl[:, fc * 128:(fc + 1) * 128], hid_ap[:, fc, :], fc == 0, fc == 31, [slT, hidT[fc]], [b[1]])
                CP(mix_t[:, e, :], b[0][:], [b[1]], [mixT[e]], eng="act")
                issue_load()
            norm(lambda c: mix_t[:, c, :], mixT, 24, l, "res")

            if l == L - 1:
                for tb in range(4):
                    for half in range(2):
                        b = bank()
                        for cc in range(4):
                            c = half * 4 + cc
                            TR(b[0][:, cc * 128:(cc + 1) * 128], h_t[:, c, tb * 128:(tb + 1) * 128], [hT[c]], [b[1]])
                        CP(io_ap[:, tb, half * 512:(half + 1) * 512], b[0][:], [b[1]], [ioT[tb]], eng=("act" if half else "dve"))
                sch.op("sp", lambda e: e.dma_start(out=y_d[i * TT:(i + 1) * TT, :].rearrange("(tb p) d -> p tb d", p=P), in_=io_ap),
                       [ioT], [], dma_sem="yout")

        try:
            if _STAGE < 3:
                raise _Stop()
            for i in range(NT):
                for l in range(L):
                    tile_layer(i, l)
        except _Stop:
            for nm in sch.sem:
                if sch.val[nm] > 0:
                    nc.sync.wait_ge(sch.sem[nm], sch.val[nm])
        nc.sync.wait_ge(sch.sem["yout"], sch.val["yout"])
        if dbg:
            print("sem values:", sch.val)
    return nc


def _cols(v):
    v = np.asarray(v, np.float32).reshape(-1, P)
    return v.T


def host_consts():
    c = np.zeros((P, NCST), np.float32)
    c[:, 0:128] = np.eye(P, dtype=np.float32)
    c[:, 128:256] = 1.0
    p = np.arange(P)
    c[:, 256:384] = (p[:, None] // 64 == p[None, :] // 64).astype(np.float32)
    c[:, 384:512] = ((p[:, None] // 64 == p[None, :] // 64) & ((p[:, None] % 64) <= (p[None, :] % 64))).astype(np.float32)
    t = np.arange(512)
    c[:, 512:1024] = (t % 64 != 0).astype(np.float32)[None, :]
    wins = np.array([[2, 4], [8, 16]])
    for ch in range(2):
        w = wins[ch][p // 64].astype(np.float32)
        tt = np.arange(16, dtype=np.float32)[None, :] + 1.0
        c[:, 1024 + ch * 16:1024 + (ch + 1) * 16] = 1.0 / np.minimum(tt, w[:, None])
    return c


def host_params(inp, L):
    pv = np.zeros((P, L, NV), np.float32)
    bd = np.zeros((P, L, 6, P), np.float32)
    for l in range(L):
        pv[:, l, 0:8] = _cols(inp["norm_mix_pre"][l])
        pv[:, l, 8:16] = _cols(inp["norm_mix_post"][l])
        pv[:, l, 16:24] = _cols(inp["norm_mlp_pre"][l])
        pv[:, l, 24:32] = _cols(inp["norm_mlp_post"][l])
        cw = np.asarray(inp["lru_conv_w"][l], np.float32)
        for c in range(2):
            for k in range(4):
                pv[:, l, 32 + c * 4 + k] = cw[k, c * 128:(c + 1) * 128]
        pv[:, l, 40:42] = _cols(inp["lru_conv_b"][l])
        pv[:, l, 42:44] = _cols(inp["lru_b_a"][l])
        pv[:, l, 44:46] = _cols(inp["lru_b_x"][l])
        pv[:, l, 46:48] = _cols(inp["lru_lambda"][l])
        pv[:, l, 48:50] = _cols(inp["pool_scale"][l])
        pv[:, l, 50:52] = _cols(inp["hgrn_lower_bound"][0])
        pv[:, l, 52:54] = _cols(inp["hgrn_lower_bound"][min(1, L - 1)])
        pv[:, l, 54] = np.tile(np.asarray(inp["hgrn_norm"][l], np.float32), 2)
        sw = np.asarray(inp["sconv_w"][l], np.float32)
        for c in range(2):
            for k in range(3):
                pv[:, l, 55 + c * 3 + k] = sw[k, c * 128:(c + 1) * 128]
        wa = np.asarray(inp["lru_w_a"][l], np.float32)
        wx = np.asarray(inp["lru_w_x"][l], np.float32)
        pw = np.asarray(inp["pool_w"][l], np.float32)
        for c in range(2):
            for n in range(4):
                bd[n * 32:(n + 1) * 32, l, 0 + c, n * 32:(n + 1) * 32] = wa[c * 4 + n]
                bd[n * 32:(n + 1) * 32, l, 2 + c, n * 32:(n + 1) * 32] = wx[c * 4 + n]
            for n in range(2):
                bd[n * 64:(n + 1) * 64, l, 4 + c, n * 64:(n + 1) * 64] = pw[c * 2 + n]
    return pv, bd


def make_in_maps(inp, n_cores, L):
    pv, bd = host_params(inp, L)
    cst = host_consts()
    f = lambda a: np.ascontiguousarray(np.asarray(a, np.float32))
    shared = {
        "w_in": f(inp["w_in"])[:L], "w_branch": f(inp["w_branch"])[:L].reshape(L, D, D), "w_out": f(inp["w_out"])[:L],
        "w_up": f(inp["w_up"])[:L], "w_down": f(inp["w_down"])[:L], "pvec": pv, "bdw": bd, "cst": cst,
    }
    x = f(inp["x"])
    return [dict(shared, x=np.ascontiguousarray(x[b])) for b in range(n_cores)]


def kernel(**inputs):
    B, S, _ = inputs["x"].shape
    L = inputs["w_in"].shape[0]
    nc = build_program(S // TT, L)
    in_maps = make_in_maps(inputs, B, L)
    res = run_bass_kernel_spmd(nc, in_maps, core_ids=list(range(B)))
    return np.stack([np.asarray(r["y"], np.float32) for r in res.results], axis=0)
```

```python
import numpy as np
from contextlib import ExitStack
import concourse.bass as bass
import concourse.mybir as mybir
from concourse.bass_utils import run_bass_kernel_spmd

F32 = mybir.dt.float32
BF16 = mybir.dt.bfloat16
AF = mybir.ActivationFunctionType
ALU = mybir.AluOpType

P = 128
TT = 512
D = 1024
NSLOT = 4
NPG = 31
PGE = 4096
EPS = 1e-6
TW = 528
NTMP = 28
NCST = 1058
NV = 64
_STAGE = 99


class _Stop(Exception):
    pass


class T:
    __slots__ = ("name", "w", "r")

    def __init__(self, name):
        self.name = name
        self.w = None
        self.r = {}


def _flat(ts):
    for t in ts:
        if isinstance(t, T):
            yield t
        else:
            yield from _flat(t)


class Sch:
    def __init__(self, nc, es):
        self.nc = nc
        self.es = es
        self.E = {"pe": nc.tensor, "act": nc.scalar, "dve": nc.vector, "pool": nc.gpsimd, "sp": nc.sync}
        self.sem = {}
        self.val = {}
        self.waited = {e: {} for e in self.E}
        for e in ("pe", "act", "dve", "pool"):
            self.newsem(e)

    def newsem(self, name):
        self.sem[name] = self.es.enter_context(self.nc.semaphore(name))
        self.val[name] = 0

    def op(self, eng, fn, reads=(), writes=(), dma_sem=None):
        deps = {}
        R = list(_flat(reads))
        Wr = list(_flat(writes))
        for t in R:
            if t.w is not None:
                s, v = t.w
                deps[s] = max(deps.get(s, 0), v)
        for t in Wr:
            if t.w is not None:
                s, v = t.w
                deps[s] = max(deps.get(s, 0), v)
            for s, v in t.r.items():
                deps[s] = max(deps.get(s, 0), v)
        E = self.E[eng]
        for s, v in deps.items():
            if eng == "pe" and s == "pe":
                continue
            if self.waited[eng].get(s, 0) < v:
                E.wait_ge(self.sem[s], v)
                self.waited[eng][s] = v
        inst = fn(E)
        if dma_sem is not None:
            s, inc = dma_sem, 16
        else:
            s, inc = eng, 1
        self.val[s] += inc
        inst.then_inc(self.sem[s], inc)
        v = self.val[s]
        for t in R:
            t.r[s] = v
        for t in Wr:
            t.w = (s, v)
            t.r = {}
        return inst


def build_program(NT, L, dbg=False):
    SEQ = NT * TT
    nc = bass.Bass("TRN2", target_bir_lowering=False)
    x_d = nc.dram_tensor("x", [SEQ, D], F32, kind="ExternalInput").ap()
    w_in_d = nc.dram_tensor("w_in", [L, D, 6656], F32, kind="ExternalInput").ap()
    w_br_d = nc.dram_tensor("w_branch", [L, D, D], F32, kind="ExternalInput").ap()
    w_out_d = nc.dram_tensor("w_out", [L, D, D], F32, kind="ExternalInput").ap()
    w_up_d = nc.dram_tensor("w_up", [L, D, 4096], F32, kind="ExternalInput").ap()
    w_dn_d = nc.dram_tensor("w_down", [L, 4096, D], F32, kind="ExternalInput").ap()
    pv_d = nc.dram_tensor("pvec", [P, L, NV], F32, kind="ExternalInput").ap()
    bd_d = nc.dram_tensor("bdw", [P, L, 6, P], F32, kind="ExternalInput").ap()
    cst_d = nc.dram_tensor("cst", [P, NCST], F32, kind="ExternalInput").ap()
    y_d = nc.dram_tensor("y", [SEQ, D], F32, kind="ExternalOutput").ap()
    wpg_d = nc.dram_tensor("wpg", [L, NPG, P, PGE], BF16, kind="Internal").ap()
    wbp_d = nc.dram_tensor("wbp", [L, 8, P, 1024], BF16, kind="Internal").ap()

    es = ExitStack()
    with es:
        sch = Sch(nc, es)
        for s in range(NSLOT):
            sch.newsem(f"slot{s}")
        for s in range(2):
            sch.newsem(f"wbs{s}")
        for nm in ("cst", "xin", "yout"):
            sch.newsem(nm)
        for j in range(3):
            sch.newsem(f"stg{j}")
        for j in range(2):
            sch.newsem(f"bgi{j}")
            sch.newsem(f"wst{j}")
        sch.newsem("wbi")
        for s in range(NSLOT):
            sch.newsem(f"st{s}")

        def sb(name, shape, dt):
            return es.enter_context(nc.sbuf_tensor(name, shape, dt))

        h_t = sb("h", [P, 8, TT], F32)
        u_t = sb("u", [P, 8, TT], BF16)
        mix_t = sb("mix", [P, 8, TT], F32)
        ysmg_t = sb("ysmg", [P, 16, TT], BF16)
        slots = [sb(f"slot{s}", [P, PGE], BF16) for s in range(NSLOT)]
        wbs = [sb(f"wbs{s}", [P, 1024], BF16) for s in range(2)]
        tmp_t = sb("tmp", [P, NTMP * TW], F32)
        cst_t = sb("cst_sb", [P, NCST], F32)
        pv_t = sb("pv_sb", [P, L, NV], F32)
        bd_t = sb("bd_sb", [P, L, 6, P], F32)
        dv_t = sb("dv_sb", [P, L, 8], F32)
        cbf_t = sb("cbf", [P, 2, P], BF16)
        sqb_t = sb("sqb", [P, 2, TT], BF16)
        rstd_t = sb("rstd", [P, TT], F32)
        vz_t = sb("vz", [P, 4, 4, P], BF16)
        halo_t = sb("halo", [P, L, 2, 20], F32)
        hst_t = sb("hst", [P, L, 2], F32)
        sst_t = sb("sst", [P, L, 2, 64], F32)
        sm_t = sb("sm", [P, 4, 8], F32)
        vtok_t = sb("vtok", [P, 4, 256], BF16)
        bgs_t = sb("bgs", [P, 2, 2048], F32)
        wbst_t = sb("wbst", [P, 1024], F32)
        wbstT = T("wbst")
        bgsT = [T("bgs0"), T("bgs1")]
        bgoT = [T("bgo0"), T("bgo1")]
        psb = [es.enter_context(nc.psum_tensor(f"ps{i}", [P, TT], F32)) for i in range(8)]

        hT = [T(f"h{c}") for c in range(8)]
        uT = [T(f"u{c}") for c in range(8)]
        mixT = [T(f"mix{c}") for c in range(8)]
        ioT = [[mixT[2 * tb], mixT[2 * tb + 1]] for tb in range(4)]
        ysT = [T(f"ys{c}") for c in range(8)]
        mgT = [T(f"mg{c}") for c in range(8)]
        slotT = [T(f"slot{s}") for s in range(NSLOT)]
        wbsT = [T(f"wbs{s}") for s in range(2)]
        tmpT = [T(f"tmp{k}") for k in range(NTMP)]
        hidT = [[tmpT[k] for k in range((fc * 256) // TW, ((fc + 1) * 256 - 1) // TW + 1)] for fc in range(32)]
        psT = [T(f"ps{i}") for i in range(8)]
        cstT = T("cst")
        cbfT = T("cbf")
        dvT = T("dv")
        sqbT = [T("sqb0"), T("sqb1")]
        rstdT = T("rstd")
        vzT = T("vz")
        haloT = [[T(f"halo{l}{c}") for c in range(2)] for l in range(L)]
        hstT = [[T(f"hst{l}{c}") for c in range(2)] for l in range(L)]
        sstT = [[T(f"sst{l}{c}") for c in range(2)] for l in range(L)]
        smT = T("sm")
        vtokT = T("vtok")
        wpgT = [T(f"wpg{l}") for l in range(L)]

        io_ap = mix_t[:].rearrange("p c t -> p (c t)").rearrange("p (tb d) -> p tb d", tb=4)
        hid_ap = tmp_t[:, 0:8192].bitcast(BF16).rearrange("p (f t) -> p f t", t=TT)

        def tm(k, a=0, b=TW):
            return tmp_t[:, k * TW + a:k * TW + b]

        def tmb(k, half):
            return tmp_t[:, k * TW:k * TW + 512].bitcast(BF16)[:, half * 512:(half + 1) * 512]

        ident = cst_t[:, 0:128]
        cmask2 = cst_t[:, 384:512]
        nstart = cst_t[:, 512:1024]
        invc = cst_t[:, 1024:1056].rearrange("p (c t) -> p c t", c=2)
        ones_bf = cbf_t[:, 0, :]
        bones_bf = cbf_t[:, 1, :]

        bank_ctr = [0]

        def bank():
            i = bank_ctr[0] % 8
            bank_ctr[0] += 1
            return psb[i], psT[i]

        def ACT(out, in_, func, R, Wt, bias=None, scale=None):
            kw = {}
            if bias is not None:
                kw["bias"] = bias
            if scale is not None:
                kw["scale"] = scale
            return sch.op("act", lambda e: e.activation(out=out, in_=in_, func=func, **kw), R, Wt)

        def TTOP(out, in0, in1, op, R, Wt, eng="dve"):
            return sch.op(eng, lambda e: e.tensor_tensor(out=out, in0=in0, in1=in1, op=op), R, Wt)

        def TS(out, in0, s1, s2, op0, op1, R, Wt, eng="dve"):
            if op1 is None:
                return sch.op(eng, lambda e: e.tensor_scalar(out=out, in0=in0, scalar1=s1, scalar2=None, op0=op0), R, Wt)
            return sch.op(eng, lambda e: e.tensor_scalar(out=out, in0=in0, scalar1=s1, scalar2=s2, op0=op0, op1=op1), R, Wt)

        def STT(out, in0, sc, in1, op0, op1, R, Wt):
            return sch.op("dve", lambda e: e.scalar_tensor_tensor(out=out, in0=in0, scalar=sc, in1=in1, op0=op0, op1=op1), R, Wt)

        def CP(out, in_, R, Wt, eng="dve"):
            if eng == "act":
                return sch.op("act", lambda e: e.copy(out=out, in_=in_), R, Wt)
            return sch.op(eng, lambda e: e.tensor_copy(out=out, in_=in_), R, Wt)

        def MM(out, lhsT, rhs, st, sp, R, Wt):
            return sch.op("pe", lambda e: e.matmul(out, lhsT=lhsT, rhs=rhs, start=st, stop=sp), R, Wt)

        def TR(out, in_, R, Wt):
            return sch.op("pe", lambda e: e.transpose(out, in_, ident), R + [cstT], Wt)

        n_c = 0
        for dst, src in ((cst_t[:], cst_d), (pv_t[:], pv_d), (bd_t[:], bd_d)):
            nc.sync.dma_start(out=dst, in_=src).then_inc(sch.sem["cst"], 16)
            n_c += 16
        sch.val["cst"] = n_c
        cstT.w = ("cst", n_c)

        sch.op("pool", lambda e: e.memset(halo_t[:], 0.0), [], [haloT])
        sch.op("pool", lambda e: e.memset(hst_t[:], 0.0), [], [hstT])
        sch.op("pool", lambda e: e.memset(sst_t[:], 0.0), [], [sstT])
        sch.op("pool", lambda e: e.memset(vz_t[:], 0.0), [], [vzT])
        CP(cbf_t[:], cst_t[:, 128:384].rearrange("p (a b) -> p a b", a=2), [cstT], [cbfT])
        for l in range(L):
            lam = pv_t[:, l, 46:48]
            ACT(dv_t[:, l, 0:2], lam, AF.Exp, [cstT], [dvT], scale=-1.0)
            TS(dv_t[:, l, 0:2], dv_t[:, l, 0:2], 1.0, None, ALU.add, None, [dvT], [dvT])
            ACT(dv_t[:, l, 0:2], dv_t[:, l, 0:2], AF.Ln, [dvT], [dvT])
            TS(dv_t[:, l, 0:2], dv_t[:, l, 0:2], -8.0, None, ALU.mult, None, [dvT], [dvT])
            if l == 0:
                sch.op("dve", lambda e: e.memset(dv_t[:, l, 2:4], 0.0), [], [dvT])
            else:
                assert l == 1
                TTOP(dv_t[:, l, 2:4], pv_t[:, l, 52:54], pv_t[:, l, 50:52], ALU.subtract, [cstT], [dvT])
                ACT(dv_t[:, l, 2:4], dv_t[:, l, 2:4], AF.Sigmoid, [dvT], [dvT])
            TS(dv_t[:, l, 4:6], dv_t[:, l, 2:4], -1.0, 1.0, ALU.mult, ALU.add, [dvT], [dvT])

        NSTG = 3
        stgT = [[tmpT[k] for k in range((j * 4096) // TW, ((j + 1) * 4096 - 1) // TW + 1)] for j in range(NSTG)]
        cvt_ctr = [0]
        cast_engs = ("dve", "act", "pool")
        KH = [(k, hh) for k in (0, 1, 3, 2) for hh in range(2)]
        depT = {}

        def page_pieces(l):
            wi = w_in_d[l]

            def dcp(src2d, pg):
                return [(h * 2048, 2048, (lambda a: a.rearrange("p (dc n) -> p dc n", dc=4)),
                         src2d[h * 512:(h + 1) * 512, :].rearrange("(dc p) n -> p dc n", p=P),
                         wpg_d[l, pg][:, h * 2048:(h + 1) * 2048], ("pg", pg)) for h in range(2)]
            for g in range(5):
                yield dcp(wi[:, g * 512:(g + 1) * 512], g)
            for pi_, (k, hh) in enumerate(KH):
                c0 = 2560 + k * 1024 + hh * 512
                yield dcp(wi[:, c0:c0 + 512], 5 + pi_)
            for hh in range(2):
                yield dcp(w_out_d[l][:, hh * 512:(hh + 1) * 512], 13 + hh)
            for g in range(8):
                yield dcp(w_up_d[l][:, g * 512:(g + 1) * 512], 15 + g)
            for e in range(8):
                yield [(h * 2048, 2048, (lambda a: a.rearrange("p (fc n) -> p fc n", fc=16)),
                        w_dn_d[l][h * 2048:(h + 1) * 2048, e * 128:(e + 1) * 128].rearrange("(fc p) n -> p fc n", p=P),
                        wpg_d[l, 23 + e][:, h * 2048:(h + 1) * 2048], ("pg", 23 + e)) for h in range(2)]
            for half in range(2):
                pcs = []
                for q4 in range(4):
                    k, hh = KH[half * 4 + q4]
                    pcs.append((q4 * 1024, 1024, (lambda a: a.rearrange("p (cc n) -> p cc n", cc=2)),
                                w_br_d[l][k * 256:(k + 1) * 256, hh * 512:(hh + 1) * 512].rearrange("(cc p) n -> p cc n", p=P),
                                wbp_d[l, half * 4 + q4], ("wb", half * 4 + q4)))
                yield pcs

        def convert_fg(pieces):
            n = cvt_ctr[0]
            cvt_ctr[0] += 1
            j = n % NSTG
            s_ = n % NSLOT
            stg = tmp_t[:, j * 4096:(j + 1) * 4096]
            for (off, size, vf, src, dst, key) in pieces:
                sch.op("sp", lambda e: e.dma_start(out=vf(stg[:, off:off + size]), in_=src), [], [stgT[j]], dma_sem=f"stg{j}")
            CP(slots[s_][:], stg, [stgT[j]], [slotT[s_]], eng=cast_engs[n % 3])
            for (off, size, vf, src, dst, key) in pieces:
                sch.op("act", lambda e: e.dma_start(out=dst, in_=slots[s_][:, off:off + size]), [slotT[s_]], [], dma_sem=f"st{s_}")

        bg_ctr = [0]
        bg_pending = [None]

        def bg_flush():
            if bg_pending[0] is None:
                return
            j, ch, base, l = bg_pending[0]
            bg_pending[0] = None
            tot = sum(pc[1] for pc in ch)
            CP(bgo_t[:, j, 0:tot], bgs_t[:, j, 0:tot], [bgsT[j]], [bgoT[j]], eng="pool")
            ts = []
            for (off, size, vf, src, dst, key) in ch:
                t = T("wdep")
                sch.op("pool", lambda e: e.dma_start(out=dst, in_=bgo_t[:, j, off - base:off - base + size]), [bgoT[j]], [t], dma_sem=f"bgo{j}")
                depT.setdefault((l,) + key, []).append(t)
                ts.append(t)
            for t in ts:
                t.w = (f"bgo{j}", sch.val[f"bgo{j}"])

        def convert_bg(l, pieces):
            chunks, cur, cs = [], [], 0
            for pc in pieces:
                if cs + pc[1] > 2048:
                    chunks.append(cur)
                    cur, cs = [], 0
                cur.append(pc)
                cs += pc[1]
            chunks.append(cur)
            for ch in chunks:
                n = bg_ctr[0]
                bg_ctr[0] += 1
                j = n % 2
                base = ch[0][0]
                for (off, size, vf, src, dst, key) in ch:
                    sch.op("pool", lambda e: e.dma_start(out=vf(bgs_t[:, j, off - base:off - base + size]), in_=src), [], [bgsT[j]], dma_sem=f"bgi{j}")
                bg_flush()
                bg_pending[0] = (j, ch, base, l)

        PP = {}
        WBP = {}
        for l in range(L):
            allp = list(page_pieces(l))
            PP[l] = allp[:NPG]
            WBP[l] = [pc for grp in allp[NPG:] for pc in grp]

        pgq = [(l, pg) for i in range(NT) for l in range(L) for pg in range(NPG)]
        wbq = [(l, e) for i in range(NT) for l in range(L) for e in range(8)]
        lp = [0]
        up_ = [0]
        wlp = [0]
        wup = [0]

        pend = [None]
        pend_wb = [None]

        def flush_pend():
            if pend[0] is None:
                return
            s_, l, pg, pieces = pend[0]
            pend[0] = None
            for h, (off, size, vf, src, dst, key) in enumerate(pieces):
                CP(slots[s_][:, off:off + size], bgs_t[:, h, :], [bgsT[h]], [slotT[s_]], eng=("act" if h == 0 else "dve"))
            ts = []
            for (off, size, vf, src, dst, key) in pieces:
                t = T("wdep")
                sch.op("sp", lambda e: e.dma_start(out=dst, in_=slots[s_][:, off:off + size]), [slotT[s_]], [t], dma_sem=f"st{s_}")
                ts.append(t)
            for t in ts:
                t.w = (f"st{s_}", sch.val[f"st{s_}"])
            depT[(l, "pg", pg)] = ts

        def flush_pend_wb():
            if pend_wb[0] is None:
                return
            s_, l, e_, pc = pend_wb[0]
            pend_wb[0] = None
            CP(wbs[s_][:], wbst_t[:], [wbstT], [wbsT[s_]], eng="pool")
            t = T("wdep")
            sch.op("sp", lambda e: e.dma_start(out=pc[4], in_=wbs[s_][:]), [wbsT[s_]], [t], dma_sem=f"wst{s_}")
            depT[(l, "wb", e_)] = [t]

        def issue_load():
            flush_pend()
            if lp[0] >= len(pgq):
                return
            l, pg = pgq[lp[0]]
            s = lp[0] % NSLOT
            if lp[0] < L * NPG:
                for h, (off, size, vf, src, dst, key) in enumerate(PP[l][pg]):
                    sch.op("sp", lambda e: e.dma_start(out=vf(bgs_t[:, h, :]), in_=src), [], [bgsT[h]], dma_sem=f"bgi{h}")
                pend[0] = (s, l, pg, PP[l][pg])
            else:
                sch.op("sp", lambda e: e.dma_start(out=slots[s][:], in_=wpg_d[l, pg]), [depT[(l, "pg", pg)]], [slotT[s]], dma_sem=f"slot{s}")
            lp[0] += 1

        def issue_wb():
            flush_pend_wb()
            if wlp[0] >= len(wbq):
                return
            l, e_ = wbq[wlp[0]]
            s = wlp[0] % 2
            if wlp[0] < L * 8:
                pc = WBP[l][e_]
                sch.op("sp", lambda e: e.dma_start(out=pc[2](wbst_t[:]), in_=pc[3]), [], [wbstT], dma_sem="wbi")
                pend_wb[0] = (s, l, e_, pc)
            else:
                sch.op("sp", lambda e: e.dma_start(out=wbs[s][:], in_=wbp_d[l, e_]), [depT[(l, "wb", e_)]], [wbsT[s]], dma_sem=f"wbs{s}")
            wlp[0] += 1

        def next_page():
            s = up_[0] % NSLOT
            up_[0] += 1
            return slots[s], slotT[s]

        def next_wb():
            s = wup[0] % 2
            wup[0] += 1
            return wbs[s], wbsT[s]

        if _STAGE >= 3:
            for _ in range(NSLOT):
                issue_load()
            for _ in range(2):
                issue_wb()

        def norm(src_ap, srcT, gcol, l, mode):
            pb, pbT = bank()
            for c in range(8):
                k = c % 2
                ACT(sqb_t[:, k, :], src_ap(c), AF.Square, [srcT[c]], [sqbT[k]])
                MM(pb[:], ones_bf, sqb_t[:, k, :], c == 0, c == 7, [cbfT, sqbT[k]], [pbT])
            ACT(rstd_t[:], pb[:], AF.Sqrt, [pbT], [rstdT], bias=EPS, scale=1.0 / D)
            sch.op("dve", lambda e: e.reciprocal(out=rstd_t[:], in_=rstd_t[:]), [rstdT], [rstdT])
            for c in range(8):
                g = pv_t[:, l, gcol + c:gcol + c + 1]
                if mode == "u":
                    STT(u_t[:, c, :], src_ap(c), g, rstd_t[:], ALU.mult, ALU.mult, [srcT[c], rstdT, cstT], [uT[c]])
                else:
                    k = 25 + c % 2
                    STT(tm(k, 0, 512), src_ap(c), g, rstd_t[:], ALU.mult, ALU.mult, [srcT[c], rstdT, cstT], [tmpT[k]])
                    TTOP(h_t[:, c, :], h_t[:, c, :], tm(k, 0, 512), ALU.add, [hT[c], tmpT[k]], [hT[c]], eng=pool_eng[0])

        EV = {}
        for c in range(2):
            b0 = c * 9
            EV[c] = dict(xa=b0, ga=b0 + 1, px=b0 + 2, q=b0 + 3, sgm=b0 + 4, sg=b0 + 5, sc=b0 + 6, sb=b0 + 7, z=b0 + 8)
        WK = list(range(18, 28))

        def proj_fm(sl, slT, jj, outb):
            for dc in range(8):
                MM(outb[0][:], sl[:, dc * 512 + jj * 128:dc * 512 + (jj + 1) * 128], u_t[:, dc, :], dc == 0, dc == 7,
                   [slT, uT[dc]], [outb[1]])

        pool_eng = ["pool"]

        def tile_layer(i, l):
            pool_eng[0] = "pool"
            pv = lambda a, b=None: pv_t[:, l, a:(a + 1 if b is None else b)]
            if l == 0:
                sch.op("sp", lambda e: e.dma_start(out=io_ap, in_=x_d[i * TT:(i + 1) * TT, :].rearrange("(tb p) d -> p tb d", p=P)),
                       [], [ioT], dma_sem="xin")
                for c in range(8):
                    pb, pbT = bank()
                    for tb in range(4):
                        TR(pb[:, tb * 128:(tb + 1) * 128], io_ap[:, tb, c * 128:(c + 1) * 128], [ioT[tb]], [pbT])
                    CP(h_t[:, c, :], pb[:], [pbT], [hT[c]], eng=("act" if c % 2 else "dve"))
            norm(lambda c: h_t[:, c, :], hT, 0, l, "u")
            if _STAGE < 4:
                raise _Stop()
            sl, slT = next_page()
            for c in range(2):
                e_ = EV[c]
                b = bank(); proj_fm(sl, slT, c, b)
                CP(tm(e_["xa"], 0, 3), halo_t[:, l, c, 0:3], [haloT[l][c]], [tmpT[e_["xa"]]])
                CP(tm(e_["xa"], 3, 515), b[0][:], [b[1]], [tmpT[e_["xa"]]], eng="act")
                CP(halo_t[:, l, c, 0:3], tm(e_["xa"], 512, 515), [tmpT[e_["xa"]]], [haloT[l][c]])
            for c in range(2):
                e_ = EV[c]
                b = bank(); proj_fm(sl, slT, 2 + c, b)
                ACT(tm(e_["ga"], 0, 512), b[0][:], AF.Gelu_apprx_tanh, [b[1]], [tmpT[e_["ga"]]])
            issue_load()
            sl, slT = next_page()
            for c in range(2):
                e_ = EV[c]
                b = bank(); proj_fm(sl, slT, c, b)
                CP(tm(e_["px"], 0, 15), halo_t[:, l, c, 3:18], [haloT[l][c]], [tmpT[e_["px"]]])
                CP(tm(e_["px"], 15, 527), b[0][:], [b[1]], [tmpT[e_["px"]]], eng="act")
                CP(halo_t[:, l, c, 3:18], tm(e_["px"], 512, 527), [tmpT[e_["px"]]], [haloT[l][c]])
            for c in range(2):
                e_ = EV[c]
                b = bank(); proj_fm(sl, slT, 2 + c, b)
                CP(tm(e_["q"], 0, 512), b[0][:], [b[1]], [tmpT[e_["q"]]], eng="act")
            issue_load()

            xc_k = [WK[0], WK[1]]
            for c in range(2):
                e_ = EV[c]
                xa = e_["xa"]; xc = xc_k[c]
                cw = lambda k: pv(32 + c * 4 + k)
                TS(tm(xc, 0, 512), tm(xa, 0, 512), cw(0), pv(40 + c), ALU.mult, ALU.add, [tmpT[xa], cstT], [tmpT[xc]])
                for k in range(1, 4):
                    STT(tm(xc, 0, 512), tm(xa, k, k + 512), cw(k), tm(xc, 0, 512), ALU.mult, ALU.add, [tmpT[xa], tmpT[xc], cstT], [tmpT[xc]])

            sl, slT = next_page()
            for c in range(2):
                e_ = EV[c]
                b = bank(); proj_fm(sl, slT, c, b)
                ACT(tm(e_["sgm"], 0, 512), b[0][:], AF.Sigmoid, [b[1]], [tmpT[e_["sgm"]]])
            for half in range(2):
                b = bank()
                for t2 in range(2):
                    tb = half * 2 + t2
                    for dc in range(8):
                        MM(b[0][:, t2 * 256:(t2 + 1) * 256], u_t[:, dc, tb * 128:(tb + 1) * 128], sl[:, dc * 512 + 256:dc * 512 + 512],
                           dc == 0, dc == 7, [slT, uT[dc]], [b[1]])
                CP(vtok_t[:, half * 2:half * 2 + 2, :], b[0][:].rearrange("p (a b) -> p a b", a=2), [b[1]], [vtokT])
                bv = b[0][:].rearrange("p (a h v) -> p a h v", a=2, h=4)
                for par in range(2):
                    for a_ in range(2):
                        CP(vz_t[:, half * 2 + a_, par::2, 64 * par:64 * par + 64], bv[:, a_, par::2, :], [b[1]], [vzT], eng=("act" if a_ else "dve"))
            issue_load()

            for c in range(2):
                e_ = EV[c]
                xc = xc_k[c]
                r_, i_, a2_, uu_, hs_ = WK[2], WK[3], WK[4], WK[5], WK[6]
                br = bank()
                MM(br[0][:], bd_t[:, l, 0 + c, :], tm(xc, 0, 512), True, True, [cstT, tmpT[xc]], [br[1]])
                bi = bank()
                MM(bi[0][:], bd_t[:, l, 2 + c, :], tm(xc, 0, 512), True, True, [cstT, tmpT[xc]], [bi[1]])
                ACT(tm(r_, 0, 512), br[0][:], AF.Sigmoid, [br[1], cstT], [tmpT[r_]], bias=pv(42 + c))
                ACT(tm(i_, 0, 512), bi[0][:], AF.Sigmoid, [bi[1], cstT], [tmpT[i_]], bias=pv(44 + c))
                ACT(tm(r_, 0, 512), tm(r_, 0, 512), AF.Exp, [tmpT[r_], dvT], [tmpT[r_]], scale=dv_t[:, l, c:c + 1])
                ACT(tm(a2_, 0, 512), tm(r_, 0, 512), AF.Square, [tmpT[r_]], [tmpT[a2_]])
                ACT(tm(a2_, 0, 512), tm(a2_, 0, 512), AF.Sqrt, [tmpT[a2_]], [tmpT[a2_]], bias=1.0, scale=-1.0)
                TTOP(tm(uu_, 0, 512), tm(a2_, 0, 512), tm(i_, 0, 512), ALU.mult, [tmpT[a2_], tmpT[i_]], [tmpT[uu_]])
                TTOP(tm(uu_, 0, 512), tm(uu_, 0, 512), tm(xc, 0, 512), ALU.mult, [tmpT[uu_], tmpT[xc]], [tmpT[uu_]])
                sch.op("dve", lambda e: e.tensor_tensor_scan(out=tm(hs_, 0, 512), data0=tm(r_, 0, 512), data1=tm(uu_, 0, 512),
                                                              initial=hst_t[:, l, c:c + 1], op0=ALU.mult, op1=ALU.add),
                       [tmpT[r_], tmpT[uu_], hstT[l][c]], [tmpT[hs_]])
                CP(hst_t[:, l, c:c + 1], tm(hs_, 511, 512), [tmpT[hs_]], [hstT[l][c]])
                TTOP(ysmg_t[:, 0 + c, :], tm(hs_, 0, 512), tm(e_["ga"], 0, 512), ALU.mult, [tmpT[hs_], tmpT[e_["ga"]]], [ysT[0 + c]])

            sl, slT = next_page()
            for c in range(2):
                e_ = EV[c]
                b = bank(); proj_fm(sl, slT, c, b)
                ACT(tm(e_["sg"], 0, 512), b[0][:], AF.Silu, [b[1]], [tmpT[e_["sg"]]])
            for c in range(2):
                e_ = EV[c]
                b = bank(); proj_fm(sl, slT, 2 + c, b)
                CP(tm(e_["sb"], 0, 512), b[0][:], [b[1]], [tmpT[e_["sb"]]], eng="act")
            issue_load()

            for c in range(2):
                e_ = EV[c]
                px = e_["px"]
                s2, s4, s8, s16, dd = WK[2], WK[3], WK[4], WK[5], WK[6]
                TTOP(tm(s2, 1, 527), tm(px, 1, 527), tm(px, 0, 526), ALU.add, [tmpT[px]], [tmpT[s2]])
                if c == 0:
                    TTOP(tmp_t[64:128, s4 * TW + 3:s4 * TW + 527], tmp_t[64:128, s2 * TW + 3:s2 * TW + 527],
                         tmp_t[64:128, s2 * TW + 1:s2 * TW + 525], ALU.add, [tmpT[s2]], [tmpT[s4]])
                    sel = [(0, 64, s2, 0.5), (64, 128, s4, 0.25)]
                else:
                    TTOP(tm(s4, 3, 527), tm(s2, 3, 527), tm(s2, 1, 525), ALU.add, [tmpT[s2]], [tmpT[s4]])
                    TTOP(tm(s8, 7, 527), tm(s4, 7, 527), tm(s4, 3, 523), ALU.add, [tmpT[s4]], [tmpT[s8]])
                    TTOP(tmp_t[64:128, s16 * TW + 15:s16 * TW + 527], tmp_t[64:128, s8 * TW + 15:s8 * TW + 527],
                         tmp_t[64:128, s8 * TW + 7:s8 * TW + 519], ALU.add, [tmpT[s8]], [tmpT[s16]])
                    sel = [(0, 64, s8, 0.125), (64, 128, s16, 0.0625)]
                for (p0, p1, sk, inv) in sel:
                    STT(tmp_t[p0:p1, dd * TW:dd * TW + 512], tmp_t[p0:p1, sk * TW + 15:sk * TW + 527], inv,
                        tmp_t[p0:p1, px * TW + 15:px * TW + 527], ALU.mult, ALU.subtract, [tmpT[sk], tmpT[px]], [tmpT[dd]])
                    if i == 0:
                        TTOP(tmp_t[p0:p1, dd * TW:dd * TW + 15], tmp_t[p0:p1, sk * TW + 15:sk * TW + 30], invc[p0:p1, c, 0:15],
                             ALU.mult, [tmpT[sk], cstT, tmpT[dd]], [tmpT[dd]])
                        TTOP(tmp_t[p0:p1, dd * TW:dd * TW + 15], tmp_t[p0:p1, dd * TW:dd * TW + 15], tmp_t[p0:p1, px * TW + 15:px * TW + 30],
                             ALU.subtract, [tmpT[px], tmpT[dd]], [tmpT[dd]])
                bp = bank()
                MM(bp[0][:], bd_t[:, l, 4 + c, :], tm(dd, 0, 512), True, True, [cstT, tmpT[dd]], [bp[1]])
                ACT(ysmg_t[:, 2 + c, :], bp[0][:], AF.Copy, [bp[1], cstT], [ysT[2 + c]], scale=pv(48 + c))

            sl, slT = next_page()
            for c in range(2):
                e_ = EV[c]
                b = bank(); proj_fm(sl, slT, c, b)
                CP(tm(e_["sc"], 0, 512), b[0][:], [b[1]], [tmpT[e_["sc"]]], eng="act")
            for c in range(2):
                e_ = EV[c]
                z = e_["z"]
                b = bank(); proj_fm(sl, slT, 2 + c, b)
                CP(tm(z, 0, 2), halo_t[:, l, c, 18:20], [haloT[l][c]], [tmpT[z]])
                TTOP(tm(z, 2, 514), b[0][:], tm(e_["sc"], 0, 512), ALU.mult, [b[1], tmpT[e_["sc"]]], [tmpT[z]])
                CP(halo_t[:, l, c, 18:20], tm(z, 512, 514), [tmpT[z]], [haloT[l][c]])
            issue_load()
            for c in range(2):
                e_ = EV[c]
                z = e_["z"]; acc = WK[2 + c]
                sw = lambda k: pv(55 + c * 3 + k)
                TS(tm(acc, 0, 512), tm(z, 0, 512), sw(0), None, ALU.mult, None, [tmpT[z], cstT], [tmpT[acc]])
                STT(tm(acc, 0, 512), tm(z, 1, 513), sw(1), tm(acc, 0, 512), ALU.mult, ALU.add, [tmpT[z], tmpT[acc], cstT], [tmpT[acc]])
                STT(tm(acc, 0, 512), tm(z, 2, 514), sw(2), tm(acc, 0, 512), ALU.mult, ALU.add, [tmpT[z], tmpT[acc], cstT], [tmpT[acc]])
                TTOP(ysmg_t[:, 6 + c, :], tm(acc, 0, 512), tm(e_["sb"], 0, 512), ALU.mult, [tmpT[acc], tmpT[e_["sb"]]], [ysT[6 + c]])

            if _STAGE < 5:
                raise _Stop()

            def hgrn_steps():
                for hc in range(2):
                    e_ = EV[hc]
                    q, fk, sg = e_["q"], e_["sgm"], e_["sg"]
                    w0, w1, w2, w3, w4 = WK[0], WK[1], WK[4], WK[5], WK[6]
                    hb = [WK[7], WK[8], WK[2], WK[3], WK[9]]
                    qe_s, qe_i, ke_i, osq = tmb(hb[0], 0), tmb(hb[0], 1), tmb(hb[1], 0), tmb(hb[1], 1)
                    kA, kB = tmb(hb[2], 0), tmb(hb[2], 1)
                    scm = [tmb(hb[3], 0), tmb(hb[3], 1)]
                    sbd = tmp_t[:, hb[4] * TW:hb[4] * TW + 512].bitcast(BF16)
                    v3 = lambda k: tm(k, 0, 512).rearrange("p (n t) -> p n t", t=64)
                    lb = dv_t[:, l, 2 + hc:3 + hc]
                    omlb = dv_t[:, l, 4 + hc:5 + hc]
                    TS(tm(fk, 0, 512), tm(fk, 0, 512), omlb, lb, ALU.mult, ALU.add, [tmpT[fk], dvT], [tmpT[fk]])
                    ACT(tm(w0, 0, 512), tm(fk, 0, 512), AF.Ln, [tmpT[fk]], [tmpT[w0]])
                    TS(tm(fk, 0, 512), tm(fk, 0, 512), -1.0, 1.0, ALU.mult, ALU.add, [tmpT[fk]], [tmpT[fk]])
                    sch.op("dve", lambda e: e.tensor_tensor_scan(out=tm(w1, 0, 512), data0=nstart, data1=tm(w0, 0, 512), initial=0.0,
                                                                  op0=ALU.mult, op1=ALU.add), [cstT, tmpT[w0]], [tmpT[w1]])
                    blast = v3(w1)[:, :, 63]
                    bmid = v3(w1)[:, :, 31]
                    TTOP(v3(w0), blast.unsqueeze(2).to_broadcast([P, 8, 64]), v3(w1), ALU.subtract, [tmpT[w1], tmpT[w0]], [tmpT[w0]])
                    ACT(tm(w2, 0, 512), tm(w1, 0, 512), AF.Exp, [tmpT[w1]], [tmpT[w2]])
                    ACT(tm(w0, 0, 512), tm(w0, 0, 512), AF.Exp, [tmpT[w0]], [tmpT[w0]])
                    ACT(sm_t[:, 0, :], bmid, AF.Exp, [tmpT[w1]], [smT], scale=-1.0)
                    TTOP(sm_t[:, 1, :], bmid, blast, ALU.subtract, [tmpT[w1]], [smT])
                    ACT(sm_t[:, 2, :], sm_t[:, 1, :], AF.Exp, [smT], [smT])
                    TTOP(qe_s, tm(q, 0, 512), tm(w2, 0, 512), ALU.mult, [tmpT[q], tmpT[w2]], [tmpT[hb[0]]])
                    TTOP(tm(w0, 0, 512), tm(fk, 0, 512), tm(w0, 0, 512), ALU.mult, [tmpT[fk], tmpT[w0]], [tmpT[w0]])
                    TTOP(qe_i.rearrange("p (n t) -> p n t", t=64), qe_s.rearrange("p (n t) -> p n t", t=64),
                         sm_t[:, 0, :].unsqueeze(2).to_broadcast([P, 8, 64]), ALU.mult, [tmpT[hb[0]], smT], [tmpT[hb[0]]])
                    TTOP(ke_i.rearrange("p (n t) -> p n t", t=64), v3(w0),
                         sm_t[:, 2, :].unsqueeze(2).to_broadcast([P, 8, 64]), ALU.mult, [tmpT[w0], smT], [tmpT[hb[1]]])
                    dec = v3(w2)[:, :, 63]
                    d3 = tm(w3, 0, 512).rearrange("p (v n) -> p v n", n=8)
                    CP(d3, dec.unsqueeze(1).to_broadcast([P, 64, 8]), [tmpT[w2]], [tmpT[w3]], eng=pool_eng[0])
                    sch.op(pool_eng[0], lambda e: e.memset(d3[:, :, 0], 0.0), [], [tmpT[w3]])
                    yield
                    bt = bank()
                    for tb in range(4):
                        TR(bt[0][:, tb * 128:(tb + 1) * 128], tm(w0, tb * 128, (tb + 1) * 128), [tmpT[w0]], [bt[1]])
                    yield
                    sch.op(pool_eng[0], lambda e: e.memset(kA[64:128, :], 0.0), [], [tmpT[hb[2]]])
                    sch.op(pool_eng[0], lambda e: e.memset(kB[0:64, :], 0.0), [], [tmpT[hb[2]]])
                    CP(kA[0:64, :], bt[0][0:64, :], [bt[1]], [tmpT[hb[2]]], eng="dve")
                    CP(kB[64:128, :], bt[0][64:128, :], [bt[1]], [tmpT[hb[2]]], eng="act")
                    bus = [bank(), bank()]
                    for n in range(8):
                        km = kA if n % 2 == 0 else kB
                        j = n // 2
                        bu = bus[n // 4]
                        MM(bu[0][:, (n % 4) * 128:(n % 4 + 1) * 128], km[:, j * 128:(j + 1) * 128], vtok_t[:, j, hc * 128:(hc + 1) * 128], True, True,
                           [tmpT[hb[2]], vtokT], [bu[1]])
                    yield
                    u3 = tm(w1, 0, 512).rearrange("p (v n) -> p v n", n=8)
                    for bi_ in range(2):
                        for hp in range(2):
                            src = bus[bi_][0][64 * hp:64 * hp + 64, :].rearrange("p (n c) -> p n c", c=128)[:, :, 64 * hp:64 * hp + 64]
                            CP(u3[64 * hp:64 * hp + 64, :, 4 * bi_:4 * bi_ + 4], src.rearrange("p n v -> p v n"), [bus[bi_][1], tmpT[w1]], [tmpT[w1]],
                               eng=("dve" if hp == 0 else "act"))
                    STT(u3[:, :, 0], sst_t[:, l, hc, :], tm(w2, 63, 64), u3[:, :, 0], ALU.mult, ALU.add, [sstT[l][hc], tmpT[w2], tmpT[w1]], [tmpT[w1]])
                    sch.op("dve", lambda e: e.tensor_tensor_scan(out=tm(w4, 0, 512), data0=tm(w3, 0, 512), data1=tm(w1, 0, 512), initial=0.0,
                                                                  op0=ALU.mult, op1=ALU.add), [tmpT[w3], tmpT[w1]], [tmpT[w4]])
                    s3 = tm(w4, 0, 512).rearrange("p (v n) -> p v n", n=8)
                    sbd3 = sbd.rearrange("p (n c) -> p n c", c=128)
                    sch.op(pool_eng[0], lambda e: e.memset(sbd, 0.0), [], [tmpT[hb[4]]])
                    for hp in range(2):
                        ps_ = slice(64 * hp, 64 * hp + 64)
                        CP(sbd3[ps_, 0, 64 * hp:64 * hp + 64], sst_t[ps_, l, hc, :], [sstT[l][hc]], [tmpT[hb[4]]])
                        CP(sbd3[ps_, 1:8, 64 * hp:64 * hp + 64], s3[ps_, :, 0:7].rearrange("p v n -> p n v"), [tmpT[w4]], [tmpT[hb[4]]])
                    CP(sst_t[:, l, hc, :], s3[:, :, 7], [tmpT[w4]], [sstT[l][hc]])
                    bsb = [bank(), bank()]
                    for hp in range(2):
                        for j in range(4):
                            MM(bsb[hp][0][:, j * 128:(j + 1) * 128], ke_i[64 * hp:64 * hp + 64, j * 128:(j + 1) * 128],
                               qe_i[64 * hp:64 * hp + 64, j * 128:(j + 1) * 128], True, True, [tmpT[hb[0]], tmpT[hb[1]]], [bsb[hp][1]])
                    for hp in range(2):
                        TTOP(scm[hp].rearrange("p (a t) -> p a t", t=128), bsb[hp][0][:].rearrange("p (a t) -> p a t", t=128),
                             cmask2.unsqueeze(1).to_broadcast([P, 4, 128]), ALU.mult, [bsb[hp][1], cstT], [tmpT[hb[3]]])
                    yield
                    bo = bank()
                    for j in range(4):
                        out_ = bo[0][:, j * 128:(j + 1) * 128]
                        MM(out_, vz_t[:, j, 2 * hc, :], scm[0][:, j * 128:(j + 1) * 128], True, False, [vzT, tmpT[hb[3]]], [bo[1]])
                        MM(out_, vz_t[:, j, 2 * hc + 1, :], scm[1][:, j * 128:(j + 1) * 128], False, False, [vzT, tmpT[hb[3]]], [bo[1]])
                        for n in (2 * j, 2 * j + 1):
                            MM(bo[0][:, n * 64:(n + 1) * 64], sbd3[:, n, :], qe_s[:, n * 64:(n + 1) * 64], False, n == 2 * j + 1,
                               [tmpT[hb[4]], tmpT[hb[0]]], [bo[1]])
                    yield
                    ACT(osq, bo[0][:], AF.Square, [bo[1]], [tmpT[hb[1]]])
                    bn = bank()
                    MM(bn[0][:], bones_bf, osq, True, True, [cbfT, tmpT[hb[1]]], [bn[1]])
                    ACT(tm(w3, 0, 512), bn[0][:], AF.Sqrt, [bn[1]], [tmpT[w3]], bias=EPS, scale=1.0 / 64)
                    sch.op("dve", lambda e: e.reciprocal(out=tm(w3, 0, 512), in_=tm(w3, 0, 512)), [tmpT[w3]], [tmpT[w3]])
                    TTOP(tm(w4, 0, 512), bo[0][:], tm(w3, 0, 512), ALU.mult, [bo[1], tmpT[w3]], [tmpT[w4]])
                    STT(ysmg_t[:, 4 + hc, :], tm(w4, 0, 512), pv(54), tm(sg, 0, 512), ALU.mult, ALU.mult, [tmpT[w4], tmpT[sg], cstT], [ysT[4 + hc]])
                    yield

            if _STAGE < 6:
                raise _Stop()
            hg = hgrn_steps()
            next(hg, None)
            slot_i = 0
            for ki, k in enumerate((0, 1, 3, 2)):
                if ki == 3:
                    for _ in hg:
                        pass
                for hh in range(2):
                    sl, slT = next_page()
                    wsl, wslT = next_wb()
                    for e4 in range(4):
                        e = hh * 4 + e4
                        gb = bank()
                        for dc in range(8):
                            MM(gb[0][:], sl[:, dc * 512 + e4 * 128:dc * 512 + (e4 + 1) * 128], u_t[:, dc, :], dc == 0, dc == 7, [slT, uT[dc]], [gb[1]])
                        bb = bank()
                        for cc in range(2):
                            MM(bb[0][:], wsl[:, cc * 512 + e4 * 128:cc * 512 + (e4 + 1) * 128], ysmg_t[:, k * 2 + cc, :], cc == 0, cc == 1,
                               [wslT, ysT[k * 2 + cc]], [bb[1]])
                        gs = EV[slot_i % 2]["xa"]
                        mt = EV[slot_i % 2]["ga"]
                        ACT(tm(gs, 0, 512), gb[0][:], AF.Sigmoid, [gb[1]], [tmpT[gs]])
                        if ki == 0:
                            TTOP(mix_t[:, e, :], bb[0][:], tm(gs, 0, 512), ALU.mult, [bb[1], tmpT[gs]], [mixT[e]])
                        else:
                            TTOP(tm(mt, 0, 512), bb[0][:], tm(gs, 0, 512), ALU.mult, [bb[1], tmpT[gs]], [tmpT[mt]])
                            if ki < 3:
                                TTOP(mix_t[:, e, :], mix_t[:, e, :], tm(mt, 0, 512), ALU.add, [mixT[e], tmpT[mt]], [mixT[e]], eng=pool_eng[0])
                            else:
                                TTOP(ysmg_t[:, 8 + e, :], mix_t[:, e, :], tm(mt, 0, 512), ALU.add, [mixT[e], tmpT[mt]], [mgT[e]], eng=pool_eng[0])
                        slot_i += 1
                        if ki < 3 and slot_i % 2 == 0:
                            next(hg, None)
                    issue_load()
                    issue_wb()
            for hh in range(2):
                sl, slT = next_page()
                for e2 in range(4):
                    ec = hh * 4 + e2
                    b = bank()
                    for dc in range(8):
                        MM(b[0][:], sl[:, dc * 512 + e2 * 128:dc * 512 + (e2 + 1) * 128], ysmg_t[:, 8 + dc, :], dc == 0, dc == 7,
                           [slT, mgT[dc]], [b[1]])
                    CP(mix_t[:, ec, :], b[0][:], [b[1]], [mixT[ec]], eng="act")
                issue_load()
            norm(lambda c: mix_t[:, c, :], mixT, 8, l, "res")

            if _STAGE < 7:
                raise _Stop()
            norm(lambda c: h_t[:, c, :], hT, 16, l, "u")
            for g in range(8):
                sl, slT = next_page()
                for j in range(4):
                    fc = g * 4 + j
                    b = bank()
                    for dc in range(8):
                        MM(b[0][:], sl[:, dc * 512 + j * 128:dc * 512 + (j + 1) * 128], u_t[:, dc, :], dc == 0, dc == 7, [slT, uT[dc]], [b[1]])
                    rl = WK[fc % 4]
                    ACT(tm(rl, 0, 512), b[0][:], AF.Relu, [b[1]], [tmpT[rl]])
                    TTOP(hid_ap[:, fc, :], b[0][:], tm(rl, 0, 512), ALU.mult, [b[1], tmpT[rl]], [hidT[fc]])
                issue_load()
            for e in range(8):
                sl, slT = next_page()
                b = bank()
                for fc in range(32):
                    MM(b[0][:], sl[:, fc * 128:(fc + 1) * 128], hid_ap[:, fc, :], fc == 0, fc == 31, [slT, hidT[fc]], [b[1]])
                CP(mix_t[:, e, :], b[0][:], [b[1]], [mixT[e]], eng="act")
                issue_load()
            norm(lambda c: mix_t[:, c, :], mixT, 24, l, "res")

            if l == L - 1:
                for tb in range(4):
                    for half in range(2):
                        b = bank()
                        for cc in range(4):
                            c = half * 4 + cc
                            TR(b[0][:, cc * 128:(cc + 1) * 128], h_t[:, c, tb * 128:(tb + 1) * 128], [hT[c]], [b[1]])
                        CP(io_ap[:, tb, half * 512:(half + 1) * 512], b[0][:], [b[1]], [ioT[tb]], eng=("act" if half else "dve"))
                sch.op("sp", lambda e: e.dma_start(out=y_d[i * TT:(i + 1) * TT, :].rearrange("(tb p) d -> p tb d", p=P), in_=io_ap),
                       [ioT], [], dma_sem="yout")

        try:
            if _STAGE < 3:
                raise _Stop()
            for i in range(NT):
                for l in range(L):
                    tile_layer(i, l)
            flush_pend()
            flush_pend_wb()
        except _Stop:
            for nm in sch.sem:
                if sch.val[nm] > 0:
                    nc.sync.wait_ge(sch.sem[nm], sch.val[nm])
        nc.sync.wait_ge(sch.sem["yout"], sch.val["yout"])
        if dbg:
            print("sem values:", sch.val)
    return nc


def _cols(v):
    v = np.asarray(v, np.float32).reshape(-1, P)
    return v.T


def host_consts():
    c = np.zeros((P, NCST), np.float32)
    c[:, 0:128] = np.eye(P, dtype=np.float32)
    c[:, 128:256] = 1.0
    p = np.arange(P)
    c[:, 256:384] = (p[:, None] // 64 == p[None, :] // 64).astype(np.float32)
    c[:, 384:512] = ((p[:, None] // 64 == p[None, :] // 64) & ((p[:, None] % 64) <= (p[None, :] % 64))).astype(np.float32)
    t = np.arange(512)
    c[:, 512:1024] = (t % 64 != 0).astype(np.float32)[None, :]
    wins = np.array([[2, 4], [8, 16]])
    for ch in range(2):
        w = wins[ch][p // 64].astype(np.float32)
        tt = np.arange(16, dtype=np.float32)[None, :] + 1.0
        c[:, 1024 + ch * 16:1024 + (ch + 1) * 16] = 1.0 / np.minimum(tt, w[:, None])
    return c


def host_params(inp, L):
    pv = np.zeros((P, L, NV), np.float32)
    bd = np.zeros((P, L, 6, P), np.float32)
    for l in range(L):
        pv[:, l, 0:8] = _cols(inp["norm_mix_pre"][l])
        pv[:, l, 8:16] = _cols(inp["norm_mix_post"][l])
        pv[:, l, 16:24] = _cols(inp["norm_mlp_pre"][l])
        pv[:, l, 24:32] = _cols(inp["norm_mlp_post"][l])
        cw = np.asarray(inp["lru_conv_w"][l], np.float32)
        for c in range(2):
            for k in range(4):
                pv[:, l, 32 + c * 4 + k] = cw[k, c * 128:(c + 1) * 128]
        pv[:, l, 40:42] = _cols(inp["lru_conv_b"][l])
        pv[:, l, 42:44] = _cols(inp["lru_b_a"][l])
        pv[:, l, 44:46] = _cols(inp["lru_b_x"][l])
        pv[:, l, 46:48] = _cols(inp["lru_lambda"][l])
        pv[:, l, 48:50] = _cols(inp["pool_scale"][l])
        pv[:, l, 50:52] = _cols(inp["hgrn_lower_bound"][0])
        pv[:, l, 52:54] = _cols(inp["hgrn_lower_bound"][min(1, L - 1)])
        pv[:, l, 54] = np.tile(np.asarray(inp["hgrn_norm"][l], np.float32), 2)
        sw = np.asarray(inp["sconv_w"][l], np.float32)
        for c in range(2):
            for k in range(3):
                pv[:, l, 55 + c * 3 + k] = sw[k, c * 128:(c + 1) * 128]
        wa = np.asarray(inp["lru_w_a"][l], np.float32)
        wx = np.asarray(inp["lru_w_x"][l], np.float32)
        pw = np.asarray(inp["pool_w"][l], np.float32)
        for c in range(2):
            for n in range(4):
                bd[n * 32:(n + 1) * 32, l, 0 + c, n * 32:(n + 1) * 32] = wa[c * 4 + n]
                bd[n * 32:(n + 1) * 32, l, 2 + c, n * 32:(n + 1) * 32] = wx[c * 4 + n]
            for n in range(2):
                bd[n * 64:(n + 1) * 64, l, 4 + c, n * 64:(n + 1) * 64] = pw[c * 2 + n]
    return pv, bd


def make_in_maps(inp, n_cores, L):
    pv, bd = host_params(inp, L)
    cst = host_consts()
    f = lambda a: np.ascontiguousarray(np.asarray(a, np.float32))
    shared = {
        "w_in": f(inp["w_in"])[:L], "w_branch": f(inp["w_branch"])[:L].reshape(L, D, D), "w_out": f(inp["w_out"])[:L],
        "w_up": f(inp["w_up"])[:L], "w_down": f(inp["w_down"])[:L], "pvec": pv, "bdw": bd, "cst": cst,
    }
    x = f(inp["x"])
    return [dict(shared, x=np.ascontiguousarray(x[b])) for b in range(n_cores)]


def kernel(**inputs):
    B, S, _ = inputs["x"].shape
    L = inputs["w_in"].shape[0]
    nc = build_program(S // TT, L)
    in_maps = make_in_maps(inputs, B, L)
    res = run_bass_kernel_spmd(nc, in_maps, core_ids=list(range(B)))
    return np.stack([np.asarray(r["y"], np.float32) for r in res.results], axis=0)
```

```python
import numpy as np
from contextlib import ExitStack
import concourse.bass as bass
import concourse.mybir as mybir
from concourse.bass_utils import run_bass_kernel_spmd

F32 = mybir.dt.float32
BF16 = mybir.dt.bfloat16
AF = mybir.ActivationFunctionType
ALU = mybir.AluOpType

P = 128
TT = 512
D = 1024
NSLOT = 4
NPG = 31
PGE = 4096
EPS = 1e-6
TW = 528
NTMP = 28
NCST = 1058
NV = 64
_STAGE = 99


class _Stop(Exception):
    pass


class T:
    __slots__ = ("name", "w", "r")

    def __init__(self, name):
        self.name = name
        self.w = None
        self.r = {}


def _flat(ts):
    for t in ts:
        if isinstance(t, T):
            yield t
        else:
            yield from _flat(t)


class Sch:
    def __init__(self, nc, es):
        self.nc = nc
        self.es = es
        self.E = {"pe": nc.tensor, "act": nc.scalar, "dve": nc.vector, "pool": nc.gpsimd, "sp": nc.sync}
        self.sem = {}
        self.val = {}
        self.waited = {e: {} for e in self.E}
        for e in ("pe", "act", "dve", "pool"):
            self.newsem(e)

    def newsem(self, name):
        self.sem[name] = self.es.enter_context(self.nc.semaphore(name))
        self.val[name] = 0

    def op(self, eng, fn, reads=(), writes=(), dma_sem=None):
        deps = {}
        R = list(_flat(reads))
        Wr = list(_flat(writes))
        for t in R:
            if t.w is not None:
                s, v = t.w
                deps[s] = max(deps.get(s, 0), v)
        for t in Wr:
            if t.w is not None:
                s, v = t.w
                deps[s] = max(deps.get(s, 0), v)
            for s, v in t.r.items():
                deps[s] = max(deps.get(s, 0), v)
        E = self.E[eng]
        for s, v in deps.items():
            if eng == "pe" and s == "pe":
                continue
            if self.waited[eng].get(s, 0) < v:
                E.wait_ge(self.sem[s], v)
                self.waited[eng][s] = v
        inst = fn(E)
        if dma_sem is not None:
            s, inc = dma_sem, 16
        else:
            s, inc = eng, 1
        self.val[s] += inc
        inst.then_inc(self.sem[s], inc)
        v = self.val[s]
        for t in R:
            t.r[s] = v
        for t in Wr:
            t.w = (s, v)
            t.r = {}
        return inst


def build_program(NT, L, dbg=False):
    SEQ = NT * TT
    nc = bass.Bass("TRN2", target_bir_lowering=False)
    x_d = nc.dram_tensor("x", [SEQ, D], F32, kind="ExternalInput").ap()
    w_in_d = nc.dram_tensor("w_in", [L, D, 6656], F32, kind="ExternalInput").ap()
    w_br_d = nc.dram_tensor("w_branch", [L, D, D], F32, kind="ExternalInput").ap()
    w_out_d = nc.dram_tensor("w_out", [L, D, D], F32, kind="ExternalInput").ap()
    w_up_d = nc.dram_tensor("w_up", [L, D, 4096], F32, kind="ExternalInput").ap()
    w_dn_d = nc.dram_tensor("w_down", [L, 4096, D], F32, kind="ExternalInput").ap()
    pv_d = nc.dram_tensor("pvec", [P, L, NV], F32, kind="ExternalInput").ap()
    bd_d = nc.dram_tensor("bdw", [P, L, 6, P], F32, kind="ExternalInput").ap()
    cst_d = nc.dram_tensor("cst", [P, NCST], F32, kind="ExternalInput").ap()
    y_d = nc.dram_tensor("y", [SEQ, D], F32, kind="ExternalOutput").ap()
    wpg_d = nc.dram_tensor("wpg", [L, NPG, P, PGE], BF16, kind="Internal").ap()
    wbp_d = nc.dram_tensor("wbp", [L, 8, P, 1024], BF16, kind="Internal").ap()

    es = ExitStack()
    with es:
        sch = Sch(nc, es)
        for s in range(NSLOT):
            sch.newsem(f"slot{s}")
        for s in range(2):
            sch.newsem(f"wbs{s}")
        for nm in ("cst", "xin", "yout"):
            sch.newsem(nm)
        for j in range(3):
            sch.newsem(f"stg{j}")
        for j in range(2):
            sch.newsem(f"bgi{j}")
            sch.newsem(f"wst{j}")
        sch.newsem("wbi")
        for s in range(NSLOT):
            sch.newsem(f"st{s}")

        def sb(name, shape, dt):
            return es.enter_context(nc.sbuf_tensor(name, shape, dt))

        h_t = sb("h", [P, 8, TT], F32)
        u_t = sb("u", [P, 8, TT], BF16)
        mix_t = sb("mix", [P, 8, TT], F32)
        ysmg_t = sb("ysmg", [P, 16, TT], BF16)
        slots = [sb(f"slot{s}", [P, PGE], BF16) for s in range(NSLOT)]
        wbs = [sb(f"wbs{s}", [P, 1024], BF16) for s in range(2)]
        tmp_t = sb("tmp", [P, NTMP * TW], F32)
        cst_t = sb("cst_sb", [P, NCST], F32)
        pv_t = sb("pv_sb", [P, L, NV], F32)
        bd_t = sb("bd_sb", [P, L, 6, P], F32)
        dv_t = sb("dv_sb", [P, L, 8], F32)
        cbf_t = sb("cbf", [P, 2, P], BF16)
        sqb_t = sb("sqb", [P, 2, TT], BF16)
        rstd_t = sb("rstd", [P, TT], F32)
        vz_t = sb("vz", [P, 4, 4, P], BF16)
        halo_t = sb("halo", [P, L, 2, 20], F32)
        hst_t = sb("hst", [P, L, 2], F32)
        sst_t = sb("sst", [P, L, 2, 64], F32)
        sm_t = sb("sm", [P, 4, 8], F32)
        vtok_t = sb("vtok", [P, 4, 256], BF16)
        bgs_t = sb("bgs", [P, 2, 2048], F32)
        wbst_t = sb("wbst", [P, 1024], F32)
        wbstT = T("wbst")
        bgsT = [T("bgs0"), T("bgs1")]
        bgoT = [T("bgo0"), T("bgo1")]
        psb = [es.enter_context(nc.psum_tensor(f"ps{i}", [P, TT], F32)) for i in range(8)]

        hT = [T(f"h{c}") for c in range(8)]
        uT = [T(f"u{c}") for c in range(8)]
        mixT = [T(f"mix{c}") for c in range(8)]
        ioT = [[mixT[2 * tb], mixT[2 * tb + 1]] for tb in range(4)]
        ysT = [T(f"ys{c}") for c in range(8)]
        mgT = [T(f"mg{c}") for c in range(8)]
        slotT = [T(f"slot{s}") for s in range(NSLOT)]
        wbsT = [T(f"wbs{s}") for s in range(2)]
        tmpT = [T(f"tmp{k}") for k in range(NTMP)]
        hidT = [[tmpT[k] for k in range((fc * 256) // TW, ((fc + 1) * 256 - 1) // TW + 1)] for fc in range(32)]
        psT = [T(f"ps{i}") for i in range(8)]
        cstT = T("cst")
        cbfT = T("cbf")
        dvT = T("dv")
        sqbT = [T("sqb0"), T("sqb1")]
        rstdT = T("rstd")
        vzT = T("vz")
        haloT = [[T(f"halo{l}{c}") for c in range(2)] for l in range(L)]
        hstT = [[T(f"hst{l}{c}") for c in range(2)] for l in range(L)]
        sstT = [[T(f"sst{l}{c}") for c in range(2)] for l in range(L)]
        smT = T("sm")
        vtokT = T("vtok")
        wpgT = [T(f"wpg{l}") for l in range(L)]

        io_ap = mix_t[:].rearrange("p c t -> p (c t)").rearrange("p (tb d) -> p tb d", tb=4)
        hid_ap = tmp_t[:, 0:8192].bitcast(BF16).rearrange("p (f t) -> p f t", t=TT)

        def tm(k, a=0, b=TW):
            return tmp_t[:, k * TW + a:k * TW + b]

        def tmb(k, half):
            return tmp_t[:, k * TW:k * TW + 512].bitcast(BF16)[:, half * 512:(half + 1) * 512]

        ident = cst_t[:, 0:128]
        cmask2 = cst_t[:, 384:512]
        nstart = cst_t[:, 512:1024]
        invc = cst_t[:, 1024:1056].rearrange("p (c t) -> p c t", c=2)
        ones_bf = cbf_t[:, 0, :]
        bones_bf = cbf_t[:, 1, :]

        bank_ctr = [0]

        def bank():
            i = bank_ctr[0] % 8
            bank_ctr[0] += 1
            return psb[i], psT[i]

        def ACT(out, in_, func, R, Wt, bias=None, scale=None):
            kw = {}
            if bias is not None:
                kw["bias"] = bias
            if scale is not None:
                kw["scale"] = scale
            return sch.op("act", lambda e: e.activation(out=out, in_=in_, func=func, **kw), R, Wt)

        def TTOP(out, in0, in1, op, R, Wt, eng="dve"):
            return sch.op(eng, lambda e: e.tensor_tensor(out=out, in0=in0, in1=in1, op=op), R, Wt)

        def TS(out, in0, s1, s2, op0, op1, R, Wt, eng="dve"):
            if op1 is None:
                return sch.op(eng, lambda e: e.tensor_scalar(out=out, in0=in0, scalar1=s1, scalar2=None, op0=op0), R, Wt)
            return sch.op(eng, lambda e: e.tensor_scalar(out=out, in0=in0, scalar1=s1, scalar2=s2, op0=op0, op1=op1), R, Wt)

        def STT(out, in0, sc, in1, op0, op1, R, Wt):
            return sch.op("dve", lambda e: e.scalar_tensor_tensor(out=out, in0=in0, scalar=sc, in1=in1, op0=op0, op1=op1), R, Wt)

        def CP(out, in_, R, Wt, eng="dve"):
            if eng == "act":
                return sch.op("act", lambda e: e.copy(out=out, in_=in_), R, Wt)
            return sch.op(eng, lambda e: e.tensor_copy(out=out, in_=in_), R, Wt)

        def MM(out, lhsT, rhs, st, sp, R, Wt):
            return sch.op("pe", lambda e: e.matmul(out, lhsT=lhsT, rhs=rhs, start=st, stop=sp), R, Wt)

        def TR(out, in_, R, Wt):
            return sch.op("pe", lambda e: e.transpose(out, in_, ident), R + [cstT], Wt)

        n_c = 0
        for dst, src in ((cst_t[:], cst_d), (pv_t[:], pv_d), (bd_t[:], bd_d)):
            nc.sync.dma_start(out=dst, in_=src).then_inc(sch.sem["cst"], 16)
            n_c += 16
        sch.val["cst"] = n_c
        cstT.w = ("cst", n_c)

        sch.op("pool", lambda e: e.memset(halo_t[:], 0.0), [], [haloT])
        sch.op("pool", lambda e: e.memset(hst_t[:], 0.0), [], [hstT])
        sch.op("pool", lambda e: e.memset(sst_t[:], 0.0), [], [sstT])
        sch.op("pool", lambda e: e.memset(vz_t[:], 0.0), [], [vzT])
        CP(cbf_t[:], cst_t[:, 128:384].rearrange("p (a b) -> p a b", a=2), [cstT], [cbfT])
        for l in range(L):
            lam = pv_t[:, l, 46:48]
            ACT(dv_t[:, l, 0:2], lam, AF.Exp, [cstT], [dvT], scale=-1.0)
            TS(dv_t[:, l, 0:2], dv_t[:, l, 0:2], 1.0, None, ALU.add, None, [dvT], [dvT])
            ACT(dv_t[:, l, 0:2], dv_t[:, l, 0:2], AF.Ln, [dvT], [dvT])
            TS(dv_t[:, l, 0:2], dv_t[:, l, 0:2], -8.0, None, ALU.mult, None, [dvT], [dvT])
            if l == 0:
                sch.op("dve", lambda e: e.memset(dv_t[:, l, 2:4], 0.0), [], [dvT])
            else:
                assert l == 1
                TTOP(dv_t[:, l, 2:4], pv_t[:, l, 52:54], pv_t[:, l, 50:52], ALU.subtract, [cstT], [dvT])
                ACT(dv_t[:, l, 2:4], dv_t[:, l, 2:4], AF.Sigmoid, [dvT], [dvT])
            TS(dv_t[:, l, 4:6], dv_t[:, l, 2:4], -1.0, 1.0, ALU.mult, ALU.add, [dvT], [dvT])

        NSTG = 3
        stgT = [[tmpT[k] for k in range((j * 4096) // TW, ((j + 1) * 4096 - 1) // TW + 1)] for j in range(NSTG)]
        cvt_ctr = [0]
        cast_engs = ("dve", "act", "pool")
        KH = [(k, hh) for k in (0, 1, 3, 2) for hh in range(2)]
        depT = {}

        def page_pieces(l):
            wi = w_in_d[l]

            def dcp(src2d, pg):
                return [(h * 2048, 2048, (lambda a: a.rearrange("p (dc n) -> p dc n", dc=4)),
                         src2d[h * 512:(h + 1) * 512, :].rearrange("(dc p) n -> p dc n", p=P),
                         wpg_d[l, pg][:, h * 2048:(h + 1) * 2048], ("pg", pg)) for h in range(2)]
            for g in range(5):
                yield dcp(wi[:, g * 512:(g + 1) * 512], g)
            for pi_, (k, hh) in enumerate(KH):
                c0 = 2560 + k * 1024 + hh * 512
                yield dcp(wi[:, c0:c0 + 512], 5 + pi_)
            for hh in range(2):
                yield dcp(w_out_d[l][:, hh * 512:(hh + 1) * 512], 13 + hh)
            for g in range(8):
                yield dcp(w_up_d[l][:, g * 512:(g + 1) * 512], 15 + g)
            for e in range(8):
                yield [(h * 2048, 2048, (lambda a: a.rearrange("p (fc n) -> p fc n", fc=16)),
                        w_dn_d[l][h * 2048:(h + 1) * 2048, e * 128:(e + 1) * 128].rearrange("(fc p) n -> p fc n", p=P),
                        wpg_d[l, 23 + e][:, h * 2048:(h + 1) * 2048], ("pg", 23 + e)) for h in range(2)]
            for half in range(2):
                pcs = []
                for q4 in range(4):
                    k, hh = KH[half * 4 + q4]
                    pcs.append((q4 * 1024, 1024, (lambda a: a.rearrange("p (cc n) -> p cc n", cc=2)),
                                w_br_d[l][k * 256:(k + 1) * 256, hh * 512:(hh + 1) * 512].rearrange("(cc p) n -> p cc n", p=P),
                                wbp_d[l, half * 4 + q4], ("wb", half * 4 + q4)))
                yield pcs

        def convert_fg(pieces):
            n = cvt_ctr[0]
            cvt_ctr[0] += 1
            j = n % NSTG
            s_ = n % NSLOT
            stg = tmp_t[:, j * 4096:(j + 1) * 4096]
            for (off, size, vf, src, dst, key) in pieces:
                sch.op("sp", lambda e: e.dma_start(out=vf(stg[:, off:off + size]), in_=src), [], [stgT[j]], dma_sem=f"stg{j}")
            CP(slots[s_][:], stg, [stgT[j]], [slotT[s_]], eng=cast_engs[n % 3])
            for (off, size, vf, src, dst, key) in pieces:
                sch.op("act", lambda e: e.dma_start(out=dst, in_=slots[s_][:, off:off + size]), [slotT[s_]], [], dma_sem=f"st{s_}")

        bg_ctr = [0]
        bg_pending = [None]

        def bg_flush():
            if bg_pending[0] is None:
                return
            j, ch, base, l = bg_pending[0]
            bg_pending[0] = None
            tot = sum(pc[1] for pc in ch)
            CP(bgo_t[:, j, 0:tot], bgs_t[:, j, 0:tot], [bgsT[j]], [bgoT[j]], eng="pool")
            ts = []
            for (off, size, vf, src, dst, key) in ch:
                t = T("wdep")
                sch.op("pool", lambda e: e.dma_start(out=dst, in_=bgo_t[:, j, off - base:off - base + size]), [bgoT[j]], [t], dma_sem=f"bgo{j}")
                depT.setdefault((l,) + key, []).append(t)
                ts.append(t)
            for t in ts:
                t.w = (f"bgo{j}", sch.val[f"bgo{j}"])

        def convert_bg(l, pieces):
            chunks, cur, cs = [], [], 0
            for pc in pieces:
                if cs + pc[1] > 2048:
                    chunks.append(cur)
                    cur, cs = [], 0
                cur.append(pc)
                cs += pc[1]
            chunks.append(cur)
            for ch in chunks:
                n = bg_ctr[0]
                bg_ctr[0] += 1
                j = n % 2
                base = ch[0][0]
                for (off, size, vf, src, dst, key) in ch:
                    sch.op("pool", lambda e: e.dma_start(out=vf(bgs_t[:, j, off - base:off - base + size]), in_=src), [], [bgsT[j]], dma_sem=f"bgi{j}")
                bg_flush()
                bg_pending[0] = (j, ch, base, l)

        PP = {}
        WBP = {}
        for l in range(L):
            allp = list(page_pieces(l))
            PP[l] = allp[:NPG]
            WBP[l] = [pc for grp in allp[NPG:] for pc in grp]

        pgq = [(l, pg) for i in range(NT) for l in range(L) for pg in range(NPG)]
        wbq = [(l, e) for i in range(NT) for l in range(L) for e in range(8)]
        lp = [0]
        up_ = [0]
        wlp = [0]
        wup = [0]

        pend = [None]
        pend_wb = [None]

        def flush_pend():
            if pend[0] is None:
                return
            s_, l, pg, pieces = pend[0]
            pend[0] = None
            for h, (off, size, vf, src, dst, key) in enumerate(pieces):
                CP(slots[s_][:, off:off + size], bgs_t[:, h, :], [bgsT[h]], [slotT[s_]], eng=("act" if h == 0 else "dve"))
            ts = []
            for (off, size, vf, src, dst, key) in pieces:
                t = T("wdep")
                sch.op("sp", lambda e: e.dma_start(out=dst, in_=slots[s_][:, off:off + size]), [slotT[s_]], [t], dma_sem=f"st{s_}")
                ts.append(t)
            for t in ts:
                t.w = (f"st{s_}", sch.val[f"st{s_}"])
            depT[(l, "pg", pg)] = ts

        def flush_pend_wb():
            if pend_wb[0] is None:
                return
            s_, l, e_, pc = pend_wb[0]
            pend_wb[0] = None
            CP(wbs[s_][:], wbst_t[:], [wbstT], [wbsT[s_]], eng="pool")
            t = T("wdep")
            sch.op("sp", lambda e: e.dma_start(out=pc[4], in_=wbs[s_][:]), [wbsT[s_]], [t], dma_sem=f"wst{s_}")
            depT[(l, "wb", e_)] = [t]

        def issue_load():
            flush_pend()
            if lp[0] >= len(pgq):
                return
            l, pg = pgq[lp[0]]
            s = lp[0] % NSLOT
            if lp[0] < L * NPG:
                for h, (off, size, vf, src, dst, key) in enumerate(PP[l][pg]):
                    sch.op("sp", lambda e: e.dma_start(out=vf(bgs_t[:, h, :]), in_=src), [], [bgsT[h]], dma_sem=f"bgi{h}")
                pend[0] = (s, l, pg, PP[l][pg])
            else:
                sch.op("sp", lambda e: e.dma_start(out=slots[s][:], in_=wpg_d[l, pg]), [depT[(l, "pg", pg)]], [slotT[s]], dma_sem=f"slot{s}")
            lp[0] += 1

        def issue_wb():
            flush_pend_wb()
            if wlp[0] >= len(wbq):
                return
            l, e_ = wbq[wlp[0]]
            s = wlp[0] % 2
            if wlp[0] < L * 8:
                pc = WBP[l][e_]
                sch.op("sp", lambda e: e.dma_start(out=pc[2](wbst_t[:]), in_=pc[3]), [], [wbstT], dma_sem="wbi")
                pend_wb[0] = (s, l, e_, pc)
            else:
                sch.op("sp", lambda e: e.dma_start(out=wbs[s][:], in_=wbp_d[l, e_]), [depT[(l, "wb", e_)]], [wbsT[s]], dma_sem=f"wbs{s}")
            wlp[0] += 1

        def next_page():
            s = up_[0] % NSLOT
            up_[0] += 1
            return slots[s], slotT[s]

        def next_wb():
            s = wup[0] % 2
            wup[0] += 1
            return wbs[s], wbsT[s]

        if _STAGE >= 3:
            for _ in range(NSLOT):
                issue_load()
            for _ in range(2):
                issue_wb()

        def norm(src_ap, srcT, gcol, l, mode):
            pb, pbT = bank()
            for c in range(8):
                k = c % 2
                ACT(sqb_t[:, k, :], src_ap(c), AF.Square, [srcT[c]], [sqbT[k]])
                MM(pb[:], ones_bf, sqb_t[:, k, :], c == 0, c == 7, [cbfT, sqbT[k]], [pbT])
            ACT(rstd_t[:], pb[:], AF.Sqrt, [pbT], [rstdT], bias=EPS, scale=1.0 / D)
            sch.op("dve", lambda e: e.reciprocal(out=rstd_t[:], in_=rstd_t[:]), [rstdT], [rstdT])
            for c in range(8):
                g = pv_t[:, l, gcol + c:gcol + c + 1]
                if mode == "u":
                    STT(u_t[:, c, :], src_ap(c), g, rstd_t[:], ALU.mult, ALU.mult, [srcT[c], rstdT, cstT], [uT[c]])
                else:
                    k = 25 + c % 2
                    STT(tm(k, 0, 512), src_ap(c), g, rstd_t[:], ALU.mult, ALU.mult, [srcT[c], rstdT, cstT], [tmpT[k]])
                    TTOP(h_t[:, c, :], h_t[:, c, :], tm(k, 0, 512), ALU.add, [hT[c], tmpT[k]], [hT[c]], eng=("pool" if c % 2 else "dve"))

        EV = {}
        for c in range(2):
            b0 = c * 9
            EV[c] = dict(xa=b0, ga=b0 + 1, px=b0 + 2, q=b0 + 3, sgm=b0 + 4, sg=b0 + 5, sc=b0 + 6, sb=b0 + 7, z=b0 + 8)
        WK = list(range(18, 28))

        def proj_fm(sl, slT, jj, outb):
            for dc in range(8):
                MM(outb[0][:], sl[:, dc * 512 + jj * 128:dc * 512 + (jj + 1) * 128], u_t[:, dc, :], dc == 0, dc == 7,
                   [slT, uT[dc]], [outb[1]])

        pool_eng = ["pool"]

        def tile_layer(i, l):
            pool_eng[0] = "pool"
            pv = lambda a, b=None: pv_t[:, l, a:(a + 1 if b is None else b)]
            if l == 0:
                if i >= 2:
                    xin_ap = bgs_t[:].rearrange("p a (b d) -> p (a b) d", d=D)
                    xinT = [bgsT[0], bgsT[0], bgsT[1], bgsT[1]]
                else:
                    xin_ap, xinT = io_ap, ioT
                    sch.op("sp", lambda e: e.dma_start(out=io_ap, in_=x_d[i * TT:(i + 1) * TT, :].rearrange("(tb p) d -> p tb d", p=P)),
                           [], [ioT], dma_sem="xin")
                for c in range(8):
                    pb, pbT = bank()
                    for tb in range(4):
                        TR(pb[:, tb * 128:(tb + 1) * 128], xin_ap[:, tb, c * 128:(c + 1) * 128], [xinT[tb]], [pbT])
                    CP(h_t[:, c, :], pb[:], [pbT], [hT[c]], eng=("act" if c % 2 else "dve"))
            norm(lambda c: h_t[:, c, :], hT, 0, l, "u")
            if _STAGE < 4:
                raise _Stop()
            sl, slT = next_page()
            for c in range(2):
                e_ = EV[c]
                b = bank(); proj_fm(sl, slT, c, b)
                CP(tm(e_["xa"], 0, 3), halo_t[:, l, c, 0:3], [haloT[l][c]], [tmpT[e_["xa"]]])
                CP(tm(e_["xa"], 3, 515), b[0][:], [b[1]], [tmpT[e_["xa"]]], eng="act")
                CP(halo_t[:, l, c, 0:3], tm(e_["xa"], 512, 515), [tmpT[e_["xa"]]], [haloT[l][c]])
            for c in range(2):
                e_ = EV[c]
                b = bank(); proj_fm(sl, slT, 2 + c, b)
                ACT(tm(e_["ga"], 0, 512), b[0][:], AF.Gelu_apprx_tanh, [b[1]], [tmpT[e_["ga"]]])
            issue_load()
            sl, slT = next_page()
            for c in range(2):
                e_ = EV[c]
                b = bank(); proj_fm(sl, slT, c, b)
                CP(tm(e_["px"], 0, 15), halo_t[:, l, c, 3:18], [haloT[l][c]], [tmpT[e_["px"]]])
                CP(tm(e_["px"], 15, 527), b[0][:], [b[1]], [tmpT[e_["px"]]], eng="act")
                CP(halo_t[:, l, c, 3:18], tm(e_["px"], 512, 527), [tmpT[e_["px"]]], [haloT[l][c]])
            for c in range(2):
                e_ = EV[c]
                b = bank(); proj_fm(sl, slT, 2 + c, b)
                CP(tm(e_["q"], 0, 512), b[0][:], [b[1]], [tmpT[e_["q"]]], eng="act")
            issue_load()

            xc_k = [WK[0], WK[1]]
            for c in range(2):
                e_ = EV[c]
                xa = e_["xa"]; xc = xc_k[c]
                cw = lambda k: pv(32 + c * 4 + k)
                TS(tm(xc, 0, 512), tm(xa, 0, 512), cw(0), pv(40 + c), ALU.mult, ALU.add, [tmpT[xa], cstT], [tmpT[xc]])
                for k in range(1, 4):
                    STT(tm(xc, 0, 512), tm(xa, k, k + 512), cw(k), tm(xc, 0, 512), ALU.mult, ALU.add, [tmpT[xa], tmpT[xc], cstT], [tmpT[xc]])

            sl, slT = next_page()
            for c in range(2):
                e_ = EV[c]
                b = bank(); proj_fm(sl, slT, c, b)
                ACT(tm(e_["sgm"], 0, 512), b[0][:], AF.Sigmoid, [b[1]], [tmpT[e_["sgm"]]])
            for half in range(2):
                b = bank()
                for t2 in range(2):
                    tb = half * 2 + t2
                    for dc in range(8):
                        MM(b[0][:, t2 * 256:(t2 + 1) * 256], u_t[:, dc, tb * 128:(tb + 1) * 128], sl[:, dc * 512 + 256:dc * 512 + 512],
                           dc == 0, dc == 7, [slT, uT[dc]], [b[1]])
                CP(vtok_t[:, half * 2:half * 2 + 2, :], b[0][:].rearrange("p (a b) -> p a b", a=2), [b[1]], [vtokT])
                bv = b[0][:].rearrange("p (a h v) -> p a h v", a=2, h=4)
                for par in range(2):
                    for a_ in range(2):
                        CP(vz_t[:, half * 2 + a_, par::2, 64 * par:64 * par + 64], bv[:, a_, par::2, :], [b[1]], [vzT], eng=("act" if a_ else "dve"))
            issue_load()

            for c in range(2):
                e_ = EV[c]
                xc = xc_k[c]
                r_, i_, a2_, uu_, hs_ = WK[2], WK[3], WK[4], WK[5], WK[6]
                br = bank()
                MM(br[0][:], bd_t[:, l, 0 + c, :], tm(xc, 0, 512), True, True, [cstT, tmpT[xc]], [br[1]])
                bi = bank()
                MM(bi[0][:], bd_t[:, l, 2 + c, :], tm(xc, 0, 512), True, True, [cstT, tmpT[xc]], [bi[1]])
                ACT(tm(r_, 0, 512), br[0][:], AF.Sigmoid, [br[1], cstT], [tmpT[r_]], bias=pv(42 + c))
                ACT(tm(i_, 0, 512), bi[0][:], AF.Sigmoid, [bi[1], cstT], [tmpT[i_]], bias=pv(44 + c))
                ACT(tm(r_, 0, 512), tm(r_, 0, 512), AF.Exp, [tmpT[r_], dvT], [tmpT[r_]], scale=dv_t[:, l, c:c + 1])
                ACT(tm(a2_, 0, 512), tm(r_, 0, 512), AF.Square, [tmpT[r_]], [tmpT[a2_]])
                ACT(tm(a2_, 0, 512), tm(a2_, 0, 512), AF.Sqrt, [tmpT[a2_]], [tmpT[a2_]], bias=1.0, scale=-1.0)
                TTOP(tm(uu_, 0, 512), tm(a2_, 0, 512), tm(i_, 0, 512), ALU.mult, [tmpT[a2_], tmpT[i_]], [tmpT[uu_]])
                TTOP(tm(uu_, 0, 512), tm(uu_, 0, 512), tm(xc, 0, 512), ALU.mult, [tmpT[uu_], tmpT[xc]], [tmpT[uu_]])
                sch.op("dve", lambda e: e.tensor_tensor_scan(out=tm(hs_, 0, 512), data0=tm(r_, 0, 512), data1=tm(uu_, 0, 512),
                                                              initial=hst_t[:, l, c:c + 1], op0=ALU.mult, op1=ALU.add),
                       [tmpT[r_], tmpT[uu_], hstT[l][c]], [tmpT[hs_]])
                CP(hst_t[:, l, c:c + 1], tm(hs_, 511, 512), [tmpT[hs_]], [hstT[l][c]])
                TTOP(ysmg_t[:, 0 + c, :], tm(hs_, 0, 512), tm(e_["ga"], 0, 512), ALU.mult, [tmpT[hs_], tmpT[e_["ga"]]], [ysT[0 + c]])

            sl, slT = next_page()
            for c in range(2):
                e_ = EV[c]
                b = bank(); proj_fm(sl, slT, c, b)
                ACT(tm(e_["sg"], 0, 512), b[0][:], AF.Silu, [b[1]], [tmpT[e_["sg"]]])
            for c in range(2):
                e_ = EV[c]
                b = bank(); proj_fm(sl, slT, 2 + c, b)
                CP(tm(e_["sb"], 0, 512), b[0][:], [b[1]], [tmpT[e_["sb"]]], eng="act")
            issue_load()

            for c in range(2):
                e_ = EV[c]
                px = e_["px"]
                s2, s4, s8, s16, dd = WK[2], WK[3], WK[4], WK[5], WK[6]
                TTOP(tm(s2, 1, 527), tm(px, 1, 527), tm(px, 0, 526), ALU.add, [tmpT[px]], [tmpT[s2]])
                if c == 0:
                    TTOP(tmp_t[64:128, s4 * TW + 3:s4 * TW + 527], tmp_t[64:128, s2 * TW + 3:s2 * TW + 527],
                         tmp_t[64:128, s2 * TW + 1:s2 * TW + 525], ALU.add, [tmpT[s2]], [tmpT[s4]])
                    sel = [(0, 64, s2, 0.5), (64, 128, s4, 0.25)]
                else:
                    TTOP(tm(s4, 3, 527), tm(s2, 3, 527), tm(s2, 1, 525), ALU.add, [tmpT[s2]], [tmpT[s4]])
                    TTOP(tm(s8, 7, 527), tm(s4, 7, 527), tm(s4, 3, 523), ALU.add, [tmpT[s4]], [tmpT[s8]])
                    TTOP(tmp_t[64:128, s16 * TW + 15:s16 * TW + 527], tmp_t[64:128, s8 * TW + 15:s8 * TW + 527],
                         tmp_t[64:128, s8 * TW + 7:s8 * TW + 519], ALU.add, [tmpT[s8]], [tmpT[s16]])
                    sel = [(0, 64, s8, 0.125), (64, 128, s16, 0.0625)]
                for (p0, p1, sk, inv) in sel:
                    STT(tmp_t[p0:p1, dd * TW:dd * TW + 512], tmp_t[p0:p1, sk * TW + 15:sk * TW + 527], inv,
                        tmp_t[p0:p1, px * TW + 15:px * TW + 527], ALU.mult, ALU.subtract, [tmpT[sk], tmpT[px]], [tmpT[dd]])
                    if i == 0:
                        TTOP(tmp_t[p0:p1, dd * TW:dd * TW + 15], tmp_t[p0:p1, sk * TW + 15:sk * TW + 30], invc[p0:p1, c, 0:15],
                             ALU.mult, [tmpT[sk], cstT, tmpT[dd]], [tmpT[dd]])
                        TTOP(tmp_t[p0:p1, dd * TW:dd * TW + 15], tmp_t[p0:p1, dd * TW:dd * TW + 15], tmp_t[p0:p1, px * TW + 15:px * TW + 30],
                             ALU.subtract, [tmpT[px], tmpT[dd]], [tmpT[dd]])
                bp = bank()
                MM(bp[0][:], bd_t[:, l, 4 + c, :], tm(dd, 0, 512), True, True, [cstT, tmpT[dd]], [bp[1]])
                ACT(ysmg_t[:, 2 + c, :], bp[0][:], AF.Copy, [bp[1], cstT], [ysT[2 + c]], scale=pv(48 + c))

            sl, slT = next_page()
            for c in range(2):
                e_ = EV[c]
                b = bank(); proj_fm(sl, slT, c, b)
                CP(tm(e_["sc"], 0, 512), b[0][:], [b[1]], [tmpT[e_["sc"]]], eng="act")
            for c in range(2):
                e_ = EV[c]
                z = e_["z"]
                b = bank(); proj_fm(sl, slT, 2 + c, b)
                CP(tm(z, 0, 2), halo_t[:, l, c, 18:20], [haloT[l][c]], [tmpT[z]])
                TTOP(tm(z, 2, 514), b[0][:], tm(e_["sc"], 0, 512), ALU.mult, [b[1], tmpT[e_["sc"]]], [tmpT[z]])
                CP(halo_t[:, l, c, 18:20], tm(z, 512, 514), [tmpT[z]], [haloT[l][c]])
            issue_load()
            for c in range(2):
                e_ = EV[c]
                z = e_["z"]; acc = WK[2 + c]
                sw = lambda k: pv(55 + c * 3 + k)
                TS(tm(acc, 0, 512), tm(z, 0, 512), sw(0), None, ALU.mult, None, [tmpT[z], cstT], [tmpT[acc]])
                STT(tm(acc, 0, 512), tm(z, 1, 513), sw(1), tm(acc, 0, 512), ALU.mult, ALU.add, [tmpT[z], tmpT[acc], cstT], [tmpT[acc]])
                STT(tm(acc, 0, 512), tm(z, 2, 514), sw(2), tm(acc, 0, 512), ALU.mult, ALU.add, [tmpT[z], tmpT[acc], cstT], [tmpT[acc]])
                TTOP(ysmg_t[:, 6 + c, :], tm(acc, 0, 512), tm(e_["sb"], 0, 512), ALU.mult, [tmpT[acc], tmpT[e_["sb"]]], [ysT[6 + c]])

            if _STAGE < 5:
                raise _Stop()

            def hgrn_steps():
                for hc in range(2):
                    e_ = EV[hc]
                    q, fk, sg = e_["q"], e_["sgm"], e_["sg"]
                    w0, w1, w2, w3, w4 = WK[0], WK[1], WK[4], WK[5], WK[6]
                    hb = [WK[7], WK[8], WK[2], WK[3], WK[9]]
                    qe_s, qe_i, ke_i, osq = tmb(hb[0], 0), tmb(hb[0], 1), tmb(hb[1], 0), tmb(hb[1], 1)
                    kA, kB = tmb(hb[2], 0), tmb(hb[2], 1)
                    scm = [tmb(hb[3], 0), tmb(hb[3], 1)]
                    sbd = tmp_t[:, hb[4] * TW:hb[4] * TW + 512].bitcast(BF16)
                    v3 = lambda k: tm(k, 0, 512).rearrange("p (n t) -> p n t", t=64)
                    lb = dv_t[:, l, 2 + hc:3 + hc]
                    omlb = dv_t[:, l, 4 + hc:5 + hc]
                    TS(tm(fk, 0, 512), tm(fk, 0, 512), omlb, lb, ALU.mult, ALU.add, [tmpT[fk], dvT], [tmpT[fk]])
                    ACT(tm(w0, 0, 512), tm(fk, 0, 512), AF.Ln, [tmpT[fk]], [tmpT[w0]])
                    TS(tm(fk, 0, 512), tm(fk, 0, 512), -1.0, 1.0, ALU.mult, ALU.add, [tmpT[fk]], [tmpT[fk]])
                    sch.op("dve", lambda e: e.tensor_tensor_scan(out=tm(w1, 0, 512), data0=nstart, data1=tm(w0, 0, 512), initial=0.0,
                                                                  op0=ALU.mult, op1=ALU.add), [cstT, tmpT[w0]], [tmpT[w1]])
                    blast = v3(w1)[:, :, 63]
                    bmid = v3(w1)[:, :, 31]
                    TTOP(v3(w0), blast.unsqueeze(2).to_broadcast([P, 8, 64]), v3(w1), ALU.subtract, [tmpT[w1], tmpT[w0]], [tmpT[w0]])
                    ACT(tm(w2, 0, 512), tm(w1, 0, 512), AF.Exp, [tmpT[w1]], [tmpT[w2]])
                    ACT(tm(w0, 0, 512), tm(w0, 0, 512), AF.Exp, [tmpT[w0]], [tmpT[w0]])
                    ACT(sm_t[:, 0, :], bmid, AF.Exp, [tmpT[w1]], [smT], scale=-1.0)
                    TTOP(sm_t[:, 1, :], bmid, blast, ALU.subtract, [tmpT[w1]], [smT])
                    ACT(sm_t[:, 2, :], sm_t[:, 1, :], AF.Exp, [smT], [smT])
                    TTOP(qe_s, tm(q, 0, 512), tm(w2, 0, 512), ALU.mult, [tmpT[q], tmpT[w2]], [tmpT[hb[0]]])
                    TTOP(tm(w0, 0, 512), tm(fk, 0, 512), tm(w0, 0, 512), ALU.mult, [tmpT[fk], tmpT[w0]], [tmpT[w0]])
                    TTOP(qe_i.rearrange("p (n t) -> p n t", t=64), qe_s.rearrange("p (n t) -> p n t", t=64),
                         sm_t[:, 0, :].unsqueeze(2).to_broadcast([P, 8, 64]), ALU.mult, [tmpT[hb[0]], smT], [tmpT[hb[0]]])
                    TTOP(ke_i.rearrange("p (n t) -> p n t", t=64), v3(w0),
                         sm_t[:, 2, :].unsqueeze(2).to_broadcast([P, 8, 64]), ALU.mult, [tmpT[w0], smT], [tmpT[hb[1]]])
                    dec = v3(w2)[:, :, 63]
                    d3 = tm(w3, 0, 512).rearrange("p (v n) -> p v n", n=8)
                    CP(d3, dec.unsqueeze(1).to_broadcast([P, 64, 8]), [tmpT[w2]], [tmpT[w3]], eng=pool_eng[0])
                    sch.op(pool_eng[0], lambda e: e.memset(d3[:, :, 0], 0.0), [], [tmpT[w3]])
                    yield
                    bt = bank()
                    for tb in range(4):
                        TR(bt[0][:, tb * 128:(tb + 1) * 128], tm(w0, tb * 128, (tb + 1) * 128), [tmpT[w0]], [bt[1]])
                    yield
                    sch.op(pool_eng[0], lambda e: e.memset(kA[64:128, :], 0.0), [], [tmpT[hb[2]]])
                    sch.op(pool_eng[0], lambda e: e.memset(kB[0:64, :], 0.0), [], [tmpT[hb[2]]])
                    CP(kA[0:64, :], bt[0][0:64, :], [bt[1]], [tmpT[hb[2]]], eng="dve")
                    CP(kB[64:128, :], bt[0][64:128, :], [bt[1]], [tmpT[hb[2]]], eng="act")
                    bus = [bank(), bank()]
                    for n in range(8):
                        km = kA if n % 2 == 0 else kB
                        j = n // 2
                        bu = bus[n // 4]
                        MM(bu[0][:, (n % 4) * 128:(n % 4 + 1) * 128], km[:, j * 128:(j + 1) * 128], vtok_t[:, j, hc * 128:(hc + 1) * 128], True, True,
                           [tmpT[hb[2]], vtokT], [bu[1]])
                    yield
                    u3 = tm(w1, 0, 512).rearrange("p (v n) -> p v n", n=8)
                    for bi_ in range(2):
                        for hp in range(2):
                            src = bus[bi_][0][64 * hp:64 * hp + 64, :].rearrange("p (n c) -> p n c", c=128)[:, :, 64 * hp:64 * hp + 64]
                            CP(u3[64 * hp:64 * hp + 64, :, 4 * bi_:4 * bi_ + 4], src.rearrange("p n v -> p v n"), [bus[bi_][1], tmpT[w1]], [tmpT[w1]],
                               eng=("dve" if hp == 0 else "act"))
                    STT(u3[:, :, 0], sst_t[:, l, hc, :], tm(w2, 63, 64), u3[:, :, 0], ALU.mult, ALU.add, [sstT[l][hc], tmpT[w2], tmpT[w1]], [tmpT[w1]])
                    sch.op("dve", lambda e: e.tensor_tensor_scan(out=tm(w4, 0, 512), data0=tm(w3, 0, 512), data1=tm(w1, 0, 512), initial=0.0,
                                                                  op0=ALU.mult, op1=ALU.add), [tmpT[w3], tmpT[w1]], [tmpT[w4]])
                    s3 = tm(w4, 0, 512).rearrange("p (v n) -> p v n", n=8)
                    sbd3 = sbd.rearrange("p (n c) -> p n c", c=128)
                    sch.op(pool_eng[0], lambda e: e.memset(sbd, 0.0), [], [tmpT[hb[4]]])
                    for hp in range(2):
                        ps_ = slice(64 * hp, 64 * hp + 64)
                        CP(sbd3[ps_, 0, 64 * hp:64 * hp + 64], sst_t[ps_, l, hc, :], [sstT[l][hc]], [tmpT[hb[4]]])
                        CP(sbd3[ps_, 1:8, 64 * hp:64 * hp + 64], s3[ps_, :, 0:7].rearrange("p v n -> p n v"), [tmpT[w4]], [tmpT[hb[4]]])
                    CP(sst_t[:, l, hc, :], s3[:, :, 7], [tmpT[w4]], [sstT[l][hc]])
                    bsb = [bank(), bank()]
                    for hp in range(2):
                        for j in range(4):
                            MM(bsb[hp][0][:, j * 128:(j + 1) * 128], ke_i[64 * hp:64 * hp + 64, j * 128:(j + 1) * 128],
                               qe_i[64 * hp:64 * hp + 64, j * 128:(j + 1) * 128], True, True, [tmpT[hb[0]], tmpT[hb[1]]], [bsb[hp][1]])
                    for hp in range(2):
                        TTOP(scm[hp].rearrange("p (a t) -> p a t", t=128), bsb[hp][0][:].rearrange("p (a t) -> p a t", t=128),
                             cmask2.unsqueeze(1).to_broadcast([P, 4, 128]), ALU.mult, [bsb[hp][1], cstT], [tmpT[hb[3]]])
                    yield
                    bo = bank()
                    for j in range(4):
                        out_ = bo[0][:, j * 128:(j + 1) * 128]
                        MM(out_, vz_t[:, j, 2 * hc, :], scm[0][:, j * 128:(j + 1) * 128], True, False, [vzT, tmpT[hb[3]]], [bo[1]])
                        MM(out_, vz_t[:, j, 2 * hc + 1, :], scm[1][:, j * 128:(j + 1) * 128], False, False, [vzT, tmpT[hb[3]]], [bo[1]])
                        for n in (2 * j, 2 * j + 1):
                            MM(bo[0][:, n * 64:(n + 1) * 64], sbd3[:, n, :], qe_s[:, n * 64:(n + 1) * 64], False, n == 2 * j + 1,
                               [tmpT[hb[4]], tmpT[hb[0]]], [bo[1]])
                    yield
                    ACT(osq, bo[0][:], AF.Square, [bo[1]], [tmpT[hb[1]]])
                    bn = bank()
                    MM(bn[0][:], bones_bf, osq, True, True, [cbfT, tmpT[hb[1]]], [bn[1]])
                    ACT(tm(w3, 0, 512), bn[0][:], AF.Sqrt, [bn[1]], [tmpT[w3]], bias=EPS, scale=1.0 / 64)
                    sch.op("dve", lambda e: e.reciprocal(out=tm(w3, 0, 512), in_=tm(w3, 0, 512)), [tmpT[w3]], [tmpT[w3]])
                    TTOP(tm(w4, 0, 512), bo[0][:], tm(w3, 0, 512), ALU.mult, [bo[1], tmpT[w3]], [tmpT[w4]])
                    STT(ysmg_t[:, 4 + hc, :], tm(w4, 0, 512), pv(54), tm(sg, 0, 512), ALU.mult, ALU.mult, [tmpT[w4], tmpT[sg], cstT], [ysT[4 + hc]])
                    yield

            if _STAGE < 6:
                raise _Stop()
            hg = hgrn_steps()
            next(hg, None)
            slot_i = 0
            for ki, k in enumerate((0, 1, 3, 2)):
                if ki == 3:
                    for _ in hg:
                        pass
                for hh in range(2):
                    sl, slT = next_page()
                    wsl, wslT = next_wb()
                    for e4 in range(4):
                        e = hh * 4 + e4
                        gb = bank()
                        for dc in range(8):
                            MM(gb[0][:], sl[:, dc * 512 + e4 * 128:dc * 512 + (e4 + 1) * 128], u_t[:, dc, :], dc == 0, dc == 7, [slT, uT[dc]], [gb[1]])
                        bb = bank()
                        for cc in range(2):
                            MM(bb[0][:], wsl[:, cc * 512 + e4 * 128:cc * 512 + (e4 + 1) * 128], ysmg_t[:, k * 2 + cc, :], cc == 0, cc == 1,
                               [wslT, ysT[k * 2 + cc]], [bb[1]])
                        gs = EV[slot_i % 2]["xa"]
                        mt = EV[slot_i % 2]["ga"]
                        ACT(tm(gs, 0, 512), gb[0][:], AF.Sigmoid, [gb[1]], [tmpT[gs]])
                        if ki == 0:
                            TTOP(mix_t[:, e, :], bb[0][:], tm(gs, 0, 512), ALU.mult, [bb[1], tmpT[gs]], [mixT[e]])
                        else:
                            TTOP(tm(mt, 0, 512), bb[0][:], tm(gs, 0, 512), ALU.mult, [bb[1], tmpT[gs]], [tmpT[mt]])
                            if ki < 3:
                                TTOP(mix_t[:, e, :], mix_t[:, e, :], tm(mt, 0, 512), ALU.add, [mixT[e], tmpT[mt]], [mixT[e]], eng=pool_eng[0])
                            else:
                                TTOP(ysmg_t[:, 8 + e, :], mix_t[:, e, :], tm(mt, 0, 512), ALU.add, [mixT[e], tmpT[mt]], [mgT[e]], eng=pool_eng[0])
                        slot_i += 1
                        if ki < 3 and slot_i % 2 == 0:
                            next(hg, None)
                    issue_load()
                    issue_wb()
            for hh in range(2):
                sl, slT = next_page()
                for e2 in range(4):
                    ec = hh * 4 + e2
                    b = bank()
                    for dc in range(8):
                        MM(b[0][:], sl[:, dc * 512 + e2 * 128:dc * 512 + (e2 + 1) * 128], ysmg_t[:, 8 + dc, :], dc == 0, dc == 7,
                           [slT, mgT[dc]], [b[1]])
                    CP(mix_t[:, ec, :], b[0][:], [b[1]], [mixT[ec]], eng="act")
                issue_load()
            norm(lambda c: mix_t[:, c, :], mixT, 8, l, "res")

            if _STAGE < 7:
                raise _Stop()
            if l == L - 1 and 1 <= i < NT - 1:
                sch.op("sp", lambda e: e.dma_start(out=bgs_t[:].rearrange("p a (b d) -> p (a b) d", d=D),
                                                   in_=x_d[(i + 1) * TT:(i + 2) * TT, :].rearrange("(tb p) d -> p tb d", p=P)),
                       [], [bgsT], dma_sem="xin")
            norm(lambda c: h_t[:, c, :], hT, 16, l, "u")
            for g in range(8):
                sl, slT = next_page()
                for j in range(4):
                    fc = g * 4 + j
                    b = bank()
                    for dc in range(8):
                        MM(b[0][:], sl[:, dc * 512 + j * 128:dc * 512 + (j + 1) * 128], u_t[:, dc, :], dc == 0, dc == 7, [slT, uT[dc]], [b[1]])
                    rl = WK[fc % 4]
                    ACT(tm(rl, 0, 512), b[0][:], AF.Relu, [b[1]], [tmpT[rl]])
                    TTOP(hid_ap[:, fc, :], b[0][:], tm(rl, 0, 512), ALU.mult, [b[1], tmpT[rl]], [hidT[fc]])
                issue_load()
            for e in range(8):
                sl, slT = next_page()
                b = bank()
                for fc in range(32):
                    MM(b[0][:], sl[:, fc * 128:(fc + 1) * 128], hid_ap[:, fc, :], fc == 0, fc == 31, [slT, hidT[fc]], [b[1]])
                CP(mix_t[:, e, :], b[0][:], [b[1]], [mixT[e]], eng="act")
                issue_load()
            norm(lambda c: mix_t[:, c, :], mixT, 24, l, "res")

            if l == L - 1:
                for tb in range(4):
                    for half in range(2):
                        b = bank()
                        for cc in range(4):
                            c = half * 4 + cc
                            TR(b[0][:, cc * 128:(cc + 1) * 128], h_t[:, c, tb * 128:(tb + 1) * 128], [hT[c]], [b[1]])
                        CP(io_ap[:, tb, half * 512:(half + 1) * 512], b[0][:], [b[1]], [ioT[tb]], eng=("act" if half else "dve"))
                sch.op("sp", lambda e: e.dma_start(out=y_d[i * TT:(i + 1) * TT, :].rearrange("(tb p) d -> p tb d", p=P), in_=io_ap),
                       [ioT], [], dma_sem="yout")

        try:
            if _STAGE < 3:
                raise _Stop()
            for i in range(NT):
                for l in range(L):
                    tile_layer(i, l)
            flush_pend()
            flush_pend_wb()
        except _Stop:
            for nm in sch.sem:
                if sch.val[nm] > 0:
                    nc.sync.wait_ge(sch.sem[nm], sch.val[nm])
        nc.sync.wait_ge(sch.sem["yout"], sch.val["yout"])
        if dbg:
            print("sem values:", sch.val)
    return nc


def _cols(v):
    v = np.asarray(v, np.float32).reshape(-1, P)
    return v.T


def host_consts():
    c = np.zeros((P, NCST), np.float32)
    c[:, 0:128] = np.eye(P, dtype=np.float32)
    c[:, 128:256] = 1.0
    p = np.arange(P)
    c[:, 256:384] = (p[:, None] // 64 == p[None, :] // 64).astype(np.float32)
    c[:, 384:512] = ((p[:, None] // 64 == p[None, :] // 64) & ((p[:, None] % 64) <= (p[None, :] % 64))).astype(np.float32)
    t = np.arange(512)
    c[:, 512:1024] = (t % 64 != 0).astype(np.float32)[None, :]
    wins = np.array([[2, 4], [8, 16]])
    for ch in range(2):
        w = wins[ch][p // 64].astype(np.float32)
        tt = np.arange(16, dtype=np.float32)[None, :] + 1.0
        c[:, 1024 + ch * 16:1024 + (ch + 1) * 16] = 1.0 / np.minimum(tt, w[:, None])
    return c


def host_params(inp, L):
    pv = np.zeros((P, L, NV), np.float32)
    bd = np.zeros((P, L, 6, P), np.float32)
    for l in range(L):
        pv[:, l, 0:8] = _cols(inp["norm_mix_pre"][l])
        pv[:, l, 8:16] = _cols(inp["norm_mix_post"][l])
        pv[:, l, 16:24] = _cols(inp["norm_mlp_pre"][l])
        pv[:, l, 24:32] = _cols(inp["norm_mlp_post"][l])
        cw = np.asarray(inp["lru_conv_w"][l], np.float32)
        for c in range(2):
            for k in range(4):
                pv[:, l, 32 + c * 4 + k] = cw[k, c * 128:(c + 1) * 128]
        pv[:, l, 40:42] = _cols(inp["lru_conv_b"][l])
        pv[:, l, 42:44] = _cols(inp["lru_b_a"][l])
        pv[:, l, 44:46] = _cols(inp["lru_b_x"][l])
        pv[:, l, 46:48] = _cols(inp["lru_lambda"][l])
        pv[:, l, 48:50] = _cols(inp["pool_scale"][l])
        pv[:, l, 50:52] = _cols(inp["hgrn_lower_bound"][0])
        pv[:, l, 52:54] = _cols(inp["hgrn_lower_bound"][min(1, L - 1)])
        pv[:, l, 54] = np.tile(np.asarray(inp["hgrn_norm"][l], np.float32), 2)
        sw = np.asarray(inp["sconv_w"][l], np.float32)
        for c in range(2):
            for k in range(3):
                pv[:, l, 55 + c * 3 + k] = sw[k, c * 128:(c + 1) * 128]
        wa = np.asarray(inp["lru_w_a"][l], np.float32)
        wx = np.asarray(inp["lru_w_x"][l], np.float32)
        pw = np.asarray(inp["pool_w"][l], np.float32)
        for c in range(2):
            for n in range(4):
                bd[n * 32:(n + 1) * 32, l, 0 + c, n * 32:(n + 1) * 32] = wa[c * 4 + n]
                bd[n * 32:(n + 1) * 32, l, 2 + c, n * 32:(n + 1) * 32] = wx[c * 4 + n]
            for n in range(2):
                bd[n * 64:(n + 1) * 64, l, 4 + c, n * 64:(n + 1) * 64] = pw[c * 2 + n]
    return pv, bd


def make_in_maps(inp, n_cores, L):
    pv, bd = host_params(inp, L)
    cst = host_consts()
    f = lambda a: np.ascontiguousarray(np.asarray(a, np.float32))
    shared = {
        "w_in": f(inp["w_in"])[:L], "w_branch": f(inp["w_branch"])[:L].reshape(L, D, D), "w_out": f(inp["w_out"])[:L],
        "w_up": f(inp["w_up"])[:L], "w_down": f(inp["w_down"])[:L], "pvec": pv, "bdw": bd, "cst": cst,
    }
    x = f(inp["x"])
    return [dict(shared, x=np.ascontiguousarray(x[b])) for b in range(n_cores)]


def kernel(**inputs):
    B, S, _ = inputs["x"].shape
    L = inputs["w_in"].shape[0]
    nc = build_program(S // TT, L)
    in_maps = make_in_maps(inputs, B, L)
    res = run_bass_kernel_spmd(nc, in_maps, core_ids=list(range(B)))
    return np.stack([np.asarray(r["y"], np.float32) for r in res.results], axis=0)
```
